# Optimizing a Trainium2 kernel written in Bass

```python
import math
import jax
import jax.numpy as jnp
from jax import lax
import numpy as np

D_MODEL = 1024
BATCH = 4
SEQ = 8192
DEPTH = 2

GRID_W = 64
CTX_LEN = 256
NORM_EPS = 1e-6
N_BRANCHES = 4

S5_WIDTH = 256
S5_GROUP_SIZE = 16
S5_GROUPS = S5_WIDTH // S5_GROUP_SIZE
S5_STATE = 64
S5_DT_MIN = 1e-3
S5_DT_MAX = 1e-1

GLA_HEADS = 4
GLA_DK = 64
GLA_DV = 128
GLA_GATE_RANK = 16
GLA_TAU = 16.0
GLA_CHUNK = 64
ROPE_BASE = 10000.0

NA_HEADS = 4
NA_HEAD_DIM = 64
NA_WIN_ROWS = 8
NA_WIN_COLS = 16

CONV_WIDTH = 256
CONV_KERNEL = 31

MOE_GROUPS = 4
MOE_EXPERTS_PER_GROUP = 8
MOE_TOP_K = 2
MOE_HIDDEN = 256

IN_PARTS = (
    ('s5_u', S5_WIDTH),
    ('gla_q', GLA_HEADS * GLA_DK),
    ('gla_k', GLA_HEADS * GLA_DK),
    ('gla_v', GLA_HEADS * GLA_DV),
    ('gla_r', GLA_HEADS * GLA_DV),
    ('gla_a', 2 * GLA_GATE_RANK),
    ('na_q', NA_HEADS * NA_HEAD_DIM),
    ('na_k', NA_HEADS * NA_HEAD_DIM),
    ('na_v', NA_HEADS * NA_HEAD_DIM),
    ('conv_in', 2 * CONV_WIDTH),
    ('gates', N_BRANCHES * D_MODEL),
)
IN_WIDTH = sum(width for _, width in IN_PARTS)
ALL_PARTS = tuple(name for name, _ in IN_PARTS)
CTX_STATE_PARTS = ('s5_u', 'gla_k', 'gla_v', 'gla_a', 'na_k', 'na_v')

kernel_name = 'hybrid_prefix_ssm_gla_natten_conv_hmoe'


def _orient(a, reverse):
    return jnp.flip(a, axis=1) if reverse else a


def rms_norm(x, g):
    xf = x.astype(jnp.float32)
    y = xf * lax.rsqrt(jnp.mean(xf * xf, axis=-1, keepdims=True) + NORM_EPS)
    return (y * g.astype(jnp.float32)).astype(x.dtype)


def layer_norm(x, g, b):
    xf = x.astype(jnp.float32)
    xc = xf - jnp.mean(xf, axis=-1, keepdims=True)
    y = xc * lax.rsqrt(jnp.mean(xc * xc, axis=-1, keepdims=True) + NORM_EPS)
    return (y * g.astype(jnp.float32) + b.astype(jnp.float32)).astype(x.dtype)


def modulate(x, g, shift, scale):
    return rms_norm(x, g) * (1 + scale) + shift


def in_proj(h, w_in, names):
    offsets = {}
    start = 0
    for name, width in IN_PARTS:
        offsets[name] = (start, width)
        start += width
    if tuple(names) == ALL_PARTS:
        w = w_in
    else:
        w = jnp.concatenate([w_in[:, offsets[n][0]:offsets[n][0] + offsets[n][1]] for n in names], axis=1)
    y = h @ w
    out = {}
    col = 0
    for n in names:
        width = offsets[n][1]
        out[n] = y[..., col:col + width]
        col += width
    return out


def s5_discretise(lam_re, lam_im, log_dt, b_re, b_im):
    lr = lam_re.astype(jnp.float32)
    li = lam_im.astype(jnp.float32)
    dt = jnp.exp(log_dt.astype(jnp.float32))[:, None]
    mag = jnp.exp(lr * dt)
    abar_re = mag * jnp.cos(li * dt)
    abar_im = mag * jnp.sin(li * dt)
    den = lr * lr + li * li
    nr = abar_re - 1.0
    ni = abar_im
    coef_re = (nr * lr + ni * li) / den
    coef_im = (ni * lr - nr * li) / den
    br = b_re.astype(jnp.float32)
    bi = b_im.astype(jnp.float32)
    bbar_re = coef_re[..., None] * br - coef_im[..., None] * bi
    bbar_im = coef_re[..., None] * bi + coef_im[..., None] * br
    return abar_re, abar_im, bbar_re, bbar_im


def _ssm_combine(e1, e2):
    a1r, a1i, b1r, b1i = e1
    a2r, a2i, b2r, b2i = e2
    ar = a1r * a2r - a1i * a2i
    ai = a1r * a2i + a1i * a2r
    br = a2r * b1r - a2i * b1i + b2r
    bi = a2r * b1i + a2i * b1r + b2i
    return ar, ai, br, bi


def s5_states(u, abar_re, abar_im, bbar_re, bbar_im, h0_re, h0_im):
    bu_re = jnp.einsum('blgh,gph->blgp', u, bbar_re)
    bu_im = jnp.einsum('blgh,gph->blgp', u, bbar_im)
    if h0_re is not None:
        bu_re = bu_re.at[:, 0].add(abar_re * h0_re - abar_im * h0_im)
        bu_im = bu_im.at[:, 0].add(abar_re * h0_im + abar_im * h0_re)
    length = u.shape[1]
    a_re = jnp.broadcast_to(abar_re, (1, length) + abar_re.shape)
    a_im = jnp.broadcast_to(abar_im, (1, length) + abar_im.shape)
    _, _, h_re, h_im = lax.associative_scan(_ssm_combine, (a_re, a_im, bu_re, bu_im), axis=1)
    return h_re, h_im


def s5_readout(h_re, h_im, c_re, c_im):
    return jnp.einsum('blgp,ghp->blgh', h_re, c_re) - jnp.einsum('blgp,ghp->blgh', h_im, c_im)


def s5_output(y, lp):
    z = jax.nn.gelu(y)
    z = z * jax.nn.sigmoid(z @ lp['s5_w_glu'].astype(jnp.float32) + lp['s5_b_glu'].astype(jnp.float32))
    return z.astype(lp['s5_w_out'].dtype) @ lp['s5_w_out']


def s5_mixer(u, uc, lp, ctx_out):
    bsz, length, _ = u.shape
    lc = uc.shape[1]
    uf = u.astype(jnp.float32)
    ucf = uc.astype(jnp.float32)
    ug = uf.reshape(bsz, length, S5_GROUPS, S5_GROUP_SIZE)
    ucg = ucf.reshape(bsz, lc, S5_GROUPS, S5_GROUP_SIZE)
    d_skip = lp['s5_d'].astype(jnp.float32)
    y = uf * d_skip
    yc = ucf * d_skip if ctx_out else None
    for direction in range(2):
        rev = direction == 1
        abr, abi, bbr, bbi = s5_discretise(lp['s5_lam_re'][direction], lp['s5_lam_im'][direction],
                                           lp['s5_log_dt'][direction], lp['s5_b_re'][direction],
                                           lp['s5_b_im'][direction])
        c_re = lp['s5_c_re'][direction].astype(jnp.float32)
        c_im = lp['s5_c_im'][direction].astype(jnp.float32)
        hcr, hci = s5_states(_orient(ucg, rev), abr, abi, bbr, bbi, None, None)
        hr, hi = s5_states(_orient(ug, rev), abr, abi, bbr, bbi, hcr[:, -1], hci[:, -1])
        y = y + _orient(s5_readout(hr, hi, c_re, c_im), rev).reshape(bsz, length, S5_WIDTH)
        if ctx_out:
            yc = yc + _orient(s5_readout(hcr, hci, c_re, c_im), rev).reshape(bsz, lc, S5_WIDTH)
    out = s5_output(y, lp)
    outc = s5_output(yc, lp) if ctx_out else None
    return out, outc


def axial_rope_angles(length, dim):
    t = jnp.arange(length, dtype=jnp.int32)
    row = (t // GRID_W).astype(jnp.float32)
    col = (t % GRID_W).astype(jnp.float32)
    half = dim // 2
    inv_freq = ROPE_BASE ** (-jnp.arange(0, half, 2, dtype=jnp.float32) / half)
    return row[:, None] * inv_freq, col[:, None] * inv_freq


def _rotate(x, ang):
    f = x.shape[-1] // 2
    cos = jnp.cos(ang)[:, None, :]
    sin = jnp.sin(ang)[:, None, :]
    x1 = x[..., :f]
    x2 = x[..., f:]
    return jnp.concatenate([x1 * cos - x2 * sin, x2 * cos + x1 * sin], axis=-1)


def apply_axial_rope(x, ang_row, ang_col):
    half = x.shape[-1] // 2
    return jnp.concatenate([_rotate(x[..., :half], ang_row), _rotate(x[..., half:], ang_col)], axis=-1)


def gla_log_gates(a_low, w_a2, b_a):
    gates = []
    for direction in range(2):
        z = a_low[..., direction * GLA_GATE_RANK:(direction + 1) * GLA_GATE_RANK] @ w_a2[direction] + b_a[direction]
        g = jax.nn.log_sigmoid(z.astype(jnp.float32)) / GLA_TAU
        gates.append(g.reshape(g.shape[0], g.shape[1], GLA_HEADS, GLA_DK))
    return gates


def gla_chunk_states(k, v, g, s0):
    bsz, length, nh, dk = k.shape
    n = length // GLA_CHUNK
    kc = k.reshape(bsz, n, GLA_CHUNK, nh, dk)
    vc = v.reshape(bsz, n, GLA_CHUNK, nh, v.shape[-1])
    b = jnp.cumsum(g.reshape(bsz, n, GLA_CHUNK, nh, dk), axis=2)
    b_last = b[:, :, -1]
    k_end = kc * jnp.exp(b_last[:, :, None] - b)
    kv = jnp.einsum('bnchd,bnche->bnhde', k_end, vc)
    decay = jnp.exp(b_last)

    def step(s, inp):
        dec, kv_n = inp
        return dec[..., None] * s + kv_n, s

    s_final, s_before = lax.scan(step, s0, (jnp.moveaxis(decay, 1, 0), jnp.moveaxis(kv, 1, 0)))
    return jnp.moveaxis(s_before, 0, 1), s_final, b


def gla_chunk_output(q, k, v, b, s_before):
    bsz, length, nh, dk = q.shape
    n = length // GLA_CHUNK
    dv = v.shape[-1]
    qc = q.reshape(bsz, n, GLA_CHUNK, nh, dk)
    kc = k.reshape(bsz, n, GLA_CHUNK, nh, dk)
    vc = v.reshape(bsz, n, GLA_CHUNK, nh, dv)
    q_t = qc * jnp.exp(b)
    k_t = kc * jnp.exp(-b)
    att = jnp.einsum('bnihd,bnjhd->bnhij', q_t, k_t)
    mask = jnp.tril(jnp.ones((GLA_CHUNK, GLA_CHUNK), dtype=bool))
    att = jnp.where(mask, att, 0.0)
    o = jnp.einsum('bnhij,bnjhe->bnihe', att, vc) + jnp.einsum('bnihd,bnhde->bnihe', q_t, s_before)
    return o.reshape(bsz, length, nh, dv)


def gla_readout(o, r, norm_g, w_out):
    of = o * lax.rsqrt(jnp.mean(o * o, axis=-1, keepdims=True) + NORM_EPS) * norm_g.astype(jnp.float32)
    rf = jax.nn.silu(r.astype(jnp.float32)).reshape(o.shape)
    y = (of * rf).reshape(o.shape[0], o.shape[1], GLA_HEADS * GLA_DV)
    return y.astype(w_out.dtype) @ w_out


def gla_mixer(p, pc, lp, ctx_out):
    bsz, length, _ = p['gla_k'].shape

    def heads(a, dh):
        return a.astype(jnp.float32).reshape(a.shape[0], a.shape[1], GLA_HEADS, dh)

    scale = GLA_DK ** -0.5
    ang_r, ang_c = axial_rope_angles(length, GLA_DK)
    q = apply_axial_rope(heads(p['gla_q'], GLA_DK), ang_r, ang_c) * scale
    k = apply_axial_rope(heads(p['gla_k'], GLA_DK), ang_r, ang_c)
    v = heads(p['gla_v'], GLA_DV)
    kc = heads(pc['gla_k'], GLA_DK)
    vc = heads(pc['gla_v'], GLA_DV)
    qc = heads(pc['gla_q'], GLA_DK) * scale if ctx_out else None
    g = gla_log_gates(p['gla_a'], lp['gla_w_a2'], lp['gla_b_a'])
    gc = gla_log_gates(pc['gla_a'], lp['gla_w_a2'], lp['gla_b_a'])
    s0 = jnp.zeros((bsz, GLA_HEADS, GLA_DK, GLA_DV), jnp.float32)
    o = jnp.zeros_like(v)
    oc = jnp.zeros_like(vc) if ctx_out else None
    for direction in range(2):
        rev = direction == 1
        kcd, vcd, gcd = _orient(kc, rev), _orient(vc, rev), _orient(gc[direction], rev)
        sb_c, sf_c, b_c = gla_chunk_states(kcd, vcd, gcd, s0)
        if ctx_out:
            oc = oc + _orient(gla_chunk_output(_orient(qc, rev), kcd, vcd, b_c, sb_c), rev)
        kd, vd, gd = _orient(k, rev), _orient(v, rev), _orient(g[direction], rev)
        sb, _, b = gla_chunk_states(kd, vd, gd, sf_c)
        o = o + _orient(gla_chunk_output(_orient(q, rev), kd, vd, b, sb), rev)
    out = gla_readout(o, p['gla_r'], lp['gla_norm_g'], lp['gla_w_out'])
    outc = gla_readout(oc, pc['gla_r'], lp['gla_norm_g'], lp['gla_w_out']) if ctx_out else None
    return out, outc


def na_mixer(p, pc, lp, ctx_out):
    bsz, length, _ = p['na_q'].shape
    lc = pc['na_k'].shape[1]
    rows = length // GRID_W
    wr = min(NA_WIN_ROWS, rows)
    scale = NA_HEAD_DIM ** -0.5
    grid = (bsz, rows, GRID_W, NA_HEADS, NA_HEAD_DIM)
    q = p['na_q'].astype(jnp.float32).reshape(grid) * scale
    k = p['na_k'].astype(jnp.float32).reshape(grid)
    v = p['na_v'].astype(jnp.float32).reshape(grid)
    kc = pc['na_k'].astype(jnp.float32).reshape(bsz, lc, NA_HEADS, NA_HEAD_DIM)
    vc = pc['na_v'].astype(jnp.float32).reshape(bsz, lc, NA_HEADS, NA_HEAD_DIM)
    r_idx = jnp.arange(rows)
    c_idx = jnp.arange(GRID_W)
    row_start = jnp.clip(r_idx - wr // 2, 0, rows - wr)
    col_start = jnp.clip(c_idx - NA_WIN_COLS // 2, 0, GRID_W - NA_WIN_COLS)
    col_in = (c_idx[None, :] >= col_start[:, None]) & (c_idx[None, :] < col_start[:, None] + NA_WIN_COLS)
    dc_idx = jnp.clip(c_idx[None, :] - c_idx[:, None] + NA_WIN_COLS - 1, 0, 2 * NA_WIN_COLS - 2)
    rpb = lp['na_rpb'].astype(jnp.float32)
    scores = []
    key_rows = []
    for off in range(wr):
        kr = row_start + off
        dr_idx = kr - r_idx + NA_WIN_ROWS - 1
        bias = rpb[:, dr_idx][:, :, dc_idx]
        s = jnp.einsum('brqhd,brkhd->bhrqk', q, k[:, kr]) + bias[None]
        scores.append(jnp.where(col_in, s, -jnp.inf))
        key_rows.append(kr)
    s_lat = jnp.stack(scores, axis=4)
    s_ctx = jnp.einsum('brqhd,bchd->bhrqc', q, kc)
    m = jnp.maximum(jnp.max(s_lat, axis=(4, 5)), jnp.max(s_ctx, axis=-1))
    p_lat = jnp.exp(s_lat - m[..., None, None])
    p_ctx = jnp.exp(s_ctx - m[..., None])
    denom = jnp.sum(p_lat, axis=(4, 5)) + jnp.sum(p_ctx, axis=-1)
    o = jnp.einsum('bhrqc,bchd->brqhd', p_ctx, vc)
    for off, kr in enumerate(key_rows):
        o = o + jnp.einsum('bhrqk,brkhd->brqhd', p_lat[:, :, :, :, off], v[:, kr])
    o = o / jnp.transpose(denom, (0, 2, 3, 1))[..., None]
    w_out = lp['na_w_out']
    out = o.reshape(bsz, length, NA_HEADS * NA_HEAD_DIM).astype(w_out.dtype) @ w_out
    if not ctx_out:
        return out, None
    qc = pc['na_q'].astype(jnp.float32).reshape(bsz, lc, NA_HEADS, NA_HEAD_DIM) * scale
    pcw = jax.nn.softmax(jnp.einsum('bqhd,bkhd->bhqk', qc, kc), axis=-1)
    oc = jnp.einsum('bhqk,bkhd->bqhd', pcw, vc).reshape(bsz, lc, NA_HEADS * NA_HEAD_DIM)
    return out, oc.astype(w_out.dtype) @ w_out


def conformer_conv(a, dw, dw_b, ln_g, ln_b, w_out):
    val, gate = jnp.split(a, 2, axis=-1)
    y = (val * jax.nn.sigmoid(gate)).astype(dw.dtype)
    y = lax.conv_general_dilated(y, dw[:, None, :], window_strides=(1,),
                                 padding=((CONV_KERNEL // 2, CONV_KERNEL // 2),),
                                 dimension_numbers=('NWC', 'WIO', 'NWC'),
                                 feature_group_count=CONV_WIDTH) + dw_b
    y = jax.nn.silu(layer_norm(y, ln_g, ln_b))
    return y @ w_out


def merge_branches(gate_pre, gate_b, branches, w_out):
    g = jax.nn.sigmoid(gate_pre + gate_b).reshape(gate_pre.shape[:-1] + (N_BRANCHES, D_MODEL))
    merged = g[..., 0, :] * branches[0]
    for i in range(1, N_BRANCHES):
        merged = merged + g[..., i, :] * branches[i]
    return merged @ w_out


def token_mixer(h, hc, lp, ctx_out):
    p = in_proj(h, lp['w_in'], ALL_PARTS)
    pc = in_proj(hc, lp['w_in'], ALL_PARTS if ctx_out else CTX_STATE_PARTS)
    y_s5, yc_s5 = s5_mixer(p['s5_u'], pc['s5_u'], lp, ctx_out)
    y_gla, yc_gla = gla_mixer(p, pc, lp, ctx_out)
    y_na, yc_na = na_mixer(p, pc, lp, ctx_out)
    conv_args = (lp['conv_dw'], lp['conv_dw_b'], lp['conv_ln_g'], lp['conv_ln_b'], lp['conv_w_out'])
    y_cv = conformer_conv(p['conv_in'], *conv_args)
    out = merge_branches(p['gates'], lp['gate_b'], (y_s5, y_gla, y_na, y_cv), lp['w_mix_out'])
    if not ctx_out:
        return out, None
    yc_cv = conformer_conv(pc['conv_in'], *conv_args)
    outc = merge_branches(pc['gates'], lp['gate_b'], (yc_s5, yc_gla, yc_na, yc_cv), lp['w_mix_out'])
    return out, outc


def hierarchical_moe(h, w_group, b_group, w_expert, b_expert, w1, w3, w2):
    shape = h.shape
    t = h.reshape(-1, shape[-1])
    n_tok = t.shape[0]
    group_prob = jax.nn.softmax((t @ w_group + b_group).astype(jnp.float32), axis=-1)
    group_p, group_idx = lax.top_k(group_prob, 1)
    exp_logits = (t @ w_expert + b_expert).astype(jnp.float32).reshape(n_tok, MOE_GROUPS, MOE_EXPERTS_PER_GROUP)
    sel_logits = jnp.take_along_axis(exp_logits, group_idx[:, :, None], axis=1)[:, 0]
    top_val, top_idx = lax.top_k(sel_logits, MOE_TOP_K)
    top_w = jax.nn.softmax(top_val, axis=-1) * group_p
    slot_w = jnp.sum(top_w[..., None] * jax.nn.one_hot(top_idx, MOE_EXPERTS_PER_GROUP, dtype=jnp.float32), axis=1)
    combine = (jax.nn.one_hot(group_idx[:, 0], MOE_GROUPS, dtype=jnp.float32)[:, :, None]
               * slot_w[:, None, :]).astype(t.dtype)
    y = jnp.zeros_like(t)
    for gi in range(MOE_GROUPS):
        a = jnp.einsum('td,edf->tef', t, w1[gi])
        b = jnp.einsum('td,edf->tef', t, w3[gi])
        act = jax.nn.silu(a) * b * combine[:, gi, :, None]
        y = y + jnp.einsum('tef,efd->td', act, w2[gi])
    return y.reshape(shape)


def setup_inputs(seed: int = 0) -> dict:
    key = jax.random.key(seed)
    keys = iter(jax.random.split(key, 64))

    def nrm(shape, scale):
        return jax.random.normal(next(keys), shape, jnp.float32) * scale

    def gain(shape):
        return 1.0 + nrm(shape, 0.05)

    G, P, H = S5_GROUPS, S5_STATE, S5_GROUP_SIZE
    n_exp = MOE_GROUPS * MOE_EXPERTS_PER_GROUP
    lam_im_base = jnp.pi * jnp.arange(P, dtype=jnp.float32)
    return {
        'x': nrm((BATCH, SEQ, D_MODEL), 1.0),
        'c': nrm((BATCH, D_MODEL), 1.0),
        'ctx': nrm((BATCH, CTX_LEN, D_MODEL), 1.0),
        'c_ctx': nrm((D_MODEL,), 1.0),
        'norm1_g': gain((DEPTH, D_MODEL)),
        'norm2_g': gain((DEPTH, D_MODEL)),
        'w_mod': nrm((DEPTH, D_MODEL, 6 * D_MODEL), 0.5 * D_MODEL ** -0.5),
        'b_mod': nrm((DEPTH, 6 * D_MODEL), 0.02),
        'w_in': nrm((DEPTH, D_MODEL, IN_WIDTH), D_MODEL ** -0.5),
        'gate_b': nrm((DEPTH, N_BRANCHES * D_MODEL), 0.1),
        'w_mix_out': nrm((DEPTH, D_MODEL, D_MODEL), D_MODEL ** -0.5),
        's5_lam_re': -0.5 + nrm((DEPTH, 2, G, P), 0.01),
        's5_lam_im': lam_im_base + nrm((DEPTH, 2, G, P), 0.01),
        's5_log_dt': jax.random.uniform(next(keys), (DEPTH, 2, G), jnp.float32,
                                        minval=math.log(S5_DT_MIN), maxval=math.log(S5_DT_MAX)),
        's5_b_re': nrm((DEPTH, 2, G, P, H), (2 * H) ** -0.5),
        's5_b_im': nrm((DEPTH, 2, G, P, H), (2 * H) ** -0.5),
        's5_c_re': nrm((DEPTH, 2, G, H, P), 0.5),
        's5_c_im': nrm((DEPTH, 2, G, H, P), 0.5),
        's5_d': nrm((DEPTH, S5_WIDTH), 0.5),
        's5_w_glu': nrm((DEPTH, S5_WIDTH, S5_WIDTH), S5_WIDTH ** -0.5),
        's5_b_glu': nrm((DEPTH, S5_WIDTH), 0.02),
        's5_w_out': nrm((DEPTH, S5_WIDTH, D_MODEL), S5_WIDTH ** -0.5),
        'gla_w_a2': nrm((DEPTH, 2, GLA_GATE_RANK, GLA_HEADS * GLA_DK), GLA_GATE_RANK ** -0.5),
        'gla_b_a': nrm((DEPTH, 2, GLA_HEADS * GLA_DK), 0.1),
        'gla_norm_g': gain((DEPTH, GLA_DV)),
        'gla_w_out': nrm((DEPTH, GLA_HEADS * GLA_DV, D_MODEL), (GLA_HEADS * GLA_DV) ** -0.5),
        'na_rpb': nrm((DEPTH, NA_HEADS, 2 * NA_WIN_ROWS - 1, 2 * NA_WIN_COLS - 1), 0.1),
        'na_w_out': nrm((DEPTH, NA_HEADS * NA_HEAD_DIM, D_MODEL), (NA_HEADS * NA_HEAD_DIM) ** -0.5),
        'conv_dw': nrm((DEPTH, CONV_KERNEL, CONV_WIDTH), CONV_KERNEL ** -0.5),
        'conv_dw_b': nrm((DEPTH, CONV_WIDTH), 0.02),
        'conv_ln_g': gain((DEPTH, CONV_WIDTH)),
        'conv_ln_b': nrm((DEPTH, CONV_WIDTH), 0.02),
        'conv_w_out': nrm((DEPTH, CONV_WIDTH, D_MODEL), CONV_WIDTH ** -0.5),
        'moe_w_group': nrm((DEPTH, D_MODEL, MOE_GROUPS), D_MODEL ** -0.5),
        'moe_b_group': nrm((DEPTH, MOE_GROUPS), 0.01),
        'moe_w_expert': nrm((DEPTH, D_MODEL, n_exp), D_MODEL ** -0.5),
        'moe_b_expert': nrm((DEPTH, n_exp), 0.01),
        'moe_w1': nrm((DEPTH, MOE_GROUPS, MOE_EXPERTS_PER_GROUP, D_MODEL, MOE_HIDDEN), D_MODEL ** -0.5),
        'moe_w3': nrm((DEPTH, MOE_GROUPS, MOE_EXPERTS_PER_GROUP, D_MODEL, MOE_HIDDEN), D_MODEL ** -0.5),
        'moe_w2': nrm((DEPTH, MOE_GROUPS, MOE_EXPERTS_PER_GROUP, MOE_HIDDEN, D_MODEL), MOE_HIDDEN ** -0.5),
        'final_norm_g': gain((D_MODEL,)),
    }


def reference(x, c, ctx, c_ctx, norm1_g, norm2_g, w_mod, b_mod, w_in, gate_b, w_mix_out,
              s5_lam_re, s5_lam_im, s5_log_dt, s5_b_re, s5_b_im, s5_c_re, s5_c_im, s5_d,
              s5_w_glu, s5_b_glu, s5_w_out, gla_w_a2, gla_b_a, gla_norm_g, gla_w_out,
              na_rpb, na_w_out, conv_dw, conv_dw_b, conv_ln_g, conv_ln_b, conv_w_out,
              moe_w_group, moe_b_group, moe_w_expert, moe_b_expert, moe_w1, moe_w3, moe_w2,
              final_norm_g):
    xc = ctx
    c_act = jax.nn.silu(c)
    cc_act = jax.nn.silu(c_ctx)
    for i in range(DEPTH):
        last = i == DEPTH - 1
        lp = {
            'w_in': w_in[i], 'gate_b': gate_b[i], 'w_mix_out': w_mix_out[i],
            's5_lam_re': s5_lam_re[i], 's5_lam_im': s5_lam_im[i], 's5_log_dt': s5_log_dt[i],
            's5_b_re': s5_b_re[i], 's5_b_im': s5_b_im[i], 's5_c_re': s5_c_re[i], 's5_c_im': s5_c_im[i],
            's5_d': s5_d[i], 's5_w_glu': s5_w_glu[i], 's5_b_glu': s5_b_glu[i], 's5_w_out': s5_w_out[i],
            'gla_w_a2': gla_w_a2[i], 'gla_b_a': gla_b_a[i], 'gla_norm_g': gla_norm_g[i],
            'gla_w_out': gla_w_out[i], 'na_rpb': na_rpb[i], 'na_w_out': na_w_out[i],
            'conv_dw': conv_dw[i], 'conv_dw_b': conv_dw_b[i], 'conv_ln_g': conv_ln_g[i],
            'conv_ln_b': conv_ln_b[i], 'conv_w_out': conv_w_out[i],
        }
        moe_args = (moe_w_group[i], moe_b_group[i], moe_w_expert[i], moe_b_expert[i],
                    moe_w1[i], moe_w3[i], moe_w2[i])
        mod = (c_act @ w_mod[i] + b_mod[i])[:, None, :]
        sh1, sc1, g1, sh2, sc2, g2 = jnp.split(mod, 6, axis=-1)
        n_ctx_mod = 2 if last else 6
        modc = cc_act @ w_mod[i][:, :n_ctx_mod * D_MODEL] + b_mod[i][:n_ctx_mod * D_MODEL]
        modc_parts = jnp.split(modc, n_ctx_mod, axis=-1)
        h = modulate(x, norm1_g[i], sh1, sc1)
        hc = modulate(xc, norm1_g[i], modc_parts[0], modc_parts[1])
        mix, mixc = token_mixer(h, hc, lp, not last)
        x = x + g1 * mix
        x = x + g2 * hierarchical_moe(modulate(x, norm2_g[i], sh2, sc2), *moe_args)
        if not last:
            xc = xc + modc_parts[2] * mixc
            xc = xc + modc_parts[5] * hierarchical_moe(modulate(xc, norm2_g[i], modc_parts[3], modc_parts[4]), *moe_args)
    return rms_norm(x, final_norm_g)
```

```python
import functools
import numpy as np
import ml_dtypes
import concourse.bass as bass
import concourse.mybir as mybir
from concourse.bass_utils import run_bass_kernel_spmd
from contextlib import ExitStack

F32 = mybir.dt.float32
BF16 = mybir.dt.bfloat16
AF = mybir.ActivationFunctionType
ALU = mybir.AluOpType
AX = mybir.AxisListType
NPBF = ml_dtypes.bfloat16
PI = float(np.pi)

NCORE = 8
D = 1024
B = 4
L = 8192
LC = 256
NLOC = 4096 + 128
EXT = LC + L
EPS = 1e-6


class View:
    __slots__ = ("b", "ap")

    def __init__(self, b, ap):
        self.b = b
        self.ap = ap

    def __getitem__(self, k):
        return View(self.b, self.ap[k])

    def bc(self, shape):
        return View(self.b, self.ap.to_broadcast(list(shape)))

    def re(self, s, **kw):
        return View(self.b, self.ap.rearrange(s, **kw))


class Buf:
    __slots__ = ("t", "name", "lw", "rd", "dsem", "dcnt", "dram")

    def __init__(self, t, name, dram=False):
        self.t = t
        self.name = name
        self.lw = None
        self.rd = []
        self.dsem = None
        self.dcnt = 0
        self.dram = dram

    def __getitem__(self, k):
        return View(self, self.t[k])


def _v(x):
    return x.ap if isinstance(x, View) else x


class K:
    def __init__(self):
        self.nc = bass.Bass("TRN2", target_bir_lowering=False)
        self.es = ExitStack()
        nc = self.nc
        self.eng = {"pe": nc.tensor, "act": nc.scalar, "dve": nc.vector, "pool": nc.gpsimd, "sp": nc.sync}
        self.sem = {}
        self.cnt = {}
        self.semobj = {}
        for e in self.eng:
            s = self.es.enter_context(nc.semaphore("s_" + e))
            self.sem[e] = s
            self.cnt[e] = 0
            self.semobj[("E", e)] = s
        self.waited = {e: {} for e in self.eng}
        self.nb = 0
        self.ninst = 0
        self.dbufs = []
        self.stk = [self.es]

    def sb(self, shape, dt=F32, name=None):
        self.nb += 1
        name = name or f"sb{self.nb}"
        return Buf(self.stk[-1].enter_context(self.nc.sbuf_tensor(name, list(shape), dt)), name)

    def barrier(self):
        deps = [(("E", e), self.cnt[e]) for e in self.eng] + [(b.dsem, b.dcnt) for b in self.dbufs]
        for e in self.eng:
            self._wait(e, deps)

    def push(self):
        self.stk.append(ExitStack())

    def pop(self):
        self.barrier()
        self.stk.pop().close()

    def ps(self, shape, dt=F32, name=None):
        self.nb += 1
        name = name or f"ps{self.nb}"
        return Buf(self.es.enter_context(self.nc.psum_tensor(name, list(shape), dt)), name)

    def din(self, name, shape, dt=F32):
        return Buf(self.nc.dram_tensor(name, list(shape), dt, kind="ExternalInput").ap(), name, dram=True)

    def dout(self, name, shape, dt=F32):
        return Buf(self.nc.dram_tensor(name, list(shape), dt, kind="ExternalOutput").ap(), name, dram=True)

    def _wait(self, e, deps):
        eng = self.eng[e]
        w = self.waited[e]
        best = {}
        for d in deps:
            if d is None:
                continue
            kk, v = d
            if kk == ("E", e) and v > self.cnt[e]:
                v = self.cnt[e]
            if best.get(kk, 0) < v:
                best[kk] = v
        for kk, v in best.items():
            if w.get(kk, 0) < v:
                eng.wait_ge(self.semobj[kk], v)
                w[kk] = v

    def op(self, e, fn, reads=(), writes=(), inc=True):
        reads = [r.b if isinstance(r, View) else r for r in reads if r is not None and not isinstance(r, (int, float))]
        writes = [r.b if isinstance(r, View) else r for r in writes]
        deps = []
        for b in reads:
            deps.append(b.lw)
        for b in writes:
            deps.append(b.lw)
            deps.extend(b.rd)
        self._wait(e, deps)
        inst = fn(self.eng[e])
        self.ninst += 1
        if inc:
            self.cnt[e] += 1
            inst.then_inc(self.sem[e], 1)
            tk = (("E", e), self.cnt[e])
        else:
            tk = (("E", e), self.cnt[e] + 1)
        for b in writes:
            b.lw = tk
            b.rd = []
        for b in reads:
            if b not in writes and not b.dram:
                b.rd.append(tk)
        return inst

    def dma(self, out, in_, q="sp", chain=False, **kw):
        ob, ib = out.b, in_.b
        own = ib if ob.dram else ob
        if own.dsem is None:
            s = self.es.enter_context(self.nc.semaphore("d_" + own.name))
            own.dsem = ("D", own.name)
            self.semobj[own.dsem] = s
            self.dbufs.append(own)
        key = own.dsem
        deps = [ib.lw]
        ch = chain and ob.lw is not None and ob.lw[0] == key
        if not ch:
            deps.append(ob.lw)
        deps.extend(ob.rd)
        self._wait(q, deps)
        inst = self.eng[q].dma_start(out=out.ap, in_=in_.ap, **kw)
        self.ninst += 1
        own.dcnt += 16
        inst.then_inc(self.semobj[key], 16)
        tk = (key, own.dcnt)
        if not ob.dram:
            ob.lw = tk
            if not ch:
                ob.rd = []
        if not ib.dram:
            ib.rd.append(tk)
        return inst

    def finish(self, q="sp"):
        self._wait(q, [(b.dsem, b.dcnt) for b in self.dbufs])
        self.es.close()
        return self.nc

    def mm(self, out, lhsT, rhs, start=True, stop=True, inc=True):
        return self.op("pe", lambda e: e.matmul(out.ap, lhsT=lhsT.ap, rhs=rhs.ap, start=start, stop=stop),
                       reads=[lhsT, rhs] + ([] if start else [out]), writes=[out], inc=inc)

    def tr(self, out, in_, ident, inc=True):
        return self.op("pe", lambda e: e.transpose(out.ap, in_.ap, ident.ap), reads=[in_, ident], writes=[out], inc=inc)

    def act(self, out, in_, func, bias=None, scale=None, accum=None, eng="act"):
        kw = {}
        if bias is not None:
            kw["bias"] = _v(bias)
        if scale is not None:
            kw["scale"] = _v(scale)
        if accum is not None:
            kw["accum_out"] = accum.ap
        return self.op(eng, lambda e: e.activation(out=out.ap, in_=in_.ap, func=func, **kw),
                       reads=[in_, bias, scale], writes=[out] + ([accum] if accum is not None else []))

    def tt(self, out, in0, in1, op, eng="dve"):
        return self.op(eng, lambda e: e.tensor_tensor(out=out.ap, in0=in0.ap, in1=in1.ap, op=op), reads=[in0, in1], writes=[out])

    def ts(self, out, in0, s1, op0, s2=None, op1=None, eng="dve"):
        if op1 is None:
            return self.op(eng, lambda e: e.tensor_scalar(out=out.ap, in0=in0.ap, scalar1=_v(s1), scalar2=None, op0=op0),
                           reads=[in0, s1], writes=[out])
        return self.op(eng, lambda e: e.tensor_scalar(out=out.ap, in0=in0.ap, scalar1=_v(s1), scalar2=_v(s2), op0=op0, op1=op1),
                       reads=[in0, s1, s2], writes=[out])

    def stt(self, out, in0, scalar, in1, op0, op1, eng="dve"):
        return self.op(eng, lambda e: e.scalar_tensor_tensor(out=out.ap, in0=in0.ap, scalar=_v(scalar), in1=in1.ap, op0=op0, op1=op1),
                       reads=[in0, scalar, in1], writes=[out])

    def scan(self, out, d0, d1, initial, op0=ALU.mult, op1=ALU.add):
        return self.op("dve", lambda e: e.tensor_tensor_scan(out=out.ap, data0=d0.ap, data1=d1.ap, initial=_v(initial), op0=op0, op1=op1),
                       reads=[d0, d1, initial], writes=[out])

    def copy(self, out, in_, eng="dve"):
        if eng == "act":
            return self.act(out, in_, AF.Copy)
        return self.op(eng, lambda e: e.tensor_copy(out=out.ap, in_=in_.ap), reads=[in_], writes=[out])

    def memset(self, out, val, eng="dve"):
        return self.op(eng, lambda e: e.memset(out.ap, val), writes=[out])

    def recip(self, out, in_):
        return self.op("dve", lambda e: e.reciprocal(out=out.ap, in_=in_.ap), reads=[in_], writes=[out])

    def reduce(self, out, in_, op, axis=AX.X):
        return self.op("dve", lambda e: e.tensor_reduce(out=out.ap, in_=in_.ap, axis=axis, op=op), reads=[in_], writes=[out])

    def wrap(self, out, in_, shift):
        return self.op("dve", lambda e: e.add_range_wrap(out=out.ap, in_=in_.ap, shift=shift, bound=PI, period=2 * PI),
                       reads=[in_], writes=[out])


def run(nc, in_maps):
    return run_bass_kernel_spmd(nc, in_maps, core_ids=list(range(len(in_maps)))).results


@functools.lru_cache(None)
def build_M():
    k = K()
    cT = k.din("cT", [128, 8, 8]); w = k.din("w", [1024, 1536]); bm = k.din("bm", [128, 12])
    out = k.dout("mod", [128, 12, 8])
    ct = k.sb([128, 8, 8]); ca = k.sb([128, 8, 8]); wt = k.sb([128, 8, 1536]); bt = k.sb([128, 12]); ot = k.sb([128, 12, 8])
    ps = [k.ps([128, 8]) for _ in range(2)]
    k.dma(ct[:], cT[:, :, :]); k.dma(bt[:], bm[:, :])
    wv = w[:, :].re("(k p) c -> p k c", p=128)
    for kk in range(8):
        k.dma(wt[:, kk, :], wv[:, kk, :], chain=True)
    k.act(ca[:], ct[:], AF.Silu)
    for m in range(12):
        p = ps[m % 2]
        for kk in range(8):
            k.mm(p[:], wt[:, kk, m * 128:(m + 1) * 128], ca[:, kk, :], start=(kk == 0), stop=(kk == 7), inc=(kk == 7))
        k.act(ot[:, m, :], p[:], AF.Identity, bias=bt[:, m:m + 1])
    k.dma(out[:, :, :], ot[:])
    return k.finish()


def run_M(inp):
    c, c_ctx, w_mod, b_mod = inp["c"], inp["c_ctx"], inp["w_mod"], inp["b_mod"]
    cc = np.zeros((8, D), np.float32); cc[:4] = c; cc[4] = c_ctx
    cT = np.ascontiguousarray(cc.T.reshape(8, 128, 8).transpose(1, 0, 2))
    maps = []
    for core in range(8):
        l, q = core // 4, core % 4
        maps.append({"cT": cT, "w": np.ascontiguousarray(w_mod[l][:, q * 1536:(q + 1) * 1536]),
                     "bm": np.ascontiguousarray(b_mod[l][q * 1536:(q + 1) * 1536].reshape(12, 128).T)})
    res = run(build_M(), maps)
    mod = np.zeros((2, 6 * D, 8), np.float32)
    for core in range(8):
        l, q = core // 4, core % 4
        mod[l, q * 1536:(q + 1) * 1536] = res[core]["mod"].transpose(1, 0, 2).reshape(1536, 8)
    return mod.reshape(2, 6, D, 8)


def pk(v):
    return np.ascontiguousarray(np.asarray(v, np.float32).reshape(8, 128).T)


TOKBLK = [(i * 512, 512) for i in range(8)] + [(4096, 128)]
OFF = dict(s5_u=0, gla_q=256, gla_k=512, gla_v=768, gla_r=1280, gla_a=1792, na_q=1824, na_k=2080, na_v=2336, conv_in=2592, gates=3104)


def norm_mod_T(k, x_dram, ntile, A_lat, B_lat, A_ctx, B_ctx, hT, ident_b, ps_t, hT32=None, ident_f=None, after_tile=None):
    xt = [k.sb([128, 1024]) for _ in range(2)]
    xn = [k.sb([128, 1024], BF16 if hT32 is None else F32) for _ in range(2)]
    junk = k.sb([128, 1024], BF16)
    tmpf = k.sb([128, 8, 128])
    ss = [k.sb([128, 1]) for _ in range(2)]
    rs = [k.sb([128, 1]) for _ in range(2)]
    for i in range(ntile):
        x_, n_, s_, r_ = xt[i % 2], xn[i % 2], ss[i % 2], rs[i % 2]
        k.dma(x_[:], x_dram[i * 128:(i + 1) * 128, :])
        k.act(junk[:], x_[:], AF.Square, accum=s_[:])
        k.ts(r_[:], s_[:], 1.0 / D, ALU.mult, EPS, ALU.add)
        k.act(r_[:], r_[:], AF.Sqrt)
        k.recip(r_[:], r_[:])
        k.ts(n_[:], x_[:], r_[:, 0:1], ALU.mult)
        tp = ps_t[i % 2]
        for kk in range(8):
            k.tr(tp[:, kk, :], n_[:, kk * 128:(kk + 1) * 128], (ident_b if hT32 is None else ident_f)[:], inc=(kk == 7))
        A_, B_ = (A_ctx, B_ctx) if i == ntile - 1 else (A_lat, B_lat)
        k.tt(tmpf[:], tp[:], View(A_, A_.t[:, :].unsqueeze(2).to_broadcast([128, 8, 128])), ALU.mult)
        Bb = View(B_, B_.t[:, :].unsqueeze(2).to_broadcast([128, 8, 128]))
        if hT32 is None:
            k.tt(hT[:, :, i * 128:(i + 1) * 128], tmpf[:], Bb, ALU.add, eng="pool")
        else:
            k.tt(hT32[:], tmpf[:], Bb, ALU.add)
            k.copy(hT[:, :, i * 128:(i + 1) * 128], hT32[:], eng="pool")
            after_tile(i)


def mod_prep(k, modv, gn):
    mt = k.sb([128, 8, 4]); gt = k.sb([128, 8])
    k.dma(mt[:], modv[:, :, :]); k.dma(gt[:], gn[:, :])
    res = []
    for j in (0, 2):
        A = k.sb([128, 8]); Bv = k.sb([128, 8])
        k.ts(A[:], mt[:, :, j + 1], 1.0, ALU.add)
        k.tt(A[:], A[:], gt[:], ALU.mult)
        k.copy(Bv[:], mt[:, :, j])
        res += [A, Bv]
    return res


@functools.lru_cache(None)
def build_A():
    k = K()
    xa = k.din("xa", [NLOC, D]); modv = k.din("modv", [128, 8, 4]); g1n = k.din("g1n", [128, 8])
    w_in = k.din("w_in", [D, 7200]); gate_b = k.din("gate_b", [128, 32])
    ropeC = k.din("ropeC", [128, NLOC]); ropeS = k.din("ropeS", [128, NLOC]); ident = k.din("ident", [128, 128])
    o_uT = k.dout("uT", [256, NLOC]); o_gq = k.dout("gqT", [256, NLOC]); o_gk = k.dout("gkT", [256, NLOC])
    o_ga = k.dout("gaT", [32, NLOC]); o_gr = k.dout("grsT", [512, NLOC], BF16); o_gv = k.dout("gv", [NLOC, 512], BF16)
    o_nq = k.dout("nqT", [256, NLOC], BF16); o_nk = k.dout("nkT", [256, NLOC], BF16); o_nv = k.dout("nv", [NLOC, 256], BF16)
    o_cy = k.dout("cyT", [256, NLOC], BF16); o_gs = k.dout("gsT", [4096, NLOC], BF16)

    hT = k.sb([128, 8, NLOC], BF16, "hT")
    idf = k.sb([128, 128]); idb = k.sb([128, 128], BF16)
    k.dma(idf[:], ident[:, :]); k.copy(idb[:], idf[:])
    A_lat, B_lat, A_ctx, B_ctx = mod_prep(k, modv, g1n)
    gb = k.sb([128, 32]); k.dma(gb[:], gate_b[:, :])
    rc = k.sb([128, NLOC]); rsn = k.sb([128, NLOC])
    k.dma(rc[:], ropeC[:, :]); k.dma(rsn[:], ropeS[:, :])
    ps_t = [k.ps([128, 8, 128], BF16) for _ in range(2)]
    norm_mod_T(k, xa, 33, A_lat, B_lat, A_ctx, B_ctx, hT, idb, ps_t)

    pss = [k.ps([128, 512]) for _ in range(6)]
    psi = [0]

    def nps():
        psi[0] += 1
        return pss[psi[0] % 6]

    wf = [k.sb([128, 8, 512]) for _ in range(2)]
    wq = k.sb([128, 8, 256])
    wbq = k.sb([128, 8, 256], BF16)
    wb = [k.sb([128, 8, 512], BF16) for _ in range(3)]
    wi = [0]
    stf = [k.sb([128, 512]) for _ in range(3)]
    stb = [k.sb([128, 512], BF16) for _ in range(3)]
    t1 = k.sb([128, 512]); t2 = k.sb([128, 512])
    si = [0]

    def load_w(col0, ncols, scale=None, swap=False):
        wi[0] += 1
        f = wq if swap else wf[wi[0] % 2]
        b = wbq if swap else wb[wi[0] % 3]
        for kk in range(8):
            k.dma(f[:, kk, :ncols], w_in[kk * 128:(kk + 1) * 128, col0:col0 + ncols], chain=True)
        if swap:
            sc = 1.0 if scale is None else scale
            fv = f[:, :, :].re("p k (g t c) -> p (k g) t c", t=2, c=16)
            bv = b[:, :, :].re("p k (g t c) -> p (k g) t c", t=2, c=16)
            k.act(bv[:, :, 0, :], fv[:, :, 1, :], AF.Copy, scale=-sc)
            k.act(bv[:, :, 1, :], fv[:, :, 0, :], AF.Copy, scale=sc)
        elif scale is not None:
            k.act(b[:, :, :ncols], f[:, :, :ncols], AF.Copy, scale=scale)
        else:
            k.copy(b[:, :, :ncols], f[:, :, :ncols], eng=("dve" if wi[0] % 2 else "pool"))
        return b

    def fm_mm(b, c0, M, tok0, n):
        p = nps()
        for kk in range(8):
            k.mm(p[:M, :n], b[:, kk, c0:c0 + M], hT[:, kk, tok0:tok0 + n], start=(kk == 0), stop=(kk == 7), inc=(kk == 7))
        return p

    def fm_simple(col0, ncols, out, post, bf, scale=None):
        for g0 in range(0, ncols, 512):
            gn = min(512, ncols - g0)
            b = load_w(col0 + g0, gn, scale=scale)
            for c0 in range(0, gn, 128):
                M = min(128, gn - c0)
                for (tok0, n) in TOKBLK:
                    p = fm_mm(b, c0, M, tok0, n)
                    si[0] += 1
                    st = (stb if bf else stf)[si[0] % 3]
                    post(st[:M, :n], p[:M, :n], (g0 + c0) // 128)
                    k.dma(out[g0 + c0:g0 + c0 + M, tok0:tok0 + n], st[:M, :n])

    def tm_part(col0, ncols, out):
        b = load_w(col0, ncols)
        for i in range(33):
            p = nps()
            for kk in range(8):
                k.mm(p[:, :ncols], hT[:, kk, i * 128:(i + 1) * 128], b[:, kk, :ncols], start=(kk == 0), stop=(kk == 7), inc=(kk == 7))
            si[0] += 1
            st = stb[si[0] % 3]
            k.copy(st[:, :ncols], p[:, :ncols], eng=("act" if i % 2 else "dve"))
            k.dma(out[i * 128:(i + 1) * 128, :], st[:, :ncols])

    def rope_part(col0, out, scale):
        b = load_w(col0, 256, scale=scale)
        bs = load_w(col0, 256, scale=scale, swap=True)
        for c0 in (0, 128):
            for (tok0, n) in TOKBLK:
                p = fm_mm(b, c0, 128, tok0, n)
                p2 = fm_mm(bs, c0, 128, tok0, n)
                si[0] += 1
                st = stf[si[0] % 3]
                k.tt(t1[:, :n], p[:, :n], rc[:, tok0:tok0 + n], ALU.mult)
                k.tt(t2[:, :n], p2[:, :n], rsn[:, tok0:tok0 + n], ALU.mult)
                k.tt(st[:, :n], t1[:, :n], t2[:, :n], ALU.add, eng="pool")
                k.dma(out[c0:c0 + 128, tok0:tok0 + n], st[:, :n])

    cp = [0]

    def post_copy(st, p, t):
        cp[0] += 1
        k.copy(st, p, eng=("act" if cp[0] % 2 else "dve"))

    fm_simple(OFF["s5_u"], 256, o_uT, post_copy, False)
    rope_part(OFF["gla_q"], o_gq, 0.125)
    rope_part(OFF["gla_k"], o_gk, None)
    tm_part(OFF["gla_v"], 512, o_gv)
    fm_simple(OFF["gla_r"], 512, o_gr, lambda st, p, t: k.act(st, p, AF.Silu), True)
    fm_simple(OFF["gla_a"], 32, o_ga, post_copy, False)
    fm_simple(OFF["na_q"], 256, o_nq, post_copy, True, scale=0.125)
    fm_simple(OFF["na_k"], 256, o_nk, post_copy, True)
    tm_part(OFF["na_v"], 256, o_nv)
    b = load_w(OFF["conv_in"], 512)
    for ct in range(2):
        for (tok0, n) in TOKBLK:
            pv = fm_mm(b, ct * 128, 128, tok0, n)
            pg = fm_mm(b, 256 + ct * 128, 128, tok0, n)
            si[0] += 1
            st = stb[si[0] % 3]
            k.act(t1[:, :n], pg[:, :n], AF.Sigmoid)
            k.tt(st[:, :n], pv[:, :n], t1[:, :n], ALU.mult)
            k.dma(o_cy[ct * 128:(ct + 1) * 128, tok0:tok0 + n], st[:, :n])
    for g0 in range(0, 4096, 512):
        b = load_w(OFF["gates"] + g0, 512)
        for c0 in range(0, 512, 128):
            t = (g0 + c0) // 128
            for (tok0, n) in TOKBLK:
                p = fm_mm(b, c0, 128, tok0, n)
                si[0] += 1
                st = stb[si[0] % 3]
                k.act(st[:, :n], p[:, :n], AF.Sigmoid, bias=gb[:, t:t + 1])
                k.dma(o_gs[t * 128:(t + 1) * 128, tok0:tok0 + n], st[:, :n])
    return k.finish()


def rope_tables():
    inv = (10000.0 ** (-np.arange(0, 32, 2, dtype=np.float32) / 32)).astype(np.float32)
    tabs = []
    for half in range(2):
        t = np.arange(4096) + half * 4096
        row = (t // 64).astype(np.float32); col = (t % 64).astype(np.float32)
        ang = np.zeros((64, 4096), np.float32)
        ang[0:16] = inv[:, None] * row[None]; ang[16:32] = ang[0:16]
        ang[32:48] = inv[:, None] * col[None]; ang[48:64] = ang[32:48]
        c = np.ones((128, NLOC), np.float32); s = np.zeros((128, NLOC), np.float32)
        c[:, :4096] = np.tile(np.cos(ang), (2, 1)); s[:, :4096] = np.tile(np.sin(ang), (2, 1))
        tabs.append((c, s))
    return tabs


def shard_tokens(x, xc):
    out = []
    for core in range(8):
        b, h = core // 2, core % 2
        out.append(np.ascontiguousarray(np.concatenate([x[b, h * 4096:(h + 1) * 4096], xc[b, h * 128:(h + 1) * 128]], 0)))
    return out


def gather_fm(res, name):
    C = res[0][name].shape[0]
    out = np.empty((B, C, EXT), res[0][name].dtype)
    for core in range(8):
        b, h = core // 2, core % 2
        a = res[core][name]
        out[b, :, LC + h * 4096:LC + (h + 1) * 4096] = a[:, :4096]
        out[b, :, h * 128:(h + 1) * 128] = a[:, 4096:]
    return out


def gather_tm(res, name):
    C = res[0][name].shape[1]
    out = np.empty((B, EXT, C), res[0][name].dtype)
    for core in range(8):
        b, h = core // 2, core % 2
        a = res[core][name]
        out[b, LC + h * 4096:LC + (h + 1) * 4096] = a[:4096]
        out[b, h * 128:(h + 1) * 128] = a[4096:]
    return out


def run_A(x, xc, mod_l, inp, l):
    tabs = rope_tables()
    xs = shard_tokens(x, xc)
    ident = np.eye(128, dtype=np.float32)
    maps = []
    for core in range(8):
        b, h = core // 2, core % 2
        modv = np.stack([pk(mod_l[0, :, b]), pk(mod_l[1, :, b]), pk(mod_l[0, :, 4]), pk(mod_l[1, :, 4])], axis=2)
        maps.append({"xa": xs[core], "modv": np.ascontiguousarray(modv), "g1n": pk(inp["norm1_g"][l]), "w_in": inp["w_in"][l],
                     "gate_b": np.ascontiguousarray(inp["gate_b"][l].reshape(32, 128).T),
                     "ropeC": tabs[h][0], "ropeS": tabs[h][1], "ident": ident})
    return run(build_A(), maps)


def sincos(k, th, shape):
    I32 = mybir.dt.int32
    res = []
    for shift in (PI / 2, 0.0):
        a = k.sb(shape); ni = k.sb(shape, I32); nf = k.sb(shape)
        k.ts(a[:], th, shift, ALU.add)
        k.ts(nf[:], a[:], 1.0 / (2 * PI), ALU.mult)
        k.copy(ni[:], nf[:])
        k.copy(nf[:], ni[:])
        k.stt(a[:], nf[:], -2 * PI, a[:], ALU.mult, ALU.add)
        k.ts(nf[:], a[:], PI, ALU.is_gt, -2 * PI, ALU.mult)
        k.tt(a[:], a[:], nf[:], ALU.add)
        k.ts(nf[:], a[:], -PI, ALU.is_lt, 2 * PI, ALU.mult)
        k.tt(a[:], a[:], nf[:], ALU.add)
        k.ts(a[:], a[:], -PI, ALU.max, PI, ALU.min)
        k.act(a[:], a[:], AF.Sin)
        res.append(a)
    return res[0], res[1]


S5BL = 256
S5NB = EXT // S5BL


@functools.lru_cache(None)
def build_S5():
    k = K()
    BL, NB = S5BL, S5NB
    uT = k.din("uT", [128, EXT]); lamS = k.din("lamS", [128, 16, 3]); lamR = k.din("lamR", [128, 16, 64, 2]); ldtR = k.din("ldtR", [128, 16])
    Bre = k.din("Bre", [128, 16, 64]); Bim = k.din("Bim", [128, 16, 64]); CW0 = k.din("CW0", [128, 16, 128]); CWs0 = k.din("CWs0", [128, 16, 128])
    dsk = k.din("dsk", [128, 1]); swapP = k.din("swapP", [128, 128]); sgn = k.din("sgn", [128, 1])
    yT = k.dout("yT", [128, EXT])

    Tc = k.sb([128, 16, BL], name="Tc"); Ts = k.sb([128, 16, BL], name="Ts"); magS = k.sb([128, 16])
    WB = k.sb([128, 16, 128], BF16); WBs = k.sb([128, 16, 128], BF16)
    CW = k.sb([128, 16, 128], BF16); CWs = k.sb([128, 16, 128], BF16)
    sw = k.sb([128, 128]); dk = k.sb([128, 1])
    k.push()
    ls = k.sb([128, 16, 3]); k.dma(ls[:], lamS[:, :, :])
    sg = k.sb([128, 1]); k.dma(sg[:], sgn[:, :])
    k.dma(dk[:], dsk[:, :])
    k.dma(sw[:], swapP[:, :])
    dtS = k.sb([128, 16]); thS = k.sb([128, 16])
    k.act(dtS[:], ls[:, :, 2], AF.Exp)
    k.tt(thS[:], ls[:, :, 1], dtS[:], ALU.mult)
    k.tt(magS[:], ls[:, :, 0], dtS[:], ALU.mult)
    k.act(magS[:], magS[:], AF.Exp)
    cosS, sinS = sincos(k, thS[:], [128, 16])
    t_a = k.sb([128, 16, BL // 2]); t_b = k.sb([128, 16, BL // 2])
    zc = k.sb([128, 16]); zs = k.sb([128, 16]); z1 = k.sb([128, 16]); z2 = k.sb([128, 16])
    k.copy(Tc[:, :, 0], cosS[:]); k.copy(Ts[:, :, 0], sinS[:])
    k.copy(zc[:], cosS[:]); k.copy(zs[:], sinS[:])
    m = 1
    while m < BL:
        zcb = View(zc, zc.t[:, :].unsqueeze(2).to_broadcast([128, 16, m]))
        zsb = View(zs, zs.t[:, :].unsqueeze(2).to_broadcast([128, 16, m]))
        k.tt(t_a[:, :, :m], Tc[:, :, 0:m], zcb, ALU.mult)
        k.tt(t_b[:, :, :m], Ts[:, :, 0:m], zsb, ALU.mult)
        k.tt(t_a[:, :, :m], t_a[:, :, :m], t_b[:, :, :m], ALU.subtract)
        k.tt(t_b[:, :, :m], Ts[:, :, 0:m], zcb, ALU.mult)
        k.copy(Tc[:, :, m:2 * m], t_a[:, :, :m])
        k.tt(t_a[:, :, :m], Tc[:, :, 0:m], zsb, ALU.mult)
        k.tt(Ts[:, :, m:2 * m], t_a[:, :, :m], t_b[:, :, :m], ALU.add)
        k.tt(z1[:], zc[:], zc[:], ALU.mult); k.tt(z2[:], zs[:], zs[:], ALU.mult)
        k.tt(zs[:], zc[:], zs[:], ALU.mult); k.ts(zs[:], zs[:], 2.0, ALU.mult)
        k.tt(zc[:], z1[:], z2[:], ALU.subtract)
        m *= 2
    Tsg = Ts
    k.ts(Tsg[:], Ts[:], sg[:, 0:1], ALU.mult)

    k.pop()
    k.push()
    lr_ = k.sb([128, 16, 64, 2]); k.dma(lr_[:], lamR[:, :, :, :])
    dR = k.sb([128, 16]); k.dma(dR[:], ldtR[:, :]); k.act(dR[:], dR[:], AF.Exp)
    dRb = View(dR, dR.t[:, :].unsqueeze(2).to_broadcast([128, 16, 64]))
    lrR = lr_[:, :, :, 0]; liR = lr_[:, :, :, 1]
    thR = k.sb([128, 16, 64]); mgR = k.sb([128, 16, 64])
    k.tt(thR[:], liR, dRb, ALU.mult)
    k.tt(mgR[:], lrR, dRb, ALU.mult)
    k.act(mgR[:], mgR[:], AF.Exp)
    cosR, sinR = sincos(k, thR[:], [128, 16, 64])
    are = cosR; aim = sinR
    k.tt(are[:], mgR[:], cosR[:], ALU.mult); k.tt(aim[:], mgR[:], sinR[:], ALU.mult)
    k.ts(are[:], are[:], -1.0, ALU.add)
    den = thR; tmp = mgR
    k.tt(den[:], lrR, lrR, ALU.mult); k.tt(tmp[:], liR, liR, ALU.mult); k.tt(den[:], den[:], tmp[:], ALU.add)
    k.recip(den[:], den[:])
    cre = k.sb([128, 16, 64]); cim = k.sb([128, 16, 64])
    k.tt(cre[:], are[:], lrR, ALU.mult); k.tt(tmp[:], aim[:], liR, ALU.mult); k.tt(cre[:], cre[:], tmp[:], ALU.add); k.tt(cre[:], cre[:], den[:], ALU.mult)
    k.tt(cim[:], aim[:], lrR, ALU.mult); k.tt(tmp[:], are[:], liR, ALU.mult); k.tt(cim[:], cim[:], tmp[:], ALU.subtract); k.tt(cim[:], cim[:], den[:], ALU.mult)
    br = k.sb([128, 16, 64]); bi = k.sb([128, 16, 64])
    k.dma(br[:], Bre[:, :, :]); k.dma(bi[:], Bim[:, :, :])
    WBf = k.sb([128, 16, 128])
    k.tt(WBf[:, :, 0:64], cre[:], br[:], ALU.mult); k.tt(tmp[:], cim[:], bi[:], ALU.mult); k.tt(WBf[:, :, 0:64], WBf[:, :, 0:64], tmp[:], ALU.subtract)
    k.tt(WBf[:, :, 64:128], cre[:], bi[:], ALU.mult); k.tt(tmp[:], cim[:], br[:], ALU.mult); k.tt(WBf[:, :, 64:128], WBf[:, :, 64:128], tmp[:], ALU.add)
    k.copy(WB[:], WBf[:])
    k.copy(WBs[:, :, 0:64], WBf[:, :, 64:128]); k.copy(WBs[:, :, 64:128], WBf[:, :, 0:64])
    cwf = k.sb([128, 16, 128]); cwsf = k.sb([128, 16, 128])
    k.dma(cwf[:], CW0[:, :, :]); k.dma(cwsf[:], CWs0[:, :, :])
    k.copy(CW[0:64], cwf[0:64]); k.act(CW[64:128], cwf[64:128], AF.Copy, scale=-1.0)
    k.act(CWs[0:64], cwsf[0:64], AF.Copy, scale=-1.0); k.copy(CWs[64:128], cwsf[64:128])

    k.pop()
    uf = k.sb([128, EXT], name="uf"); ub = k.sb([128, EXT], BF16, name="ub"); yacc = k.sb([128, EXT], name="yacc")
    for c in range(0, EXT, 2112):
        k.dma(uf[:, c:c + 2112], uT[:, c:c + 2112], chain=True)
    k.copy(ub[:], uf[:], eng="pool")
    k.ts(yacc[:], uf[:], dk[:, 0:1], ALU.mult)

    Gt = self_t = k.es.enter_context(k.nc.sbuf_tensor("Gbig", [128, 16, BL], F32))
    G = [Buf(Gt[:, g, :], f"G{g}") for g in range(16)]
    M1 = [k.sb([128, 8, BL], BF16) for _ in range(2)]; M2 = [k.sb([128, 8, BL], BF16) for _ in range(2)]
    Wt = [k.sb([128, BL]) for _ in range(2)]; T1 = [k.sb([128, BL]) for _ in range(2)]; T2 = [k.sb([128, BL]) for _ in range(2)]
    carry = k.sb([128, 16]); gl = k.sb([128, 16]); c1 = k.sb([128, 16]); c2 = k.sb([128, 16])
    k.memset(carry[:], 0.0)
    psS = [k.ps([128, BL]) for _ in range(2)]; psW = [k.ps([128, BL]) for _ in range(2)]
    psY = [k.ps([128, BL]) for _ in range(2)]; psG = k.ps([128, 16])
    it = 0
    for s in range(NB):
        for d in range(2):
            blk = s if d == 0 else (0 if s == 0 else NB - s)
            c0 = blk * BL
            rhs = ub[:, c0:c0 + BL]
            if d == 1:
                rhs = rhs[:, ::-1]
            for g8 in range(8):
                vg = d * 8 + g8
                it += 1
                pS, pW = psS[it % 2], psW[it % 2]
                k.mm(pS[:], WB[:, vg, :], rhs)
                k.mm(pW[:], WBs[:, vg, :], rhs)
                t1, t2, w = T1[it % 2], T2[it % 2], Wt[it % 2]
                k.tt(t1[:], pS[:], Tc[:, vg, :], ALU.mult)
                k.tt(t2[:], pW[:], Tsg[:, vg, :], ALU.mult)
                k.tt(w[:], t1[:], t2[:], ALU.add, eng="pool")
                k.scan(G[vg][:], View(magS, magS.t[:, vg:vg + 1].to_broadcast([128, BL])), w[:], carry[:, vg:vg + 1])
                k.tt(M1[d][:, g8, :], G[vg][:], Tc[:, vg, :], ALU.mult, eng="pool")
                k.tt(M2[d][:, g8, :], G[vg][:], Tsg[:, vg, :], ALU.mult, eng="pool")
                k.copy(gl[:, vg:vg + 1], G[vg][:, BL - 1:BL], eng="act")
            py = psY[d]
            for g8 in range(8):
                vg = d * 8 + g8
                k.mm(py[:], CW[:, vg, :], M1[d][:, g8, :], start=(g8 == 0), stop=False, inc=False)
                k.mm(py[:], CWs[:, vg, :], M2[d][:, g8, :], start=False, stop=(g8 == 7), inc=(g8 == 7))
            src = py[:, :] if d == 0 else py[:, ::-1]
            k.tt(yacc[:, c0:c0 + BL], src, yacc[:, c0:c0 + BL], ALU.add)
        k.mm(psG[:], sw[:], gl[:])
        k.tt(c1[:], gl[:], Tc[:, :, BL - 1], ALU.mult)
        k.tt(c2[:], psG[:], Tsg[:, :, BL - 1], ALU.mult)
        k.tt(carry[:], c1[:], c2[:], ALU.subtract)
    for c in range(0, EXT, 2112):
        k.dma(yT[:, c:c + 2112], yacc[:, c:c + 2112])
    return k.finish()


def s5_maps(uT_all, inp, l):
    maps = []
    swapP = np.zeros((128, 128), np.float32)
    for p in range(64):
        swapP[p, p + 64] = 1.0; swapP[p + 64, p] = 1.0
    sgn = np.ones((128, 1), np.float32); sgn[64:] = -1.0
    for core in range(8):
        b, ct = core // 2, core % 2
        gs = slice(8 * ct, 8 * ct + 8)
        fl = lambda a: np.asarray(a[l][:, gs]).reshape((16,) + a.shape[3:])
        lre, lim, ldt = fl(inp["s5_lam_re"]), fl(inp["s5_lam_im"]), fl(inp["s5_log_dt"])
        bre, bim, cre, cim = fl(inp["s5_b_re"]), fl(inp["s5_b_im"]), fl(inp["s5_c_re"]), fl(inp["s5_c_im"])
        lamS = np.zeros((128, 16, 3), np.float32)
        lamS[:64, :, 0] = lre.T; lamS[64:, :, 0] = lre.T; lamS[:64, :, 1] = lim.T; lamS[64:, :, 1] = lim.T; lamS[:, :, 2] = ldt[None, :]
        lamR = np.broadcast_to(np.stack([lre, lim], -1)[None], (128, 16, 64, 2)).astype(np.float32)
        ldtR = np.broadcast_to(ldt[None], (128, 16)).astype(np.float32)
        Bre = np.zeros((128, 16, 64), np.float32); Bim = np.zeros((128, 16, 64), np.float32)
        CW0 = np.zeros((128, 16, 128), np.float32); CWs0 = np.zeros((128, 16, 128), np.float32)
        for vg in range(16):
            r0 = 16 * (vg % 8)
            Bre[r0:r0 + 16, vg, :] = bre[vg].T; Bim[r0:r0 + 16, vg, :] = bim[vg].T
            CW0[:64, vg, r0:r0 + 16] = cre[vg].T; CW0[64:, vg, r0:r0 + 16] = cim[vg].T
            CWs0[:64, vg, r0:r0 + 16] = cim[vg].T; CWs0[64:, vg, r0:r0 + 16] = cre[vg].T
        maps.append({"uT": np.ascontiguousarray(uT_all[b, ct * 128:(ct + 1) * 128]), "lamS": lamS, "lamR": np.ascontiguousarray(lamR),
                     "ldtR": np.ascontiguousarray(ldtR), "Bre": Bre, "Bim": Bim, "CW0": CW0, "CWs0": CWs0,
                     "dsk": np.ascontiguousarray(inp["s5_d"][l][ct * 128:(ct + 1) * 128, None]), "swapP": swapP, "sgn": sgn})
    return maps


def run_S5(uT_all, inp, l):
    res = run(build_S5(), s5_maps(uT_all, inp, l))
    y = np.empty((B, 256, EXT), np.float32)
    for core in range(8):
        y[core // 2, (core % 2) * 128:(core % 2 + 1) * 128] = res[core]["yT"]
    return y


GBLK = [(0, 256)] + [(256 + 512 * i, 512) for i in range(16)]


import os


def build_GLA():
    k = K()
    DBG = int(os.environ.get('GLA_DBG', '99'))
    qT = k.din("qT", [128, EXT]); kT = k.din("kT", [128, EXT]); gaT = k.din("gaT", [32, EXT]); v = k.din("v", [EXT, 256], BF16)
    wa2 = k.din("wa2", [16, 2, 128]); nba = k.din("nba", [128, 2]); maskd = k.din("mask", [64, 2, 512]); rmaskd = k.din("rmask", [128, 512])
    ident = k.din("ident", [128, 128])
    outs = [k.dout("oTf", [256, EXT]), k.dout("oTb", [256, EXT])]
    wa = k.sb([16, 2, 128]); nb_ = k.sb([128, 2]); mk = k.sb([64, 2, 512]); rm = k.sb([128, 512]); idf = k.sb([128, 128])
    k.dma(wa[:], wa2[:, :, :]); k.dma(nb_[:], nba[:, :]); k.dma(mk[:], maskd[:, :, :]); k.dma(rm[:], rmaskd[:, :]); k.dma(idf[:], ident[:, :])
    R2 = range(2)
    qf = [k.sb([128, 512]) for _ in R2]; kf = [k.sb([128, 512]) for _ in R2]; ga = [k.sb([16, 512]) for _ in R2]
    vb = [k.sb([64, 8, 256], BF16) for _ in R2]
    e1 = [k.sb([128, 512]) for _ in R2]; bp = [k.sb([128, 512]) for _ in R2]; eb = [k.sb([128, 512]) for _ in R2]; enb = [k.sb([128, 512]) for _ in R2]
    qt = [k.sb([128, 512]) for _ in R2]; kt = [k.sb([128, 512]) for _ in R2]; kend = [k.sb([128, 8, 64]) for _ in R2]
    dec = [k.sb([128, 8]) for _ in R2]; ktok = [k.sb([64, 8, 128], BF16) for _ in R2]
    att = [[k.sb([64, 8, 64], BF16) for _ in R2] for _ in R2]
    S = [k.sb([128, 9, 128]) for _ in R2]
    osb = [k.sb([128, 512]) for _ in range(4)]
    for d in R2:
        k.memset(S[d][:, 0, :], 0.0)
    z_ps = k.ps([128, 512]); tr_ps = [k.ps([64, 4, 128])] * 2; att_ps = [k.ps([64, 8, 64])] * 2
    otmp = k.sb([128, 512])
    kvb = k.ps([128, 4, 128]); kv_ps = [kvb[:, 0, :], kvb[:, 1, :]]
    o_ps = [k.ps([128, 8, 64]) for _ in R2]; oi_ps = [k.ps([128, 8, 64]) for _ in R2]
    it = 0
    for s in range(17 if DBG == 99 else 1):
        for d in R2:
            bi = s if d == 0 else (0 if s == 0 else 17 - s)
            c0, n = GBLK[bi]
            nch = n // 64
            r = it % 2
            it += 1
            k.dma(qf[r][:, :n], qT[:, c0:c0 + n]); k.dma(kf[r][:, :n], kT[:, c0:c0 + n])
            k.dma(ga[r][:, :n], gaT[16 * d:16 * d + 16, c0:c0 + n])
            k.dma(vb[r][:, :nch, :], v[c0:c0 + n, :].re("(c j) e -> j c e", j=64))
            k.mm(z_ps[:, :n], wa[:, d, :], ga[r][:, :n])
            k.act(e1[r][:, :n], z_ps[:, :n], AF.Exp, bias=nb_[:, d:d + 1], scale=-1.0)
            k.act(e1[r][:, :n], e1[r][:, :n], AF.Ln, bias=1.0)
            if d == 0:
                k.scan(bp[r][:, :n], rm[:, :n], e1[r][:, :n], 0.0)
            else:
                k.scan(bp[r][:, :n][:, ::-1], rm[:, :n], e1[r][:, :n][:, ::-1], 0.0)
            if DBG < 2:
                continue
            k.act(eb[r][:, :n], bp[r][:, :n], AF.Exp, scale=-1.0 / 16)
            k.act(enb[r][:, :n], bp[r][:, :n], AF.Exp, scale=1.0 / 16)
            k.tt(qt[r][:, :n], qf[r][:, :n], eb[r][:, :n], ALU.mult)
            k.tt(kt[r][:, :n], kf[r][:, :n], enb[r][:, :n], ALU.mult, eng="pool")
            ebv = eb[r][:, :n].re("p (c j) -> p c j", j=64)
            k.copy(dec[r][:, :nch], ebv[:, :, 63] if d == 0 else ebv[:, :, 0])
            decb = View(dec[r], dec[r].t[:, :nch].unsqueeze(2).to_broadcast([128, nch, 64]))
            k.tt(kend[r][:, :nch, :], kt[r][:, :n].re("p (c j) -> p c j", j=64), decb, ALU.mult)
            for c in range(nch):
                tp = tr_ps[(c // 4) % 2]
                k.tr(tp[:, c % 4, :], kend[r][:, c, :], idf[:], inc=(c % 4 == 3))
                if c % 4 == 3:
                    k.copy(ktok[r][:, c - 3:c + 1, :], tp[:], eng="act")
            if DBG < 3:
                continue
            for h2 in R2:
                hs = slice(64 * h2, 64 * h2 + 64)
                for c in range(nch):
                    k.mm(att_ps[h2][:, c, :], kt[r][hs, c * 64:(c + 1) * 64], qt[r][hs, c * 64:(c + 1) * 64], inc=(c == nch - 1))
                k.tt(att[r][h2][:, :nch, :], att_ps[h2][:, :nch, :], mk[:, d, :n].re("p (c i) -> p c i", i=64), ALU.mult)
            if DBG < 4:
                continue
            order = list(range(nch)) if d == 0 else list(range(nch - 1, -1, -1))
            for step, c in enumerate(order):
                kp = kv_ps[step % 2]
                for h2 in R2:
                    k.mm(kp[64 * h2:64 * h2 + 64, :], ktok[r][:, c, 64 * h2:64 * h2 + 64], vb[r][:, c, 128 * h2:128 * h2 + 128], inc=(h2 == 1))
                k.stt(S[d][:, step + 1, :], S[d][:, step, :], dec[r][:, c:c + 1], kp[:, :], ALU.mult, ALU.add)
            if DBG < 5:
                continue
            for h2 in R2:
                hs = slice(64 * h2, 64 * h2 + 64)
                for step, c in enumerate(order):
                    k.mm(o_ps[h2][:, c, :], vb[r][:, c, 128 * h2:128 * h2 + 128], att[r][h2][:, c, :], inc=(step == nch - 1))
                if DBG < 6:
                    continue
                for step, c in enumerate(order):
                    k.mm(oi_ps[h2][:, c, :], S[d][hs, step, :], qt[r][hs, c * 64:(c + 1) * 64], inc=(step == nch - 1))
                if DBG < 7:
                    continue
                ob = osb[(2 * it + h2) % 4]
                k.copy(otmp[:, :n], oi_ps[h2][:, :nch, :].re("p c i -> p (c i)"), eng="act")
                k.tt(ob[:, :n], o_ps[h2][:, :nch, :].re("p c i -> p (c i)"), otmp[:, :n], ALU.add)
                k.dma(outs[d][128 * h2:128 * h2 + 128, c0:c0 + n], ob[:, :n])
            if DBG < 8:
                continue
            k.copy(S[d][:, 0, :], S[d][:, nch, :])
    return k.finish()


def run_GLA(gq, gk, ga, gv, inp, l):
    i_ = np.arange(64)
    mf = (i_[None, :] >= i_[:, None]).astype(np.float32)
    mask = np.stack([np.tile(mf, (1, 8)), np.tile(mf.T, (1, 8))], axis=1)
    rmask = np.ones((128, 512), np.float32); rmask[:, 0::64] = 0.0
    ident = np.eye(128, dtype=np.float32)
    maps = []
    for core in range(8):
        b, hp = core // 2, core % 2
        cs = slice(128 * hp, 128 * hp + 128)
        wa2 = np.ascontiguousarray(inp["gla_w_a2"][l][:, :, cs].transpose(1, 0, 2))
        nba = np.ascontiguousarray((inp["gla_b_a"][l][:, cs]).T) * np.float32(-1.0)
        maps.append({"qT": np.ascontiguousarray(gq[b, cs]), "kT": np.ascontiguousarray(gk[b, cs]), "gaT": np.ascontiguousarray(ga[b]),
                     "v": np.ascontiguousarray(gv[b][:, 256 * hp:256 * hp + 256]), "wa2": wa2, "nba": nba, "mask": mask, "rmask": rmask, "ident": ident})
    res = run(build_GLA(), maps)
    of = np.empty((B, 512, EXT), np.float32); ob = np.empty((B, 512, EXT), np.float32)
    for core in range(8):
        b, hp = core // 2, core % 2
        of[b, 256 * hp:256 * hp + 256] = res[core]["oTf"]; ob[b, 256 * hp:256 * hp + 256] = res[core]["oTb"]
    return of, ob


@functools.lru_cache(None)
def build_NA():
    k = K()
    qT = k.din("qT", [128, EXT], BF16); kT = k.din("kT", [128, EXT], BF16); v = k.din("v", [EXT, 128], BF16)
    rpbg = k.din("rpbg", [64, 30, 64]); maskd = k.din("mask", [64, 64])
    out = k.dout("naT", [128, EXT], BF16)
    NT = EXT // 64
    q_sb = k.sb([64, 2, EXT], BF16, "na_q"); k_sb = k.sb([64, 2, EXT], BF16, "na_k"); v_sb = k.sb([64, NT, 128], BF16, "na_v")
    o_sb = k.sb([128, EXT], BF16, "na_o")
    for h in range(2):
        k.dma(q_sb[:, h, :], qT[64 * h:64 * h + 64, :]); k.dma(k_sb[:, h, :], kT[64 * h:64 * h + 64, :])
    vv = v[:, :].re("(t p) e -> p t e", p=64)
    for t0 in range(0, NT, 33):
        k.dma(v_sb[:, t0:t0 + 33, :], vv[:, t0:t0 + 33, :], chain=True)
    rb = k.sb([64, 30, 64]); mk = k.sb([64, 64]); E = k.sb([64, 30, 64], BF16); ones = k.sb([64, 64], BF16)
    k.dma(rb[:], rpbg[:, :, :]); k.dma(mk[:], maskd[:, :])
    k.act(rb[:], rb[:], AF.Exp)
    k.tt(E[:], rb[:], View(mk, mk.t[:, :].unsqueeze(1).to_broadcast([64, 30, 64])), ALU.mult)
    k.memset(ones[:], 1.0)
    stl = [k.ps([64, 8, 64]) for _ in range(2)]; stc = [k.ps([64, 8, 64]) for _ in range(2)]; po = [k.ps([128, 8, 64]) for _ in range(2)]
    pt = [k.sb([64, 12, 64], BF16) for _ in range(2)]; pe_ = [k.sb([64, 8, 64], BF16) for _ in range(2)]
    rden = [k.sb([128, 64]) for _ in range(2)]
    it = 0
    rows = [("c", j) for j in range(4)] + [("l", r) for r in range(128)]
    for ri, (kind, r) in enumerate(rows):
        q0 = 64 * r if kind == "c" else LC + 64 * r
        p_o = po[ri % 2]
        for h in range(2):
            it += 1
            sl, sc, p_t, p_e = stl[it % 2], stc[it % 2], pt[it % 2], pe_[it % 2]
            qv = q_sb[:, h, q0:q0 + 64]
            for j in range(4):
                k.mm(sc[:, j, :], k_sb[:, h, 64 * j:64 * j + 64], qv, inc=(j == 3))
            k.act(p_t[:, 0:4, :], sc[:, 0:4, :], AF.Exp)
            tiles = [j for j in range(4)]
            if kind == "l":
                kr0 = min(max(r - 4, 0), 120)
                dr0 = kr0 - r + 7
                for off in range(8):
                    kt = LC + 64 * (kr0 + off)
                    k.mm(sl[:, off, :], k_sb[:, h, kt:kt + 64], qv, inc=(off == 7))
                k.act(p_e[:], sl[:], AF.Exp)
                k.tt(p_t[:, 4:12, :], p_e[:], E[:, 15 * h + dr0:15 * h + dr0 + 8, :], ALU.mult, eng=("pool" if it % 2 else "dve"))
                tiles += [4 + kr0 + off for off in range(8)]
            n = len(tiles)
            ov = p_o[64 * h:64 * h + 64, 0, :]; dv = p_o[64 * h:64 * h + 64, 1, :]
            for i, vt in enumerate(tiles):
                k.mm(ov, v_sb[:, vt, 64 * h:64 * h + 64], p_t[:, i, :], start=(i == 0), stop=(i == n - 1), inc=(i == n - 1))
            for i, vt in enumerate(tiles):
                k.mm(dv, ones[:], p_t[:, i, :], start=(i == 0), stop=(i == n - 1), inc=(i == n - 1))
        rd = rden[ri % 2]
        k.recip(rd[:], p_o[:, 1, :])
        k.tt(o_sb[:, q0:q0 + 64], p_o[:, 0, :], rd[:], ALU.mult)
    for c in range(0, EXT, 2112):
        k.dma(out[:, c:c + 2112], o_sb[:, c:c + 2112])
    return k.finish()


def na_tables(rpb_l):
    c = np.arange(64)
    dc = np.clip(c[:, None] - c[None, :] + 15, 0, 30)
    cs = np.clip(c - 8, 0, 48)
    mask = ((c[:, None] >= cs[None, :]) & (c[:, None] < cs[None, :] + 16)).astype(np.float32)
    g = np.asarray(rpb_l)[:, :, dc]
    return np.ascontiguousarray(g.transpose(2, 0, 1, 3)), mask


def run_NA(nq, nk, nv, inp, l):
    g, mask = na_tables(inp["na_rpb"][l])
    maps = []
    for core in range(8):
        b, hp = core // 2, core % 2
        cs = slice(128 * hp, 128 * hp + 128)
        maps.append({"qT": np.ascontiguousarray(nq[b, cs]), "kT": np.ascontiguousarray(nk[b, cs]), "v": np.ascontiguousarray(nv[b][:, cs]),
                     "rpbg": np.ascontiguousarray(g[:, 2 * hp:2 * hp + 2].reshape(64, 30, 64)), "mask": mask})
    res = run(build_NA(), maps)
    o = np.empty((B, 256, EXT), NPBF)
    for core in range(8):
        o[core // 2, 128 * (core % 2):128 * (core % 2) + 128] = res[core]["naT"]
    return o


@functools.lru_cache(None)
def build_CV():
    k = K()
    cy = k.din("cy", [128, EXT], BF16); dw = k.din("dw", [128, 31]); dwb = k.din("dwb", [128, 1])
    out = k.dout("cvT", [128, EXT])
    W = 15 + LC + 30 + L + 15
    yp = k.sb([128, W], BF16, "cv_yp"); wt = k.sb([128, 31]); bt = k.sb([128, 1])
    k.dma(wt[:], dw[:, :]); k.dma(bt[:], dwb[:, :])
    k.memset(yp[:, 0:15], 0.0); k.memset(yp[:, 271:301], 0.0); k.memset(yp[:, 301 + L:W], 0.0)
    k.dma(yp[:, 15:271], cy[:, 0:LC]); k.dma(yp[:, 301:301 + L], cy[:, LC:EXT], chain=True)
    chunks = [(0, 0, LC)] + [(LC + c, 286 + c, 2048) for c in range(0, L, 2048)]
    for (o0, i0, n) in chunks:
        acc = k.sb([128, n])
        k.ts(acc[:], yp[:, i0:i0 + n], wt[:, 0:1], ALU.mult, bt[:, 0:1], ALU.add)
        for j in range(1, 31):
            k.stt(acc[:], yp[:, i0 + j:i0 + j + n], wt[:, j:j + 1], acc[:], ALU.mult, ALU.add)
        k.dma(out[:, o0:o0 + n], acc[:])
    return k.finish()


def run_CV(cy, inp, l):
    maps = []
    for core in range(8):
        b, ct = core // 2, core % 2
        cs = slice(128 * ct, 128 * ct + 128)
        maps.append({"cy": np.ascontiguousarray(cy[b, cs]), "dw": np.ascontiguousarray(inp["conv_dw"][l][:, cs].T),
                     "dwb": np.ascontiguousarray(inp["conv_dw_b"][l][cs, None])})
    res = run(build_CV(), maps)
    o = np.empty((B, 256, EXT), np.float32)
    for core in range(8):
        o[core // 2, 128 * (core % 2):128 * (core % 2) + 128] = res[core]["cvT"]
    return o


OBLK = [(i * 256, 256) for i in range(16)] + [(4096, 128)]


def fmv(dram, c0, n):
    return dram[:, c0:c0 + n].re("(t p) c -> p t c", p=128)


@functools.lru_cache(None)
def build_O1():
    k = K()
    xa = k.din("xa", [NLOC, D]); g1b = k.din("g1b", [128, 2, D])
    s5y = k.din("s5y", [256, NLOC]); glf = k.din("glf", [512, NLOC]); glb = k.din("glb", [512, NLOC])
    grs = k.din("grs", [512, NLOC], BF16); na = k.din("na", [256, NLOC], BF16); cv = k.din("cv", [256, NLOC])
    gs = k.din("gs", [4096, NLOC], BF16)
    w_glu = k.din("w_glu", [256, 256]); b_glu = k.din("b_glu", [128, 2]); w_s5o = k.din("w_s5o", [256, D])
    gng = k.din("gng", [128, 1]); w_glo = k.din("w_glo", [512, D]); w_nao = k.din("w_nao", [256, D])
    lng = k.din("lng", [128, 2]); lnb = k.din("lnb", [128, 2]); w_cvo = k.din("w_cvo", [256, D]); w_mix = k.din("w_mix", [D, D])
    x1 = k.dout("x1", [NLOC, D])

    stage = [k.sb([128, 1024]) for _ in range(2)]
    wi = [0]

    def load_cast(w_dram, kt, ncols, name):
        wb = k.sb([128, kt, ncols], BF16, name)
        for kk in range(kt):
            wi[0] += 1
            st = stage[wi[0] % 2]
            k.dma(st[:, :ncols], w_dram[kk * 128:(kk + 1) * 128, :])
            k.copy(wb[:, kk, :], st[:, :ncols], eng=("act" if wi[0] % 2 else "dve"))
        return wb

    wglu = load_cast(w_glu, 2, 256, "wglu"); ws5o = load_cast(w_s5o, 2, D, "ws5o"); wglo = load_cast(w_glo, 4, D, "wglo")
    wnao = load_cast(w_nao, 2, D, "wnao"); wcvo = load_cast(w_cvo, 2, D, "wcvo"); wmix = load_cast(w_mix, 8, D, "wmix")
    bglu = k.sb([128, 2]); gn = k.sb([128, 1]); lg = k.sb([128, 2]); lb = k.sb([128, 2]); g1t = k.sb([128, 2, D], name="g1t")
    k.dma(bglu[:], b_glu[:, :]); k.dma(gn[:], gng[:, :]); k.dma(lg[:], lng[:, :]); k.dma(lb[:], lnb[:, :]); k.dma(g1t[:], g1b[:, :, :])
    onesb = k.sb([128, 128], BF16); k.memset(onesb[:], 1.0)

    pss = [k.ps([128, 512]) for _ in range(8)]
    psi = [0]

    def nps():
        psi[0] += 1
        return pss[psi[0] % 8]

    R2 = range(2)
    NB = 256
    yt = [k.sb([128, 2, NB]) for _ in R2]; of_ = [k.sb([128, 4, NB]) for _ in R2]; ob_ = [k.sb([128, 4, NB]) for _ in R2]
    gr = [k.sb([128, 4, NB], BF16) for _ in R2]; cvt = [k.sb([128, 2, NB]) for _ in R2]; nat = [k.sb([128, 2, NB], BF16) for _ in R2]
    gst = k.sb([128, 32, NB], BF16, "gst")
    z = k.sb([128, 2, NB]); zb = k.sb([128, 2, NB], BF16); z2b = k.sb([128, 2, NB], BF16); sg = k.sb([128, NB])
    o_ = k.sb([128, 4, NB]); osq = k.sb([128, 4, NB], BF16); rs = k.sb([128, NB]); of2 = k.sb([128, 4, NB], BF16)
    cvb = k.sb([128, 2, NB], BF16); cvq = k.sb([128, 2, NB], BF16); mean = k.sb([128, NB]); msq = k.sb([128, NB]); var = k.sb([128, NB])
    dd = k.sb([128, NB]); cvo = k.sb([128, 2, NB], BF16); tq = [k.sb([128, NB]) for _ in range(4)]; mg = k.sb([128, 8, NB], BF16)
    xts = [k.sb([128, D]) for _ in R2]; xos = [k.sb([128, D]) for _ in R2]
    ti = 0
    for bi, (c0, n) in enumerate(OBLK):
        s = bi % 2
        k.dma(yt[s][:, :, :n], fmv(s5y, c0, n)); k.dma(of_[s][:, :, :n], fmv(glf, c0, n)); k.dma(ob_[s][:, :, :n], fmv(glb, c0, n))
        k.dma(gr[s][:, :, :n], fmv(grs, c0, n)); k.dma(cvt[s][:, :, :n], fmv(cv, c0, n)); k.dma(nat[s][:, :, :n], fmv(na, c0, n))
        k.dma(gst[:, :, :n], fmv(gs, c0, n))
        k.act(z[:, :, :n], yt[s][:, :, :n], AF.Gelu_apprx_tanh)
        k.copy(zb[:, :, :n], z[:, :, :n], eng="pool")
        for jt in R2:
            p = nps()
            for it_ in R2:
                k.mm(p[:, :n], wglu[:, it_, jt * 128:(jt + 1) * 128], zb[:, it_, :n], start=(it_ == 0), stop=(it_ == 1), inc=(it_ == 1))
            k.act(sg[:, :n], p[:, :n], AF.Sigmoid, bias=bglu[:, jt:jt + 1])
            k.tt(z2b[:, jt, :n], z[:, jt, :n], sg[:, :n], ALU.mult)
        k.tt(o_[:, :, :n], of_[s][:, :, :n], ob_[s][:, :, :n], ALU.add, eng="pool")
        k.tt(osq[:, :, :n], o_[:, :, :n], o_[:, :, :n], ALU.mult)
        for hh in range(4):
            p = nps()
            k.mm(p[:, :n], onesb[:], osq[:, hh, :n])
            k.ts(rs[:, :n], p[:, :n], 1.0 / 128, ALU.mult, EPS, ALU.add)
            k.act(rs[:, :n], rs[:, :n], AF.Sqrt)
            k.recip(rs[:, :n], rs[:, :n])
            k.tt(o_[:, hh, :n], o_[:, hh, :n], rs[:, :n], ALU.mult)
            k.stt(of2[:, hh, :n], o_[:, hh, :n], gn[:, 0:1], gr[s][:, hh, :n], ALU.mult, ALU.mult)
        k.copy(cvb[:, :, :n], cvt[s][:, :, :n], eng="pool")
        k.tt(cvq[:, :, :n], cvt[s][:, :, :n], cvt[s][:, :, :n], ALU.mult)
        p1 = nps(); p2 = nps()
        for t in R2:
            k.mm(p1[:, :n], onesb[:], cvb[:, t, :n], start=(t == 0), stop=(t == 1), inc=(t == 1))
        for t in R2:
            k.mm(p2[:, :n], onesb[:], cvq[:, t, :n], start=(t == 0), stop=(t == 1), inc=(t == 1))
        k.ts(mean[:, :n], p1[:, :n], 1.0 / 256, ALU.mult)
        k.tt(msq[:, :n], mean[:, :n], mean[:, :n], ALU.mult)
        k.stt(var[:, :n], p2[:, :n], 1.0 / 256, msq[:, :n], ALU.mult, ALU.subtract)
        k.ts(var[:, :n], var[:, :n], EPS, ALU.add)
        k.act(var[:, :n], var[:, :n], AF.Sqrt)
        k.recip(var[:, :n], var[:, :n])
        for t in R2:
            k.tt(dd[:, :n], cvt[s][:, t, :n], mean[:, :n], ALU.subtract)
            k.tt(dd[:, :n], dd[:, :n], var[:, :n], ALU.mult)
            k.act(cvo[:, t, :n], dd[:, :n], AF.Silu, scale=lg[:, t:t + 1], bias=lb[:, t:t + 1])
        srcs = [(ws5o, z2b, 2), (wglo, of2, 4), (wnao, nat[s], 2), (wcvo, cvo, 2)]
        for m in range(8):
            ps4 = [nps() for _ in range(4)]
            for i, (w_, a_, kt) in enumerate(srcs):
                for t in range(kt):
                    k.mm(ps4[i][:, :n], w_[:, t, m * 128:(m + 1) * 128], a_[:, t, :n], start=(t == 0), stop=(t == kt - 1), inc=(t == kt - 1))
            for i in range(4):
                k.tt(tq[i][:, :n], ps4[i][:, :n], gst[:, 8 * i + m, :n], ALU.mult)
            k.tt(tq[0][:, :n], tq[0][:, :n], tq[1][:, :n], ALU.add, eng="pool")
            k.tt(tq[2][:, :n], tq[2][:, :n], tq[3][:, :n], ALU.add, eng="pool")
            k.tt(mg[:, m, :n], tq[0][:, :n], tq[2][:, :n], ALU.add, eng="pool")
        for tt_ in range(n // 128):
            ti += 1
            tok0 = c0 + tt_ * 128
            xt, xo = xts[ti % 2], xos[ti % 2]
            k.dma(xt[:], xa[tok0:tok0 + 128, :])
            j = 1 if tok0 >= 4096 else 0
            for hf in R2:
                p = nps()
                for ft in range(8):
                    k.mm(p[:, :], mg[:, ft, tt_ * 128:(tt_ + 1) * 128], wmix[:, ft, hf * 512:(hf + 1) * 512], start=(ft == 0), stop=(ft == 7), inc=(ft == 7))
                k.tt(xo[:, hf * 512:(hf + 1) * 512], p[:, :], g1t[:, j, hf * 512:(hf + 1) * 512], ALU.mult)
            k.tt(xo[:], xo[:], xt[:], ALU.add, eng="pool")
            k.dma(x1[tok0:tok0 + 128, :], xo[:])
    return k.finish()


def scatter_fm(a, core):
    b, h = core // 2, core % 2
    return np.ascontiguousarray(np.concatenate([a[b][:, LC + h * 4096:LC + (h + 1) * 4096], a[b][:, h * 128:(h + 1) * 128]], 1))


def bc128(v):
    return np.broadcast_to(np.asarray(v, np.float32)[None], (128,) + np.asarray(v).shape)


def run_O1(xs, resA, y5, glf, glb, nao, cvo, mod_l, inp, l, cores=range(8)):
    maps = []
    for core in cores:
        b = core // 2
        g1b = np.ascontiguousarray(np.stack([bc128(mod_l[2, :, b]), bc128(mod_l[2, :, 4])], axis=1))
        maps.append({"xa": xs[core], "g1b": g1b, "s5y": scatter_fm(y5, core), "glf": scatter_fm(glf, core), "glb": scatter_fm(glb, core),
                     "grs": resA[core]["grsT"], "na": scatter_fm(nao, core), "cv": scatter_fm(cvo, core), "gs": resA[core]["gsT"],
                     "w_glu": inp["s5_w_glu"][l], "b_glu": np.ascontiguousarray(inp["s5_b_glu"][l].reshape(2, 128).T), "w_s5o": inp["s5_w_out"][l],
                     "gng": np.ascontiguousarray(inp["gla_norm_g"][l][:, None]), "w_glo": inp["gla_w_out"][l], "w_nao": inp["na_w_out"][l],
                     "lng": np.ascontiguousarray(inp["conv_ln_g"][l].reshape(2, 128).T), "lnb": np.ascontiguousarray(inp["conv_ln_b"][l].reshape(2, 128).T),
                     "w_cvo": inp["conv_w_out"][l], "w_mix": inp["w_mix_out"][l]})
    res = run(build_O1(), maps)
    return [r["x1"] for r in res]


O2PASS = [(0, 9), (9, 8), (17, 8), (25, 8)]


@functools.lru_cache(None)
def build_O2(last):
    k = K()
    x1 = k.din("x1", [NLOC, D]); modv = k.din("modv", [128, 8, 4]); g2n = k.din("g2n", [128, 8])
    g2b = k.din("g2b", [128, 2, D]); fgb = k.din("fgb", [128, D])
    wr = k.din("wr", [D, 36]); brb = k.din("brb", [128, 36])
    w1 = k.din("w1", [32, D, 256]); w3 = k.din("w3", [32, D, 256]); w2 = k.din("w2", [32, 256, D])
    ident = k.din("ident", [128, 128])
    out = k.dout("x2", [NLOC, D])

    idf = k.sb([128, 128]); k.dma(idf[:], ident[:, :])
    A_lat, B_lat, A_ctx, B_ctx = mod_prep(k, modv, g2n)
    wrt = k.sb([128, 8, 36]); brt = k.sb([128, 36]); g2t = k.sb([128, 2, D], name="g2t"); fgt = k.sb([128, D], name="fgt")
    k.dma(wrt[:], wr[:, :].re("(k p) c -> p k c", p=128)); k.dma(brt[:], brb[:, :]); k.dma(g2t[:], g2b[:, :, :]); k.dma(fgt[:], fgb[:, :])
    pss = [k.ps([128, 512]) for _ in range(8)]
    psi = [0]

    def nps():
        psi[0] += 1
        return pss[psi[0] % 8]

    R2 = range(2)
    MT = 9
    hT = k.sb([128, 8, MT * 128], BF16, "o2_hT"); yacc = k.sb([128, MT, D], name="o2_yacc"); comb = k.sb([128, 33, 32], name="o2_comb")
    st1 = k.sb([128, 8, 256], name="st1"); st3 = k.sb([128, 8, 256], name="st3"); st2 = k.sb([128, 2, D], name="st2")
    w1b = [k.sb([128, 8, 256], BF16) for _ in R2]; w3b = [k.sb([128, 8, 256], BF16) for _ in R2]; w2b = [k.sb([128, 2, D], BF16) for _ in R2]
    sa = [k.sb([128, 512]) for _ in R2]; actb = [[k.sb([128, 512], BF16) for _ in R2] for _ in R2]
    xt = [k.sb([128, D]) for _ in R2]; xn = k.sb([128, D]); junk = k.sb([128, D], BF16); tmpf = k.sb([128, 4, 128]); h32 = k.sb([128, 8, 128])
    yo = k.sb([128, D]); yo2 = k.sb([128, D])
    ss = k.sb([128, 1]); rr = k.sb([128, 1])
    lg = k.sb([128, 36]); gm = k.sb([128, 1]); ngm = k.sb([128, 1]); eg = k.sb([128, 4]); sgm = k.sb([128, 1]); gp = k.sb([128, 1])
    ohg = k.sb([128, 4]); t48 = k.sb([128, 4, 8]); sel = k.sb([128, 8]); sel2 = k.sb([128, 8]); m1 = k.sb([128, 1]); m2 = k.sb([128, 1])
    oh1 = k.sb([128, 8]); oh2 = k.sb([128, 8]); d21 = k.sb([128, 1]); e21 = k.sb([128, 1]); den = k.sb([128, 1]); wa = k.sb([128, 1]); wb_ = k.sb([128, 1])
    sw = k.sb([128, 8])

    def rms(src):
        k.act(junk[:], src, AF.Square, accum=ss[:])
        k.ts(rr[:], ss[:], 1.0 / D, ALU.mult, EPS, ALU.add)
        k.act(rr[:], rr[:], AF.Sqrt)
        k.recip(rr[:], rr[:])

    xi = 0
    for (p0, nt) in O2PASS:
        for li in range(nt):
            gi = p0 + li
            xi += 1
            x_ = xt[xi % 2]
            k.dma(x_[:], x1[gi * 128:(gi + 1) * 128, :])
            rms(x_[:])
            k.ts(xn[:], x_[:], rr[:, 0:1], ALU.mult)
            A_, B_ = (A_ctx, B_ctx) if gi == 32 else (A_lat, B_lat)
            for half in R2:
                tp = nps()
                for kk in range(4):
                    k.tr(tp[:, kk * 128:(kk + 1) * 128], xn[:, (half * 4 + kk) * 128:(half * 4 + kk + 1) * 128], idf[:], inc=(kk == 3))
                k.tt(tmpf[:], tp[:, :].re("p (k t) -> p k t", t=128),
                     View(A_, A_.t[:, half * 4:half * 4 + 4].unsqueeze(2).to_broadcast([128, 4, 128])), ALU.mult)
                k.tt(h32[:, half * 4:half * 4 + 4, :], tmpf[:],
                     View(B_, B_.t[:, half * 4:half * 4 + 4].unsqueeze(2).to_broadcast([128, 4, 128])), ALU.add)
            k.copy(hT[:, :, li * 128:(li + 1) * 128], h32[:], eng="pool")
            pr = nps()
            for kk in range(8):
                k.mm(pr[:, 0:36], h32[:, kk, :], wrt[:, kk, :], start=(kk == 0), stop=(kk == 7), inc=(kk == 7))
            k.tt(lg[:], pr[:, 0:36], brt[:], ALU.add)
            k.reduce(gm[:], lg[:, 0:4], ALU.max)
            k.ts(ngm[:], gm[:], -1.0, ALU.mult)
            k.act(eg[:], lg[:, 0:4], AF.Exp, bias=ngm[:, 0:1], accum=sgm[:])
            k.recip(gp[:], sgm[:])
            k.ts(ohg[:], lg[:, 0:4], gm[:, 0:1], ALU.is_equal)
            ohb = View(ohg, ohg.t[:, :].unsqueeze(2).to_broadcast([128, 4, 8]))
            k.tt(t48[:], lg[:, 4:36].re("p (g e) -> p g e", e=8), ohb, ALU.mult)
            k.reduce(sel[:], t48[:, :, :].re("p g e -> p e g"), ALU.add)
            k.reduce(m1[:], sel[:], ALU.max)
            k.ts(oh1[:], sel[:], m1[:, 0:1], ALU.is_equal)
            k.stt(sel2[:], oh1[:], -1.0e30, sel[:], ALU.mult, ALU.add)
            k.reduce(m2[:], sel2[:], ALU.max)
            k.ts(oh2[:], sel2[:], m2[:, 0:1], ALU.is_equal)
            k.tt(d21[:], m2[:], m1[:], ALU.subtract)
            k.act(e21[:], d21[:], AF.Exp)
            k.ts(den[:], e21[:], 1.0, ALU.add)
            k.recip(den[:], den[:])
            k.tt(wa[:], den[:], gp[:], ALU.mult)
            k.tt(wb_[:], wa[:], e21[:], ALU.mult)
            k.ts(sw[:], oh1[:], wa[:, 0:1], ALU.mult)
            k.stt(sw[:], oh2[:], wb_[:, 0:1], sw[:], ALU.mult, ALU.add)
            k.tt(comb[:, gi, :].re("p (g e) -> p g e", e=8), ohb, View(sw, sw.t[:, :].unsqueeze(1).to_broadcast([128, 4, 8])), ALU.mult)
        blocks = [(t0, min(4, nt - t0)) for t0 in range(0, nt, 4)]
        bi = 0
        for e in range(32):
            ws = e % 2
            k.dma(st1[:], w1[e].re("(k p) f -> p k f", p=128)); k.dma(st3[:], w3[e].re("(k p) f -> p k f", p=128))
            k.dma(st2[:], w2[e].re("(k p) f -> p k f", p=128))
            k.copy(w1b[ws][:], st1[:], eng="act"); k.copy(w3b[ws][:], st3[:], eng="pool"); k.copy(w2b[ws][:], st2[:], eng="pool")
            for (bt0, bnt) in blocks:
                bi += 1
                n = bnt * 128; c0 = bt0 * 128
                for ft in R2:
                    pa = nps(); pb = nps()
                    for kk in range(8):
                        k.mm(pa[:, :n], w1b[ws][:, kk, ft * 128:(ft + 1) * 128], hT[:, kk, c0:c0 + n], start=(kk == 0), stop=(kk == 7), inc=(kk == 7))
                    for kk in range(8):
                        k.mm(pb[:, :n], w3b[ws][:, kk, ft * 128:(ft + 1) * 128], hT[:, kk, c0:c0 + n], start=(kk == 0), stop=(kk == 7), inc=(kk == 7))
                    s_ = sa[ft]
                    k.act(s_[:, :n], pa[:, :n], AF.Silu)
                    k.tt(actb[bi % 2][ft][:, :n], pb[:, :n], s_[:, :n], ALU.mult)
                for t in range(bnt):
                    gi = p0 + bt0 + t
                    for hf in R2:
                        py = nps()
                        for ft in R2:
                            k.mm(py[:, :], actb[bi % 2][ft][:, t * 128:(t + 1) * 128], w2b[ws][:, ft, hf * 512:(hf + 1) * 512], start=(ft == 0), stop=(ft == 1), inc=(ft == 1))
                        ya = yacc[:, bt0 + t, hf * 512:(hf + 1) * 512]
                        if e == 0:
                            k.ts(ya, py[:, :], comb[:, gi, e:e + 1], ALU.mult)
                        else:
                            k.stt(ya, py[:, :], comb[:, gi, e:e + 1], ya, ALU.mult, ALU.add)
        for li in range(nt):
            gi = p0 + li
            xi += 1
            x_ = xt[xi % 2]
            k.dma(x_[:], x1[gi * 128:(gi + 1) * 128, :])
            j = 1 if gi == 32 else 0
            k.tt(yo[:], yacc[:, li, :], g2t[:, j, :], ALU.mult)
            k.tt(yo[:], yo[:], x_[:], ALU.add, eng="pool")
            if last:
                rms(yo[:])
                k.stt(yo2[:], yo[:], rr[:, 0:1], fgt[:], ALU.mult, ALU.mult)
                k.dma(out[gi * 128:(gi + 1) * 128, :], yo2[:])
            else:
                k.dma(out[gi * 128:(gi + 1) * 128, :], yo[:])
    return k.finish()


def run_O2(x1s, mod_l, inp, l, last, cores=range(8)):
    wr = np.ascontiguousarray(np.concatenate([inp["moe_w_group"][l], inp["moe_w_expert"][l]], axis=1))
    brb = np.ascontiguousarray(bc128(np.concatenate([inp["moe_b_group"][l], inp["moe_b_expert"][l]])))
    w1 = inp["moe_w1"][l].reshape(32, D, 256); w3 = inp["moe_w3"][l].reshape(32, D, 256); w2 = inp["moe_w2"][l].reshape(32, 256, D)
    fgb = np.ascontiguousarray(bc128(inp["final_norm_g"])); ident = np.eye(128, dtype=np.float32)
    maps = []
    for i, core in enumerate(cores):
        b = core // 2
        modv = np.stack([pk(mod_l[3, :, b]), pk(mod_l[4, :, b]), pk(mod_l[3, :, 4]), pk(mod_l[4, :, 4])], axis=2)
        g2b = np.ascontiguousarray(np.stack([bc128(mod_l[5, :, b]), bc128(mod_l[5, :, 4])], axis=1))
        maps.append({"x1": x1s[i], "modv": np.ascontiguousarray(modv), "g2n": pk(inp["norm2_g"][l]), "g2b": g2b, "fgb": fgb,
                     "wr": wr, "brb": brb, "w1": w1, "w3": w3, "w2": w2, "ident": ident})
    res = run(build_O2(bool(last)), maps)
    return [r["x2"] for r in res]


def unshard(x2s):
    x = np.empty((B, L, D), np.float32); xc = np.empty((B, LC, D), np.float32)
    for core in range(8):
        b, h = core // 2, core % 2
        x[b, h * 4096:(h + 1) * 4096] = x2s[core][:4096]; xc[b, h * 128:(h + 1) * 128] = x2s[core][4096:]
    return x, xc


def kernel(**inp):
    inp = {k_: np.asarray(v_) for k_, v_ in inp.items()}
    mod = run_M(inp)
    x, xc = inp["x"], inp["ctx"]
    for l in range(2):
        xs = shard_tokens(x, xc)
        resA = run_A(x, xc, mod[l], inp, l)
        y5 = run_S5(gather_fm(resA, "uT"), inp, l)
        glf, glb = run_GLA(gather_fm(resA, "gqT"), gather_fm(resA, "gkT"), gather_fm(resA, "gaT"), gather_tm(resA, "gv"), inp, l)
        nao = run_NA(gather_fm(resA, "nqT"), gather_fm(resA, "nkT"), gather_tm(resA, "nv"), inp, l)
        cvo = run_CV(gather_fm(resA, "cyT"), inp, l)
        x1s = run_O1(xs, resA, y5, glf, glb, nao, cvo, mod[l], inp, l)
        del resA, y5, glf, glb, nao, cvo
        x2s = run_O2(x1s, mod[l], inp, l, l == 1)
        x, xc = unshard(x2s)
    return x
```

```python
import functools
import numpy as np
import ml_dtypes
import concourse.bass as bass
import concourse.mybir as mybir
from concourse.bass_utils import run_bass_kernel_spmd
from contextlib import ExitStack

F32 = mybir.dt.float32
BF16 = mybir.dt.bfloat16
AF = mybir.ActivationFunctionType
ALU = mybir.AluOpType
AX = mybir.AxisListType
NPBF = ml_dtypes.bfloat16
PI = float(np.pi)

NCORE = 8
D = 1024
B = 4
L = 8192
LC = 256
NLOC = 4096 + 128
EXT = LC + L
EPS = 1e-6


class View:
    __slots__ = ("b", "ap")

    def __init__(self, b, ap):
        self.b = b
        self.ap = ap

    def __getitem__(self, k):
        return View(self.b, self.ap[k])

    def bc(self, shape):
        return View(self.b, self.ap.to_broadcast(list(shape)))

    def re(self, s, **kw):
        return View(self.b, self.ap.rearrange(s, **kw))


class Buf:
    __slots__ = ("t", "name", "lw", "rd", "dsem", "dcnt", "dram")

    def __init__(self, t, name, dram=False):
        self.t = t
        self.name = name
        self.lw = None
        self.rd = []
        self.dsem = None
        self.dcnt = 0
        self.dram = dram

    def __getitem__(self, k):
        return View(self, self.t[k])


def _v(x):
    return x.ap if isinstance(x, View) else x


class K:
    def __init__(self):
        self.nc = bass.Bass("TRN2", target_bir_lowering=False)
        self.es = ExitStack()
        nc = self.nc
        self.eng = {"pe": nc.tensor, "act": nc.scalar, "dve": nc.vector, "pool": nc.gpsimd, "sp": nc.sync}
        self.sem = {}
        self.cnt = {}
        self.semobj = {}
        for e in self.eng:
            s = self.es.enter_context(nc.semaphore("s_" + e))
            self.sem[e] = s
            self.cnt[e] = 0
            self.semobj[("E", e)] = s
        self.waited = {e: {} for e in self.eng}
        self.nb = 0
        self.ninst = 0
        self.dbufs = []
        self.stk = [self.es]

    def sb(self, shape, dt=F32, name=None):
        self.nb += 1
        name = name or f"sb{self.nb}"
        return Buf(self.stk[-1].enter_context(self.nc.sbuf_tensor(name, list(shape), dt)), name)

    def barrier(self):
        deps = [(("E", e), self.cnt[e]) for e in self.eng] + [(b.dsem, b.dcnt) for b in self.dbufs]
        for e in self.eng:
            self._wait(e, deps)

    def push(self):
        self.stk.append(ExitStack())

    def pop(self):
        self.barrier()
        self.stk.pop().close()

    def ps(self, shape, dt=F32, name=None):
        self.nb += 1
        name = name or f"ps{self.nb}"
        return Buf(self.es.enter_context(self.nc.psum_tensor(name, list(shape), dt)), name)

    def din(self, name, shape, dt=F32):
        return Buf(self.nc.dram_tensor(name, list(shape), dt, kind="ExternalInput").ap(), name, dram=True)

    def dout(self, name, shape, dt=F32):
        return Buf(self.nc.dram_tensor(name, list(shape), dt, kind="ExternalOutput").ap(), name, dram=True)

    def _wait(self, e, deps):
        eng = self.eng[e]
        w = self.waited[e]
        best = {}
        for d in deps:
            if d is None:
                continue
            kk, v = d
            if kk == ("E", e) and v > self.cnt[e]:
                v = self.cnt[e]
            if best.get(kk, 0) < v:
                best[kk] = v
        for kk, v in best.items():
            if w.get(kk, 0) < v:
                eng.wait_ge(self.semobj[kk], v)
                w[kk] = v

    def op(self, e, fn, reads=(), writes=(), inc=True):
        reads = [r.b if isinstance(r, View) else r for r in reads if r is not None and not isinstance(r, (int, float))]
        writes = [r.b if isinstance(r, View) else r for r in writes]
        deps = []
        for b in reads:
            deps.append(b.lw)
        for b in writes:
            deps.append(b.lw)
            deps.extend(b.rd)
        self._wait(e, deps)
        inst = fn(self.eng[e])
        self.ninst += 1
        if inc:
            self.cnt[e] += 1
            inst.then_inc(self.sem[e], 1)
            tk = (("E", e), self.cnt[e])
        else:
            tk = (("E", e), self.cnt[e] + 1)
        for b in writes:
            b.lw = tk
            b.rd = []
        for b in reads:
            if b not in writes and not b.dram:
                b.rd.append(tk)
        return inst

    def dma(self, out, in_, q="sp", chain=False, **kw):
        ob, ib = out.b, in_.b
        own = ib if ob.dram else ob
        if own.dsem is None:
            s = self.es.enter_context(self.nc.semaphore("d_" + own.name))
            own.dsem = ("D", own.name)
            self.semobj[own.dsem] = s
            self.dbufs.append(own)
        key = own.dsem
        deps = [ib.lw]
        ch = chain and ob.lw is not None and ob.lw[0] == key
        if not ch:
            deps.append(ob.lw)
        deps.extend(ob.rd)
        self._wait(q, deps)
        inst = self.eng[q].dma_start(out=out.ap, in_=in_.ap, **kw)
        self.ninst += 1
        own.dcnt += 16
        inst.then_inc(self.semobj[key], 16)
        tk = (key, own.dcnt)
        if not ob.dram:
            ob.lw = tk
            if not ch:
                ob.rd = []
        if not ib.dram:
            ib.rd.append(tk)
        return inst

    def finish(self, q="sp"):
        self._wait(q, [(b.dsem, b.dcnt) for b in self.dbufs])
        self.es.close()
        return self.nc

    def mm(self, out, lhsT, rhs, start=True, stop=True, inc=True):
        return self.op("pe", lambda e: e.matmul(out.ap, lhsT=lhsT.ap, rhs=rhs.ap, start=start, stop=stop),
                       reads=[lhsT, rhs] + ([] if start else [out]), writes=[out], inc=inc)

    def tr(self, out, in_, ident, inc=True):
        return self.op("pe", lambda e: e.transpose(out.ap, in_.ap, ident.ap), reads=[in_, ident], writes=[out], inc=inc)

    def act(self, out, in_, func, bias=None, scale=None, accum=None, eng="act"):
        kw = {}
        if bias is not None:
            kw["bias"] = _v(bias)
        if scale is not None:
            kw["scale"] = _v(scale)
        if accum is not None:
            kw["accum_out"] = accum.ap
        return self.op(eng, lambda e: e.activation(out=out.ap, in_=in_.ap, func=func, **kw),
                       reads=[in_, bias, scale], writes=[out] + ([accum] if accum is not None else []))

    def tt(self, out, in0, in1, op, eng="dve"):
        return self.op(eng, lambda e: e.tensor_tensor(out=out.ap, in0=in0.ap, in1=in1.ap, op=op), reads=[in0, in1], writes=[out])

    def ts(self, out, in0, s1, op0, s2=None, op1=None, eng="dve"):
        if op1 is None:
            return self.op(eng, lambda e: e.tensor_scalar(out=out.ap, in0=in0.ap, scalar1=_v(s1), scalar2=None, op0=op0),
                           reads=[in0, s1], writes=[out])
        return self.op(eng, lambda e: e.tensor_scalar(out=out.ap, in0=in0.ap, scalar1=_v(s1), scalar2=_v(s2), op0=op0, op1=op1),
                       reads=[in0, s1, s2], writes=[out])

    def stt(self, out, in0, scalar, in1, op0, op1, eng="dve"):
        return self.op(eng, lambda e: e.scalar_tensor_tensor(out=out.ap, in0=in0.ap, scalar=_v(scalar), in1=in1.ap, op0=op0, op1=op1),
                       reads=[in0, scalar, in1], writes=[out])

    def scan(self, out, d0, d1, initial, op0=ALU.mult, op1=ALU.add):
        return self.op("dve", lambda e: e.tensor_tensor_scan(out=out.ap, data0=d0.ap, data1=d1.ap, initial=_v(initial), op0=op0, op1=op1),
                       reads=[d0, d1, initial], writes=[out])

    def copy(self, out, in_, eng="dve"):
        if eng == "act":
            return self.act(out, in_, AF.Copy)
        return self.op(eng, lambda e: e.tensor_copy(out=out.ap, in_=in_.ap), reads=[in_], writes=[out])

    def memset(self, out, val, eng="dve"):
        return self.op(eng, lambda e: e.memset(out.ap, val), writes=[out])

    def recip(self, out, in_):
        return self.op("dve", lambda e: e.reciprocal(out=out.ap, in_=in_.ap), reads=[in_], writes=[out])

    def reduce(self, out, in_, op, axis=AX.X):
        return self.op("dve", lambda e: e.tensor_reduce(out=out.ap, in_=in_.ap, axis=axis, op=op), reads=[in_], writes=[out])

    def wrap(self, out, in_, shift):
        return self.op("dve", lambda e: e.add_range_wrap(out=out.ap, in_=in_.ap, shift=shift, bound=PI, period=2 * PI),
                       reads=[in_], writes=[out])


def run(nc, in_maps):
    return run_bass_kernel_spmd(nc, in_maps, core_ids=list(range(len(in_maps)))).results


@functools.lru_cache(None)
def build_M():
    k = K()
    cT = k.din("cT", [128, 8, 8]); w = k.din("w", [1024, 1536]); bm = k.din("bm", [128, 12])
    out = k.dout("mod", [128, 12, 8])
    ct = k.sb([128, 8, 8]); ca = k.sb([128, 8, 8]); wt = k.sb([128, 8, 1536]); bt = k.sb([128, 12]); ot = k.sb([128, 12, 8])
    ps = [k.ps([128, 8]) for _ in range(2)]
    k.dma(ct[:], cT[:, :, :]); k.dma(bt[:], bm[:, :])
    wv = w[:, :].re("(k p) c -> p k c", p=128)
    for kk in range(8):
        k.dma(wt[:, kk, :], wv[:, kk, :], chain=True)
    k.act(ca[:], ct[:], AF.Silu)
    for m in range(12):
        p = ps[m % 2]
        for kk in range(8):
            k.mm(p[:], wt[:, kk, m * 128:(m + 1) * 128], ca[:, kk, :], start=(kk == 0), stop=(kk == 7), inc=(kk == 7))
        k.act(ot[:, m, :], p[:], AF.Identity, bias=bt[:, m:m + 1])
    k.dma(out[:, :, :], ot[:])
    return k.finish()


def run_M(inp):
    c, c_ctx, w_mod, b_mod = inp["c"], inp["c_ctx"], inp["w_mod"], inp["b_mod"]
    cc = np.zeros((8, D), np.float32); cc[:4] = c; cc[4] = c_ctx
    cT = np.ascontiguousarray(cc.T.reshape(8, 128, 8).transpose(1, 0, 2))
    maps = []
    for core in range(8):
        l, q = core // 4, core % 4
        maps.append({"cT": cT, "w": np.ascontiguousarray(w_mod[l][:, q * 1536:(q + 1) * 1536]),
                     "bm": np.ascontiguousarray(b_mod[l][q * 1536:(q + 1) * 1536].reshape(12, 128).T)})
    res = run(build_M(), maps)
    mod = np.zeros((2, 6 * D, 8), np.float32)
    for core in range(8):
        l, q = core // 4, core % 4
        mod[l, q * 1536:(q + 1) * 1536] = res[core]["mod"].transpose(1, 0, 2).reshape(1536, 8)
    return mod.reshape(2, 6, D, 8)


def pk(v):
    return np.ascontiguousarray(np.asarray(v, np.float32).reshape(8, 128).T)


TOKBLK = [(i * 512, 512) for i in range(8)] + [(4096, 128)]
OFF = dict(s5_u=0, gla_q=256, gla_k=512, gla_v=768, gla_r=1280, gla_a=1792, na_q=1824, na_k=2080, na_v=2336, conv_in=2592, gates=3104)


def norm_mod_T(k, x_dram, ntile, A_lat, B_lat, A_ctx, B_ctx, hT, ident_b, ps_t, hT32=None, ident_f=None, after_tile=None):
    xt = [k.sb([128, 1024]) for _ in range(2)]
    xn = [k.sb([128, 1024], BF16 if hT32 is None else F32) for _ in range(2)]
    junk = k.sb([128, 1024], BF16)
    tmpf = k.sb([128, 8, 128])
    ss = [k.sb([128, 1]) for _ in range(2)]
    rs = [k.sb([128, 1]) for _ in range(2)]
    for i in range(ntile):
        x_, n_, s_, r_ = xt[i % 2], xn[i % 2], ss[i % 2], rs[i % 2]
        k.dma(x_[:], x_dram[i * 128:(i + 1) * 128, :])
        k.act(junk[:], x_[:], AF.Square, accum=s_[:])
        k.ts(r_[:], s_[:], 1.0 / D, ALU.mult, EPS, ALU.add)
        k.act(r_[:], r_[:], AF.Sqrt)
        k.recip(r_[:], r_[:])
        k.ts(n_[:], x_[:], r_[:, 0:1], ALU.mult)
        tp = ps_t[i % 2]
        for kk in range(8):
            k.tr(tp[:, kk, :], n_[:, kk * 128:(kk + 1) * 128], (ident_b if hT32 is None else ident_f)[:], inc=(kk == 7))
        A_, B_ = (A_ctx, B_ctx) if i == ntile - 1 else (A_lat, B_lat)
        k.tt(tmpf[:], tp[:], View(A_, A_.t[:, :].unsqueeze(2).to_broadcast([128, 8, 128])), ALU.mult)
        Bb = View(B_, B_.t[:, :].unsqueeze(2).to_broadcast([128, 8, 128]))
        if hT32 is None:
            k.tt(hT[:, :, i * 128:(i + 1) * 128], tmpf[:], Bb, ALU.add, eng="pool")
        else:
            k.tt(hT32[:], tmpf[:], Bb, ALU.add)
            k.copy(hT[:, :, i * 128:(i + 1) * 128], hT32[:], eng="pool")
            after_tile(i)


def mod_prep(k, modv, gn):
    mt = k.sb([128, 8, 4]); gt = k.sb([128, 8])
    k.dma(mt[:], modv[:, :, :]); k.dma(gt[:], gn[:, :])
    res = []
    for j in (0, 2):
        A = k.sb([128, 8]); Bv = k.sb([128, 8])
        k.ts(A[:], mt[:, :, j + 1], 1.0, ALU.add)
        k.tt(A[:], A[:], gt[:], ALU.mult)
        k.copy(Bv[:], mt[:, :, j])
        res += [A, Bv]
    return res


@functools.lru_cache(None)
def build_A():
    k = K()
    xa = k.din("xa", [NLOC, D]); modv = k.din("modv", [128, 8, 4]); g1n = k.din("g1n", [128, 8])
    w_in = k.din("w_in", [D, 7200]); gate_b = k.din("gate_b", [128, 32])
    ropeC = k.din("ropeC", [128, NLOC]); ropeS = k.din("ropeS", [128, NLOC]); ident = k.din("ident", [128, 128])
    o_uT = k.dout("uT", [256, NLOC]); o_gq = k.dout("gqT", [256, NLOC]); o_gk = k.dout("gkT", [256, NLOC])
    o_ga = k.dout("gaT", [32, NLOC]); o_gr = k.dout("grsT", [512, NLOC], BF16); o_gv = k.dout("gv", [NLOC, 512], BF16)
    o_nq = k.dout("nqT", [256, NLOC], BF16); o_nk = k.dout("nkT", [256, NLOC], BF16); o_nv = k.dout("nv", [NLOC, 256], BF16)
    o_cy = k.dout("cyT", [256, NLOC], BF16); o_gs = k.dout("gsT", [4096, NLOC], BF16)

    hT = k.sb([128, 8, NLOC], BF16, "hT")
    idf = k.sb([128, 128]); idb = k.sb([128, 128], BF16)
    k.dma(idf[:], ident[:, :]); k.copy(idb[:], idf[:])
    A_lat, B_lat, A_ctx, B_ctx = mod_prep(k, modv, g1n)
    gb = k.sb([128, 32]); k.dma(gb[:], gate_b[:, :])
    rc = k.sb([128, NLOC]); rsn = k.sb([128, NLOC])
    k.dma(rc[:], ropeC[:, :]); k.dma(rsn[:], ropeS[:, :])
    ps_t = [k.ps([128, 8, 128], BF16) for _ in range(2)]
    norm_mod_T(k, xa, 33, A_lat, B_lat, A_ctx, B_ctx, hT, idb, ps_t)

    pss = [k.ps([128, 512]) for _ in range(6)]
    psi = [0]

    def nps():
        psi[0] += 1
        return pss[psi[0] % 6]

    wf = [k.sb([128, 8, 512]) for _ in range(2)]
    wq = k.sb([128, 8, 256])
    wbq = k.sb([128, 8, 256], BF16)
    wb = [k.sb([128, 8, 512], BF16) for _ in range(3)]
    wi = [0]
    stf = [k.sb([128, 512]) for _ in range(3)]
    stb = [k.sb([128, 512], BF16) for _ in range(3)]
    t1 = k.sb([128, 512]); t2 = k.sb([128, 512])
    si = [0]

    def load_w(col0, ncols, scale=None, swap=False):
        wi[0] += 1
        f = wq if swap else wf[wi[0] % 2]
        b = wbq if swap else wb[wi[0] % 3]
        for kk in range(8):
            k.dma(f[:, kk, :ncols], w_in[kk * 128:(kk + 1) * 128, col0:col0 + ncols], chain=True)
        if swap:
            sc = 1.0 if scale is None else scale
            fv = f[:, :, :].re("p k (g t c) -> p (k g) t c", t=2, c=16)
            bv = b[:, :, :].re("p k (g t c) -> p (k g) t c", t=2, c=16)
            k.act(bv[:, :, 0, :], fv[:, :, 1, :], AF.Copy, scale=-sc)
            k.act(bv[:, :, 1, :], fv[:, :, 0, :], AF.Copy, scale=sc)
        elif scale is not None:
            k.act(b[:, :, :ncols], f[:, :, :ncols], AF.Copy, scale=scale)
        else:
            k.copy(b[:, :, :ncols], f[:, :, :ncols], eng=("dve" if wi[0] % 2 else "pool"))
        return b

    def fm_mm(b, c0, M, tok0, n):
        p = nps()
        for kk in range(8):
            k.mm(p[:M, :n], b[:, kk, c0:c0 + M], hT[:, kk, tok0:tok0 + n], start=(kk == 0), stop=(kk == 7), inc=(kk == 7))
        return p

    def fm_simple(col0, ncols, out, post, bf, scale=None):
        for g0 in range(0, ncols, 512):
            gn = min(512, ncols - g0)
            b = load_w(col0 + g0, gn, scale=scale)
            for c0 in range(0, gn, 128):
                M = min(128, gn - c0)
                for (tok0, n) in TOKBLK:
                    p = fm_mm(b, c0, M, tok0, n)
                    si[0] += 1
                    st = (stb if bf else stf)[si[0] % 3]
                    post(st[:M, :n], p[:M, :n], (g0 + c0) // 128)
                    k.dma(out[g0 + c0:g0 + c0 + M, tok0:tok0 + n], st[:M, :n])

    def tm_part(col0, ncols, out):
        b = load_w(col0, ncols)
        for i in range(33):
            p = nps()
            for kk in range(8):
                k.mm(p[:, :ncols], hT[:, kk, i * 128:(i + 1) * 128], b[:, kk, :ncols], start=(kk == 0), stop=(kk == 7), inc=(kk == 7))
            si[0] += 1
            st = stb[si[0] % 3]
            k.copy(st[:, :ncols], p[:, :ncols], eng=("act" if i % 2 else "dve"))
            k.dma(out[i * 128:(i + 1) * 128, :], st[:, :ncols])

    def rope_part(col0, out, scale):
        b = load_w(col0, 256, scale=scale)
        bs = load_w(col0, 256, scale=scale, swap=True)
        for c0 in (0, 128):
            for (tok0, n) in TOKBLK:
                p = fm_mm(b, c0, 128, tok0, n)
                p2 = fm_mm(bs, c0, 128, tok0, n)
                si[0] += 1
                st = stf[si[0] % 3]
                k.tt(t1[:, :n], p[:, :n], rc[:, tok0:tok0 + n], ALU.mult)
                k.tt(t2[:, :n], p2[:, :n], rsn[:, tok0:tok0 + n], ALU.mult)
                k.tt(st[:, :n], t1[:, :n], t2[:, :n], ALU.add, eng="pool")
                k.dma(out[c0:c0 + 128, tok0:tok0 + n], st[:, :n])

    cp = [0]

    def post_copy(st, p, t):
        cp[0] += 1
        k.copy(st, p, eng=("act" if cp[0] % 2 else "dve"))

    fm_simple(OFF["s5_u"], 256, o_uT, post_copy, False)
    rope_part(OFF["gla_q"], o_gq, 0.125)
    rope_part(OFF["gla_k"], o_gk, None)
    tm_part(OFF["gla_v"], 512, o_gv)
    fm_simple(OFF["gla_r"], 512, o_gr, lambda st, p, t: k.act(st, p, AF.Silu), True)
    fm_simple(OFF["gla_a"], 32, o_ga, post_copy, False)
    fm_simple(OFF["na_q"], 256, o_nq, post_copy, True, scale=0.125)
    fm_simple(OFF["na_k"], 256, o_nk, post_copy, True)
    tm_part(OFF["na_v"], 256, o_nv)
    b = load_w(OFF["conv_in"], 512)
    for ct in range(2):
        for (tok0, n) in TOKBLK:
            pv = fm_mm(b, ct * 128, 128, tok0, n)
            pg = fm_mm(b, 256 + ct * 128, 128, tok0, n)
            si[0] += 1
            st = stb[si[0] % 3]
            k.act(t1[:, :n], pg[:, :n], AF.Sigmoid)
            k.tt(st[:, :n], pv[:, :n], t1[:, :n], ALU.mult)
            k.dma(o_cy[ct * 128:(ct + 1) * 128, tok0:tok0 + n], st[:, :n])
    for g0 in range(0, 4096, 512):
        b = load_w(OFF["gates"] + g0, 512)
        for c0 in range(0, 512, 128):
            t = (g0 + c0) // 128
            for (tok0, n) in TOKBLK:
                p = fm_mm(b, c0, 128, tok0, n)
                si[0] += 1
                st = stb[si[0] % 3]
                k.act(st[:, :n], p[:, :n], AF.Sigmoid, bias=gb[:, t:t + 1])
                k.dma(o_gs[t * 128:(t + 1) * 128, tok0:tok0 + n], st[:, :n])
    return k.finish()


def rope_tables():
    inv = (10000.0 ** (-np.arange(0, 32, 2, dtype=np.float32) / 32)).astype(np.float32)
    tabs = []
    for half in range(2):
        t = np.arange(4096) + half * 4096
        row = (t // 64).astype(np.float32); col = (t % 64).astype(np.float32)
        ang = np.zeros((64, 4096), np.float32)
        ang[0:16] = inv[:, None] * row[None]; ang[16:32] = ang[0:16]
        ang[32:48] = inv[:, None] * col[None]; ang[48:64] = ang[32:48]
        c = np.ones((128, NLOC), np.float32); s = np.zeros((128, NLOC), np.float32)
        c[:, :4096] = np.tile(np.cos(ang), (2, 1)); s[:, :4096] = np.tile(np.sin(ang), (2, 1))
        tabs.append((c, s))
    return tabs


def shard_tokens(x, xc):
    out = []
    for core in range(8):
        b, h = core // 2, core % 2
        out.append(np.ascontiguousarray(np.concatenate([x[b, h * 4096:(h + 1) * 4096], xc[b, h * 128:(h + 1) * 128]], 0)))
    return out


def gather_fm(res, name):
    C = res[0][name].shape[0]
    out = np.empty((B, C, EXT), res[0][name].dtype)
    for core in range(8):
        b, h = core // 2, core % 2
        a = res[core][name]
        out[b, :, LC + h * 4096:LC + (h + 1) * 4096] = a[:, :4096]
        out[b, :, h * 128:(h + 1) * 128] = a[:, 4096:]
    return out


def gather_tm(res, name):
    C = res[0][name].shape[1]
    out = np.empty((B, EXT, C), res[0][name].dtype)
    for core in range(8):
        b, h = core // 2, core % 2
        a = res[core][name]
        out[b, LC + h * 4096:LC + (h + 1) * 4096] = a[:4096]
        out[b, h * 128:(h + 1) * 128] = a[4096:]
    return out


def run_A(x, xc, mod_l, inp, l):
    tabs = rope_tables()
    xs = shard_tokens(x, xc)
    ident = np.eye(128, dtype=np.float32)
    maps = []
    for core in range(8):
        b, h = core // 2, core % 2
        modv = np.stack([pk(mod_l[0, :, b]), pk(mod_l[1, :, b]), pk(mod_l[0, :, 4]), pk(mod_l[1, :, 4])], axis=2)
        maps.append({"xa": xs[core], "modv": np.ascontiguousarray(modv), "g1n": pk(inp["norm1_g"][l]), "w_in": inp["w_in"][l],
                     "gate_b": np.ascontiguousarray(inp["gate_b"][l].reshape(32, 128).T),
                     "ropeC": tabs[h][0], "ropeS": tabs[h][1], "ident": ident})
    return run(build_A(), maps)


def sincos(k, th, shape):
    I32 = mybir.dt.int32
    res = []
    for shift in (PI / 2, 0.0):
        a = k.sb(shape); ni = k.sb(shape, I32); nf = k.sb(shape)
        k.ts(a[:], th, shift, ALU.add)
        k.ts(nf[:], a[:], 1.0 / (2 * PI), ALU.mult)
        k.copy(ni[:], nf[:])
        k.copy(nf[:], ni[:])
        k.stt(a[:], nf[:], -2 * PI, a[:], ALU.mult, ALU.add)
        k.ts(nf[:], a[:], PI, ALU.is_gt, -2 * PI, ALU.mult)
        k.tt(a[:], a[:], nf[:], ALU.add)
        k.ts(nf[:], a[:], -PI, ALU.is_lt, 2 * PI, ALU.mult)
        k.tt(a[:], a[:], nf[:], ALU.add)
        k.ts(a[:], a[:], -PI, ALU.max, PI, ALU.min)
        k.act(a[:], a[:], AF.Sin)
        res.append(a)
    return res[0], res[1]


S5BL = 256
S5NB = EXT // S5BL


@functools.lru_cache(None)
def build_S5():
    k = K()
    BL, NB = S5BL, S5NB
    uT = k.din("uT", [128, EXT]); lamS = k.din("lamS", [128, 16, 3]); lamR = k.din("lamR", [128, 16, 64, 2]); ldtR = k.din("ldtR", [128, 16])
    Bre = k.din("Bre", [128, 16, 64]); Bim = k.din("Bim", [128, 16, 64]); CW0 = k.din("CW0", [128, 16, 128]); CWs0 = k.din("CWs0", [128, 16, 128])
    dsk = k.din("dsk", [128, 1]); swapP = k.din("swapP", [128, 128]); sgn = k.din("sgn", [128, 1])
    yT = k.dout("yT", [128, EXT])

    Tc = k.sb([128, 16, BL], name="Tc"); Ts = k.sb([128, 16, BL], name="Ts"); magS = k.sb([128, 16])
    WB = k.sb([128, 16, 128], BF16); WBs = k.sb([128, 16, 128], BF16)
    CW = k.sb([128, 16, 128], BF16); CWs = k.sb([128, 16, 128], BF16)
    sw = k.sb([128, 128]); dk = k.sb([128, 1])
    k.push()
    ls = k.sb([128, 16, 3]); k.dma(ls[:], lamS[:, :, :])
    sg = k.sb([128, 1]); k.dma(sg[:], sgn[:, :])
    k.dma(dk[:], dsk[:, :])
    k.dma(sw[:], swapP[:, :])
    dtS = k.sb([128, 16]); thS = k.sb([128, 16])
    k.act(dtS[:], ls[:, :, 2], AF.Exp)
    k.tt(thS[:], ls[:, :, 1], dtS[:], ALU.mult)
    k.tt(magS[:], ls[:, :, 0], dtS[:], ALU.mult)
    k.act(magS[:], magS[:], AF.Exp)
    cosS, sinS = sincos(k, thS[:], [128, 16])
    t_a = k.sb([128, 16, BL // 2]); t_b = k.sb([128, 16, BL // 2])
    zc = k.sb([128, 16]); zs = k.sb([128, 16]); z1 = k.sb([128, 16]); z2 = k.sb([128, 16])
    k.copy(Tc[:, :, 0], cosS[:]); k.copy(Ts[:, :, 0], sinS[:])
    k.copy(zc[:], cosS[:]); k.copy(zs[:], sinS[:])
    m = 1
    while m < BL:
        zcb = View(zc, zc.t[:, :].unsqueeze(2).to_broadcast([128, 16, m]))
        zsb = View(zs, zs.t[:, :].unsqueeze(2).to_broadcast([128, 16, m]))
        k.tt(t_a[:, :, :m], Tc[:, :, 0:m], zcb, ALU.mult)
        k.tt(t_b[:, :, :m], Ts[:, :, 0:m], zsb, ALU.mult)
        k.tt(t_a[:, :, :m], t_a[:, :, :m], t_b[:, :, :m], ALU.subtract)
        k.tt(t_b[:, :, :m], Ts[:, :, 0:m], zcb, ALU.mult)
        k.copy(Tc[:, :, m:2 * m], t_a[:, :, :m])
        k.tt(t_a[:, :, :m], Tc[:, :, 0:m], zsb, ALU.mult)
        k.tt(Ts[:, :, m:2 * m], t_a[:, :, :m], t_b[:, :, :m], ALU.add)
        k.tt(z1[:], zc[:], zc[:], ALU.mult); k.tt(z2[:], zs[:], zs[:], ALU.mult)
        k.tt(zs[:], zc[:], zs[:], ALU.mult); k.ts(zs[:], zs[:], 2.0, ALU.mult)
        k.tt(zc[:], z1[:], z2[:], ALU.subtract)
        m *= 2
    Tsg = Ts
    k.ts(Tsg[:], Ts[:], sg[:, 0:1], ALU.mult)

    k.pop()
    k.push()
    lr_ = k.sb([128, 16, 64, 2]); k.dma(lr_[:], lamR[:, :, :, :])
    dR = k.sb([128, 16]); k.dma(dR[:], ldtR[:, :]); k.act(dR[:], dR[:], AF.Exp)
    dRb = View(dR, dR.t[:, :].unsqueeze(2).to_broadcast([128, 16, 64]))
    lrR = lr_[:, :, :, 0]; liR = lr_[:, :, :, 1]
    thR = k.sb([128, 16, 64]); mgR = k.sb([128, 16, 64])
    k.tt(thR[:], liR, dRb, ALU.mult)
    k.tt(mgR[:], lrR, dRb, ALU.mult)
    k.act(mgR[:], mgR[:], AF.Exp)
    cosR, sinR = sincos(k, thR[:], [128, 16, 64])
    are = cosR; aim = sinR
    k.tt(are[:], mgR[:], cosR[:], ALU.mult); k.tt(aim[:], mgR[:], sinR[:], ALU.mult)
    k.ts(are[:], are[:], -1.0, ALU.add)
    den = thR; tmp = mgR
    k.tt(den[:], lrR, lrR, ALU.mult); k.tt(tmp[:], liR, liR, ALU.mult); k.tt(den[:], den[:], tmp[:], ALU.add)
    k.recip(den[:], den[:])
    cre = k.sb([128, 16, 64]); cim = k.sb([128, 16, 64])
    k.tt(cre[:], are[:], lrR, ALU.mult); k.tt(tmp[:], aim[:], liR, ALU.mult); k.tt(cre[:], cre[:], tmp[:], ALU.add); k.tt(cre[:], cre[:], den[:], ALU.mult)
    k.tt(cim[:], aim[:], lrR, ALU.mult); k.tt(tmp[:], are[:], liR, ALU.mult); k.tt(cim[:], cim[:], tmp[:], ALU.subtract); k.tt(cim[:], cim[:], den[:], ALU.mult)
    br = k.sb([128, 16, 64]); bi = k.sb([128, 16, 64])
    k.dma(br[:], Bre[:, :, :]); k.dma(bi[:], Bim[:, :, :])
    WBf = k.sb([128, 16, 128])
    k.tt(WBf[:, :, 0:64], cre[:], br[:], ALU.mult); k.tt(tmp[:], cim[:], bi[:], ALU.mult); k.tt(WBf[:, :, 0:64], WBf[:, :, 0:64], tmp[:], ALU.subtract)
    k.tt(WBf[:, :, 64:128], cre[:], bi[:], ALU.mult); k.tt(tmp[:], cim[:], br[:], ALU.mult); k.tt(WBf[:, :, 64:128], WBf[:, :, 64:128], tmp[:], ALU.add)
    k.copy(WB[:], WBf[:])
    k.copy(WBs[:, :, 0:64], WBf[:, :, 64:128]); k.copy(WBs[:, :, 64:128], WBf[:, :, 0:64])
    cwf = k.sb([128, 16, 128]); cwsf = k.sb([128, 16, 128])
    k.dma(cwf[:], CW0[:, :, :]); k.dma(cwsf[:], CWs0[:, :, :])
    k.copy(CW[0:64], cwf[0:64]); k.act(CW[64:128], cwf[64:128], AF.Copy, scale=-1.0)
    k.act(CWs[0:64], cwsf[0:64], AF.Copy, scale=-1.0); k.copy(CWs[64:128], cwsf[64:128])

    k.pop()
    uf = k.sb([128, EXT], name="uf"); ub = k.sb([128, EXT], BF16, name="ub"); yacc = k.sb([128, EXT], name="yacc")
    for c in range(0, EXT, 2112):
        k.dma(uf[:, c:c + 2112], uT[:, c:c + 2112], chain=True)
    k.copy(ub[:], uf[:], eng="pool")
    k.ts(yacc[:], uf[:], dk[:, 0:1], ALU.mult)

    Gt = self_t = k.es.enter_context(k.nc.sbuf_tensor("Gbig", [128, 16, BL], F32))
    G = [Buf(Gt[:, g, :], f"G{g}") for g in range(16)]
    M1 = [k.sb([128, 8, BL], BF16) for _ in range(2)]; M2 = [k.sb([128, 8, BL], BF16) for _ in range(2)]
    Wt = [k.sb([128, BL]) for _ in range(4)]; T1 = [k.sb([128, BL]) for _ in range(4)]; T2 = [k.sb([128, BL]) for _ in range(4)]
    carry = [k.sb([128, 8]) for _ in range(2)]; gl = [k.sb([128, 8]) for _ in range(2)]
    c1 = [k.sb([128, 8]) for _ in range(2)]; c2 = [k.sb([128, 8]) for _ in range(2)]
    for d in range(2):
        k.memset(carry[d][:], 0.0)
    psSW = [k.ps([128, 2, BL]) for _ in range(4)]
    psY = [k.ps([128, BL]) for _ in range(2)]; psG = [k.ps([128, 8]) for _ in range(2)]
    it = 0
    pending = None
    for s in range(NB):
        for d in range(2):
            blk = s if d == 0 else (0 if s == 0 else NB - s)
            c0 = blk * BL
            rhs = ub[:, c0:c0 + BL]
            if d == 1:
                rhs = rhs[:, ::-1]
            for g8 in range(8):
                vg = d * 8 + g8
                it += 1
                pS, pW = psSW[it % 4][:, 0, :], psSW[it % 4][:, 1, :]
                k.mm(pS, WB[:, vg, :], rhs)
                k.mm(pW, WBs[:, vg, :], rhs)
                t1, t2, w = T1[it % 4], T2[it % 4], Wt[it % 4]
                k.tt(t1[:], pS, Tc[:, vg, :], ALU.mult)
                k.tt(t2[:], pW, Tsg[:, vg, :], ALU.mult)
                k.tt(w[:], t1[:], t2[:], ALU.add)
                k.scan(G[vg][:], View(magS, magS.t[:, vg:vg + 1].to_broadcast([128, BL])), w[:], carry[d][:, g8:g8 + 1])
                k.tt(M1[d][:, g8, :], G[vg][:], Tc[:, vg, :], ALU.mult, eng="pool")
                k.tt(M2[d][:, g8, :], G[vg][:], Tsg[:, vg, :], ALU.mult, eng="pool")
                k.copy(gl[d][:, g8:g8 + 1], G[vg][:, BL - 1:BL], eng="act")
            if pending is not None:
                pending()

            def pending(d=d, c0=c0):
                py = psY[d]
                for g8 in range(8):
                    vg = d * 8 + g8
                    k.mm(py[:], CW[:, vg, :], M1[d][:, g8, :], start=(g8 == 0), stop=False, inc=False)
                    k.mm(py[:], CWs[:, vg, :], M2[d][:, g8, :], start=False, stop=(g8 == 7), inc=(g8 == 7))
                src = py[:, :] if d == 0 else py[:, ::-1]
                k.tt(yacc[:, c0:c0 + BL], src, yacc[:, c0:c0 + BL], ALU.add)
                k.mm(psG[d][:], sw[:], gl[d][:])
                k.tt(c1[d][:], gl[d][:], Tc[:, 8 * d:8 * d + 8, BL - 1], ALU.mult)
                k.tt(c2[d][:], psG[d][:], Tsg[:, 8 * d:8 * d + 8, BL - 1], ALU.mult)
                k.tt(carry[d][:], c1[d][:], c2[d][:], ALU.subtract)
    pending()
    for c in range(0, EXT, 2112):
        k.dma(yT[:, c:c + 2112], yacc[:, c:c + 2112])
    return k.finish()


def s5_maps(uT_all, inp, l):
    maps = []
    swapP = np.zeros((128, 128), np.float32)
    for p in range(64):
        swapP[p, p + 64] = 1.0; swapP[p + 64, p] = 1.0
    sgn = np.ones((128, 1), np.float32); sgn[64:] = -1.0
    for core in range(8):
        b, ct = core // 2, core % 2
        gs = slice(8 * ct, 8 * ct + 8)
        fl = lambda a: np.asarray(a[l][:, gs]).reshape((16,) + a.shape[3:])
        lre, lim, ldt = fl(inp["s5_lam_re"]), fl(inp["s5_lam_im"]), fl(inp["s5_log_dt"])
        bre, bim, cre, cim = fl(inp["s5_b_re"]), fl(inp["s5_b_im"]), fl(inp["s5_c_re"]), fl(inp["s5_c_im"])
        lamS = np.zeros((128, 16, 3), np.float32)
        lamS[:64, :, 0] = lre.T; lamS[64:, :, 0] = lre.T; lamS[:64, :, 1] = lim.T; lamS[64:, :, 1] = lim.T; lamS[:, :, 2] = ldt[None, :]
        lamR = np.broadcast_to(np.stack([lre, lim], -1)[None], (128, 16, 64, 2)).astype(np.float32)
        ldtR = np.broadcast_to(ldt[None], (128, 16)).astype(np.float32)
        Bre = np.zeros((128, 16, 64), np.float32); Bim = np.zeros((128, 16, 64), np.float32)
        CW0 = np.zeros((128, 16, 128), np.float32); CWs0 = np.zeros((128, 16, 128), np.float32)
        for vg in range(16):
            r0 = 16 * (vg % 8)
            Bre[r0:r0 + 16, vg, :] = bre[vg].T; Bim[r0:r0 + 16, vg, :] = bim[vg].T
            CW0[:64, vg, r0:r0 + 16] = cre[vg].T; CW0[64:, vg, r0:r0 + 16] = cim[vg].T
            CWs0[:64, vg, r0:r0 + 16] = cim[vg].T; CWs0[64:, vg, r0:r0 + 16] = cre[vg].T
        maps.append({"uT": np.ascontiguousarray(uT_all[b, ct * 128:(ct + 1) * 128]), "lamS": lamS, "lamR": np.ascontiguousarray(lamR),
                     "ldtR": np.ascontiguousarray(ldtR), "Bre": Bre, "Bim": Bim, "CW0": CW0, "CWs0": CWs0,
                     "dsk": np.ascontiguousarray(inp["s5_d"][l][ct * 128:(ct + 1) * 128, None]), "swapP": swapP, "sgn": sgn})
    return maps


def run_S5(uT_all, inp, l):
    res = run(build_S5(), s5_maps(uT_all, inp, l))
    y = np.empty((B, 256, EXT), np.float32)
    for core in range(8):
        y[core // 2, (core % 2) * 128:(core % 2 + 1) * 128] = res[core]["yT"]
    return y


GBLK = [(0, 256)] + [(256 + 512 * i, 512) for i in range(16)]


@functools.lru_cache(None)
def build_GLA(DBG=99):
    k = K()
    qT = k.din("qT", [128, EXT]); kT = k.din("kT", [128, EXT]); gaT = k.din("gaT", [32, EXT]); v = k.din("v", [EXT, 256], BF16)
    wa2 = k.din("wa2", [16, 2, 128]); nba = k.din("nba", [128, 2]); maskd = k.din("mask", [64, 2, 512]); rmaskd = k.din("rmask", [128, 512])
    ident = k.din("ident", [128, 128])
    outs = [k.dout("oTf", [256, EXT]), k.dout("oTb", [256, EXT])]
    wa = k.sb([16, 2, 128]); nb_ = k.sb([128, 2]); mk = k.sb([64, 2, 512]); rm = k.sb([128, 512]); idf = k.sb([128, 128])
    k.dma(wa[:], wa2[:, :, :]); k.dma(nb_[:], nba[:, :]); k.dma(mk[:], maskd[:, :, :]); k.dma(rm[:], rmaskd[:, :]); k.dma(idf[:], ident[:, :])
    R2 = range(2)
    qf = [k.sb([128, 512]) for _ in R2]; kf = [k.sb([128, 512]) for _ in R2]; ga = [k.sb([16, 512]) for _ in R2]
    vb = [k.sb([64, 8, 256], BF16) for _ in R2]
    e1 = [k.sb([128, 512]) for _ in R2]; bp = [k.sb([128, 512]) for _ in R2]; eb = [k.sb([128, 512]) for _ in R2]; enb = [k.sb([128, 512]) for _ in R2]
    qt = [k.sb([128, 512]) for _ in R2]; kt = [k.sb([128, 512]) for _ in R2]; kend = [k.sb([128, 8, 64]) for _ in R2]
    dec = [k.sb([128, 8]) for _ in R2]; ktok = [k.sb([64, 8, 128], BF16) for _ in R2]
    att = [[k.sb([64, 8, 64], BF16) for _ in R2] for _ in R2]
    S = [k.sb([128, 9, 128]) for _ in R2]
    osb = [k.sb([128, 512]) for _ in range(4)]
    for d in R2:
        k.memset(S[d][:, 0, :], 0.0)
    z_ps = k.ps([128, 512]); tr_ps = [k.ps([64, 4, 128])] * 2; att_ps = [k.ps([64, 8, 64])] * 2
    otmp = k.sb([128, 512])
    kvb = k.ps([128, 4, 128])
    o_ps = [k.ps([128, 8, 64]) for _ in R2]; oi_ps = [k.ps([128, 8, 64]) for _ in R2]
    it = 0
    for s in range(17 if DBG == 99 else 1):
        for d in R2:
            bi = s if d == 0 else (0 if s == 0 else 17 - s)
            c0, n = GBLK[bi]
            nch = n // 64
            r = it % 2
            it += 1
            k.dma(qf[r][:, :n], qT[:, c0:c0 + n]); k.dma(kf[r][:, :n], kT[:, c0:c0 + n])
            k.dma(ga[r][:, :n], gaT[16 * d:16 * d + 16, c0:c0 + n])
            k.dma(vb[r][:, :nch, :], v[c0:c0 + n, :].re("(c j) e -> j c e", j=64))
            k.mm(z_ps[:, :n], wa[:, d, :], ga[r][:, :n])
            k.act(e1[r][:, :n], z_ps[:, :n], AF.Exp, bias=nb_[:, d:d + 1], scale=-1.0)
            k.act(e1[r][:, :n], e1[r][:, :n], AF.Ln, bias=1.0)
            if d == 0:
                k.scan(bp[r][:, :n], rm[:, :n], e1[r][:, :n], 0.0)
            else:
                k.scan(bp[r][:, :n][:, ::-1], rm[:, :n], e1[r][:, :n][:, ::-1], 0.0)
            if DBG < 2:
                continue
            k.act(eb[r][:, :n], bp[r][:, :n], AF.Exp, scale=-1.0 / 16)
            k.act(enb[r][:, :n], bp[r][:, :n], AF.Exp, scale=1.0 / 16)
            k.tt(qt[r][:, :n], qf[r][:, :n], eb[r][:, :n], ALU.mult)
            k.tt(kt[r][:, :n], kf[r][:, :n], enb[r][:, :n], ALU.mult, eng="pool")
            ebv = eb[r][:, :n].re("p (c j) -> p c j", j=64)
            k.copy(dec[r][:, :nch], ebv[:, :, 63] if d == 0 else ebv[:, :, 0])
            decb = View(dec[r], dec[r].t[:, :nch].unsqueeze(2).to_broadcast([128, nch, 64]))
            k.tt(kend[r][:, :nch, :], kt[r][:, :n].re("p (c j) -> p c j", j=64), decb, ALU.mult)
            for c in range(nch):
                tp = tr_ps[(c // 4) % 2]
                k.tr(tp[:, c % 4, :], kend[r][:, c, :], idf[:], inc=(c % 4 == 3))
                if c % 4 == 3:
                    k.copy(ktok[r][:, c - 3:c + 1, :], tp[:], eng="act")
            if DBG < 3:
                continue
            for h2 in R2:
                hs = slice(64 * h2, 64 * h2 + 64)
                for c in range(nch):
                    k.mm(att_ps[h2][:, c, :], kt[r][hs, c * 64:(c + 1) * 64], qt[r][hs, c * 64:(c + 1) * 64], inc=(c == nch - 1))
                k.tt(att[r][h2][:, :nch, :], att_ps[h2][:, :nch, :], mk[:, d, :n].re("p (c i) -> p c i", i=64), ALU.mult)
            if DBG < 4:
                continue
            order = list(range(nch)) if d == 0 else list(range(nch - 1, -1, -1))
            for g0 in range(0, nch, 4):
                steps = list(range(g0, min(g0 + 4, nch)))
                for step in steps:
                    c = order[step]
                    for h2 in R2:
                        k.mm(kvb[64 * h2:64 * h2 + 64, step % 4, :], ktok[r][:, c, 64 * h2:64 * h2 + 64], vb[r][:, c, 128 * h2:128 * h2 + 128],
                             inc=(h2 == 1 and step == steps[-1]))
                for step in steps:
                    c = order[step]
                    k.stt(S[d][:, step + 1, :], S[d][:, step, :], dec[r][:, c:c + 1], kvb[:, step % 4, :], ALU.mult, ALU.add)
            if DBG < 5:
                continue
            for h2 in R2:
                hs = slice(64 * h2, 64 * h2 + 64)
                for step, c in enumerate(order):
                    k.mm(o_ps[h2][:, c, :], vb[r][:, c, 128 * h2:128 * h2 + 128], att[r][h2][:, c, :], inc=(step == nch - 1))
                if DBG < 6:
                    continue
                for step, c in enumerate(order):
                    k.mm(oi_ps[h2][:, c, :], S[d][hs, step, :], qt[r][hs, c * 64:(c + 1) * 64], inc=(step == nch - 1))
                if DBG < 7:
                    continue
                ob = osb[(2 * it + h2) % 4]
                k.copy(otmp[:, :n], oi_ps[h2][:, :nch, :].re("p c i -> p (c i)"), eng="act")
                k.tt(ob[:, :n], o_ps[h2][:, :nch, :].re("p c i -> p (c i)"), otmp[:, :n], ALU.add)
                k.dma(outs[d][128 * h2:128 * h2 + 128, c0:c0 + n], ob[:, :n])
            if DBG < 8:
                continue
            k.copy(S[d][:, 0, :], S[d][:, nch, :])
    return k.finish()


def run_GLA(gq, gk, ga, gv, inp, l):
    i_ = np.arange(64)
    mf = (i_[None, :] >= i_[:, None]).astype(np.float32)
    mask = np.stack([np.tile(mf, (1, 8)), np.tile(mf.T, (1, 8))], axis=1)
    rmask = np.ones((128, 512), np.float32); rmask[:, 0::64] = 0.0
    ident = np.eye(128, dtype=np.float32)
    maps = []
    for core in range(8):
        b, hp = core // 2, core % 2
        cs = slice(128 * hp, 128 * hp + 128)
        wa2 = np.ascontiguousarray(inp["gla_w_a2"][l][:, :, cs].transpose(1, 0, 2))
        nba = np.ascontiguousarray((inp["gla_b_a"][l][:, cs]).T) * np.float32(-1.0)
        maps.append({"qT": np.ascontiguousarray(gq[b, cs]), "kT": np.ascontiguousarray(gk[b, cs]), "gaT": np.ascontiguousarray(ga[b]),
                     "v": np.ascontiguousarray(gv[b][:, 256 * hp:256 * hp + 256]), "wa2": wa2, "nba": nba, "mask": mask, "rmask": rmask, "ident": ident})
    res = run(build_GLA(), maps)
    of = np.empty((B, 512, EXT), np.float32); ob = np.empty((B, 512, EXT), np.float32)
    for core in range(8):
        b, hp = core // 2, core % 2
        of[b, 256 * hp:256 * hp + 256] = res[core]["oTf"]; ob[b, 256 * hp:256 * hp + 256] = res[core]["oTb"]
    return of, ob


@functools.lru_cache(None)
def build_NA():
    k = K()
    qT = k.din("qT", [128, EXT], BF16); kT = k.din("kT", [128, EXT], BF16); v = k.din("v", [EXT, 128], BF16)
    rpbg = k.din("rpbg", [128, 28, 64]); maskd = k.din("mask", [128, 64])
    out = k.dout("naT", [128, EXT], BF16)
    q_sb = k.sb([64, 2, EXT], BF16, "na_q"); k_sb = k.sb([64, 2, EXT], BF16, "na_k")
    v_ev = k.sb([128, 66, 128], BF16, "na_ve"); v_od = k.sb([128, 65, 128], BF16, "na_vo")
    o_sb = k.sb([128, EXT], BF16, "na_o")
    for h in range(2):
        k.dma(q_sb[:, h, :], qT[64 * h:64 * h + 64, :]); k.dma(k_sb[:, h, :], kT[64 * h:64 * h + 64, :])
    vv_e = v[:, :].re("(t p) e -> p t e", p=128); vv_o = v[64:64 + 65 * 128, :].re("(t p) e -> p t e", p=128)
    for t0 in range(0, 66, 22):
        k.dma(v_ev[:, t0:t0 + 22, :], vv_e[:, t0:t0 + 22, :], chain=True)
    for t0 in range(0, 65, 13):
        k.dma(v_od[:, t0:t0 + 13, :], vv_o[:, t0:t0 + 13, :], chain=True)
    rb = k.sb([128, 28, 64]); mk = k.sb([128, 64]); E = k.sb([128, 28, 64], BF16); ones = k.sb([128, 64], BF16)
    k.dma(rb[:], rpbg[:, :, :]); k.dma(mk[:], maskd[:, :])
    k.act(rb[:], rb[:], AF.Exp)
    k.tt(E[:], rb[:], View(mk, mk.t[:, :].unsqueeze(1).to_broadcast([128, 28, 64])), ALU.mult)
    k.memset(ones[:], 1.0)
    stl = [k.ps([128, 8, 64]) for _ in range(2)]; stc = [k.ps([128, 8, 64]) for _ in range(2)]; po = [k.ps([128, 8, 64]) for _ in range(2)]
    pt = [k.sb([128, 6, 64], BF16) for _ in range(2)]; pe_ = [k.sb([128, 4, 64], BF16) for _ in range(2)]
    rden = [k.sb([128, 64]) for _ in range(2)]
    it = 0
    rows = [("c", j) for j in range(4)] + [("l", r) for r in range(128)]
    for ri, (kind, r) in enumerate(rows):
        q0 = 64 * r if kind == "c" else LC + 64 * r
        p_o = po[ri % 2]
        for h in range(2):
            it += 1
            sl, sc, p_t, p_e = stl[it % 2], stc[it % 2], pt[it % 2], pe_[it % 2]
            qv = q_sb[:, h, q0:q0 + 64]
            for j in range(2):
                k.mm(sc[:, j, :], k_sb[:, h, 128 * j:128 * j + 128], qv, inc=(j == 1))
            k.act(p_t[:, 0:2, :], sc[:, 0:2, :], AF.Exp)
            tiles = [(v_ev, 0), (v_ev, 1)]
            if kind == "l":
                kr0 = min(max(r - 4, 0), 120)
                dr0 = kr0 - r + 7
                for j in range(4):
                    kt = LC + 64 * (kr0 + 2 * j)
                    k.mm(sl[:, j, :], k_sb[:, h, kt:kt + 128], qv, inc=(j == 3))
                    tiles.append((v_ev, kt // 128) if kt % 128 == 0 else (v_od, (kt - 64) // 128))
                k.act(p_e[:], sl[:, 0:4, :], AF.Exp)
                k.tt(p_t[:, 2:6, :], p_e[:], E[:, 14 * h + dr0:14 * h + dr0 + 7:2, :], ALU.mult, eng=("pool" if it % 2 else "dve"))
            n = len(tiles)
            ov = p_o[64 * h:64 * h + 64, 0, :]; dv = p_o[64 * h:64 * h + 64, 1, :]
            for i, (vb_, vt) in enumerate(tiles):
                k.mm(ov, vb_[:, vt, 64 * h:64 * h + 64], p_t[:, i, :], start=(i == 0), stop=(i == n - 1), inc=(i == n - 1))
            for i in range(n):
                k.mm(dv, ones[:], p_t[:, i, :], start=(i == 0), stop=(i == n - 1), inc=(i == n - 1))
        rd = rden[ri % 2]
        k.recip(rd[:], p_o[:, 1, :])
        k.tt(o_sb[:, q0:q0 + 64], p_o[:, 0, :], rd[:], ALU.mult)
    for c in range(0, EXT, 2112):
        k.dma(out[:, c:c + 2112], o_sb[:, c:c + 2112])
    return k.finish()


def na_tables(rpb_l):
    c = np.arange(64)
    dc = np.clip(c[:, None] - c[None, :] + 15, 0, 30)
    cs = np.clip(c - 8, 0, 48)
    mask = ((c[:, None] >= cs[None, :]) & (c[:, None] < cs[None, :] + 16)).astype(np.float32)
    g = np.asarray(rpb_l)[:, :, dc].transpose(2, 0, 1, 3)
    g2 = np.concatenate([g[:, :, 0:14], g[:, :, 1:15]], axis=0)
    return np.ascontiguousarray(g2), np.ascontiguousarray(np.tile(mask, (2, 1)))


def run_NA(nq, nk, nv, inp, l):
    g, mask = na_tables(inp["na_rpb"][l])
    maps = []
    for core in range(8):
        b, hp = core // 2, core % 2
        cs = slice(128 * hp, 128 * hp + 128)
        maps.append({"qT": np.ascontiguousarray(nq[b, cs]), "kT": np.ascontiguousarray(nk[b, cs]), "v": np.ascontiguousarray(nv[b][:, cs]),
                     "rpbg": np.ascontiguousarray(g[:, 2 * hp:2 * hp + 2].reshape(128, 28, 64)), "mask": mask})
    res = run(build_NA(), maps)
    o = np.empty((B, 256, EXT), NPBF)
    for core in range(8):
        o[core // 2, 128 * (core % 2):128 * (core % 2) + 128] = res[core]["naT"]
    return o


@functools.lru_cache(None)
def build_CV():
    k = K()
    cy = k.din("cy", [128, EXT], BF16); dw = k.din("dw", [128, 31]); dwb = k.din("dwb", [128, 1])
    out = k.dout("cvT", [128, EXT])
    W = 15 + LC + 30 + L + 15
    yp = k.sb([128, W], BF16, "cv_yp"); wt = k.sb([128, 31]); bt = k.sb([128, 1])
    k.dma(wt[:], dw[:, :]); k.dma(bt[:], dwb[:, :])
    k.memset(yp[:, 0:15], 0.0); k.memset(yp[:, 271:301], 0.0); k.memset(yp[:, 301 + L:W], 0.0)
    k.dma(yp[:, 15:271], cy[:, 0:LC]); k.dma(yp[:, 301:301 + L], cy[:, LC:EXT], chain=True)
    chunks = [(0, 0, LC)] + [(LC + c, 286 + c, 2048) for c in range(0, L, 2048)]
    for (o0, i0, n) in chunks:
        acc = k.sb([128, n])
        k.ts(acc[:], yp[:, i0:i0 + n], wt[:, 0:1], ALU.mult, bt[:, 0:1], ALU.add)
        for j in range(1, 31):
            k.stt(acc[:], yp[:, i0 + j:i0 + j + n], wt[:, j:j + 1], acc[:], ALU.mult, ALU.add)
        k.dma(out[:, o0:o0 + n], acc[:])
    return k.finish()


def run_CV(cy, inp, l):
    maps = []
    for core in range(8):
        b, ct = core // 2, core % 2
        cs = slice(128 * ct, 128 * ct + 128)
        maps.append({"cy": np.ascontiguousarray(cy[b, cs]), "dw": np.ascontiguousarray(inp["conv_dw"][l][:, cs].T),
                     "dwb": np.ascontiguousarray(inp["conv_dw_b"][l][cs, None])})
    res = run(build_CV(), maps)
    o = np.empty((B, 256, EXT), np.float32)
    for core in range(8):
        o[core // 2, 128 * (core % 2):128 * (core % 2) + 128] = res[core]["cvT"]
    return o


OBLK = [(i * 256, 256) for i in range(16)] + [(4096, 128)]


def fmv(dram, c0, n):
    return dram[:, c0:c0 + n].re("(t p) c -> p t c", p=128)


@functools.lru_cache(None)
def build_O1():
    k = K()
    xa = k.din("xa", [NLOC, D]); g1b = k.din("g1b", [128, 2, D])
    s5y = k.din("s5y", [256, NLOC]); glf = k.din("glf", [512, NLOC]); glb = k.din("glb", [512, NLOC])
    grs = k.din("grs", [512, NLOC], BF16); na = k.din("na", [256, NLOC], BF16); cv = k.din("cv", [256, NLOC])
    gs = k.din("gs", [4096, NLOC], BF16)
    w_glu = k.din("w_glu", [256, 256]); b_glu = k.din("b_glu", [128, 2]); w_s5o = k.din("w_s5o", [256, D])
    gng = k.din("gng", [128, 1]); w_glo = k.din("w_glo", [512, D]); w_nao = k.din("w_nao", [256, D])
    lng = k.din("lng", [128, 2]); lnb = k.din("lnb", [128, 2]); w_cvo = k.din("w_cvo", [256, D]); w_mix = k.din("w_mix", [D, D])
    x1 = k.dout("x1", [NLOC, D])

    stage = [k.sb([128, 1024]) for _ in range(2)]
    wi = [0]

    def load_cast(w_dram, kt, ncols, name):
        wb = k.sb([128, kt, ncols], BF16, name)
        for kk in range(kt):
            wi[0] += 1
            st = stage[wi[0] % 2]
            k.dma(st[:, :ncols], w_dram[kk * 128:(kk + 1) * 128, :])
            k.copy(wb[:, kk, :], st[:, :ncols], eng=("act" if wi[0] % 2 else "dve"))
        return wb

    wglu = load_cast(w_glu, 2, 256, "wglu"); ws5o = load_cast(w_s5o, 2, D, "ws5o"); wglo = load_cast(w_glo, 4, D, "wglo")
    wnao = load_cast(w_nao, 2, D, "wnao"); wcvo = load_cast(w_cvo, 2, D, "wcvo"); wmix = load_cast(w_mix, 8, D, "wmix")
    bglu = k.sb([128, 2]); gn = k.sb([128, 1]); lg = k.sb([128, 2]); lb = k.sb([128, 2]); g1t = k.sb([128, 2, D], name="g1t")
    k.dma(bglu[:], b_glu[:, :]); k.dma(gn[:], gng[:, :]); k.dma(lg[:], lng[:, :]); k.dma(lb[:], lnb[:, :]); k.dma(g1t[:], g1b[:, :, :])
    onesb = k.sb([128, 128], BF16); k.memset(onesb[:], 1.0)

    pss = [k.ps([128, 512]) for _ in range(8)]
    psi = [0]

    def nps():
        psi[0] += 1
        return pss[psi[0] % 8]

    R2 = range(2)
    NB = 256
    yt = [k.sb([128, 2, NB]) for _ in R2]; of_ = [k.sb([128, 4, NB]) for _ in R2]; ob_ = [k.sb([128, 4, NB]) for _ in R2]
    gr = [k.sb([128, 4, NB], BF16) for _ in R2]; cvt = [k.sb([128, 2, NB]) for _ in R2]; nat = [k.sb([128, 2, NB], BF16) for _ in R2]
    gst = k.sb([128, 32, NB], BF16, "gst")
    z = k.sb([128, 2, NB]); zb = k.sb([128, 2, NB], BF16); z2b = k.sb([128, 2, NB], BF16); sg = k.sb([128, NB])
    o_ = k.sb([128, 4, NB]); osq = k.sb([128, 4, NB], BF16); rs = k.sb([128, NB]); of2 = k.sb([128, 4, NB], BF16)
    cvb = k.sb([128, 2, NB], BF16); cvq = k.sb([128, 2, NB], BF16); mean = k.sb([128, NB]); msq = k.sb([128, NB]); var = k.sb([128, NB])
    dd = k.sb([128, NB]); cvo = k.sb([128, 2, NB], BF16); tq = [k.sb([128, NB]) for _ in range(4)]; mg = k.sb([128, 8, NB], BF16)
    xts = [k.sb([128, D]) for _ in R2]; xos = [k.sb([128, D]) for _ in R2]
    ti = 0
    for bi, (c0, n) in enumerate(OBLK):
        s = bi % 2
        k.dma(yt[s][:, :, :n], fmv(s5y, c0, n)); k.dma(of_[s][:, :, :n], fmv(glf, c0, n)); k.dma(ob_[s][:, :, :n], fmv(glb, c0, n))
        k.dma(gr[s][:, :, :n], fmv(grs, c0, n)); k.dma(cvt[s][:, :, :n], fmv(cv, c0, n)); k.dma(nat[s][:, :, :n], fmv(na, c0, n))
        k.dma(gst[:, :, :n], fmv(gs, c0, n))
        k.act(z[:, :, :n], yt[s][:, :, :n], AF.Gelu_apprx_tanh)
        k.copy(zb[:, :, :n], z[:, :, :n], eng="pool")
        for jt in R2:
            p = nps()
            for it_ in R2:
                k.mm(p[:, :n], wglu[:, it_, jt * 128:(jt + 1) * 128], zb[:, it_, :n], start=(it_ == 0), stop=(it_ == 1), inc=(it_ == 1))
            k.act(sg[:, :n], p[:, :n], AF.Sigmoid, bias=bglu[:, jt:jt + 1])
            k.tt(z2b[:, jt, :n], z[:, jt, :n], sg[:, :n], ALU.mult)
        k.tt(o_[:, :, :n], of_[s][:, :, :n], ob_[s][:, :, :n], ALU.add, eng="pool")
        k.tt(osq[:, :, :n], o_[:, :, :n], o_[:, :, :n], ALU.mult)
        for hh in range(4):
            p = nps()
            k.mm(p[:, :n], onesb[:], osq[:, hh, :n])
            k.ts(rs[:, :n], p[:, :n], 1.0 / 128, ALU.mult, EPS, ALU.add)
            k.act(rs[:, :n], rs[:, :n], AF.Sqrt)
            k.recip(rs[:, :n], rs[:, :n])
            k.tt(o_[:, hh, :n], o_[:, hh, :n], rs[:, :n], ALU.mult)
            k.stt(of2[:, hh, :n], o_[:, hh, :n], gn[:, 0:1], gr[s][:, hh, :n], ALU.mult, ALU.mult)
        k.copy(cvb[:, :, :n], cvt[s][:, :, :n], eng="pool")
        k.tt(cvq[:, :, :n], cvt[s][:, :, :n], cvt[s][:, :, :n], ALU.mult)
        p1 = nps(); p2 = nps()
        for t in R2:
            k.mm(p1[:, :n], onesb[:], cvb[:, t, :n], start=(t == 0), stop=(t == 1), inc=(t == 1))
        for t in R2:
            k.mm(p2[:, :n], onesb[:], cvq[:, t, :n], start=(t == 0), stop=(t == 1), inc=(t == 1))
        k.ts(mean[:, :n], p1[:, :n], 1.0 / 256, ALU.mult)
        k.tt(msq[:, :n], mean[:, :n], mean[:, :n], ALU.mult)
        k.stt(var[:, :n], p2[:, :n], 1.0 / 256, msq[:, :n], ALU.mult, ALU.subtract)
        k.ts(var[:, :n], var[:, :n], EPS, ALU.add)
        k.act(var[:, :n], var[:, :n], AF.Sqrt)
        k.recip(var[:, :n], var[:, :n])
        for t in R2:
            k.tt(dd[:, :n], cvt[s][:, t, :n], mean[:, :n], ALU.subtract)
            k.tt(dd[:, :n], dd[:, :n], var[:, :n], ALU.mult)
            k.act(cvo[:, t, :n], dd[:, :n], AF.Silu, scale=lg[:, t:t + 1], bias=lb[:, t:t + 1])
        srcs = [(ws5o, z2b, 2), (wglo, of2, 4), (wnao, nat[s], 2), (wcvo, cvo, 2)]
        for m in range(8):
            ps4 = [nps() for _ in range(4)]
            for i, (w_, a_, kt) in enumerate(srcs):
                for t in range(kt):
                    k.mm(ps4[i][:, :n], w_[:, t, m * 128:(m + 1) * 128], a_[:, t, :n], start=(t == 0), stop=(t == kt - 1), inc=(t == kt - 1))
            for i in range(4):
                k.tt(tq[i][:, :n], ps4[i][:, :n], gst[:, 8 * i + m, :n], ALU.mult)
            k.tt(tq[0][:, :n], tq[0][:, :n], tq[1][:, :n], ALU.add, eng="pool")
            k.tt(tq[2][:, :n], tq[2][:, :n], tq[3][:, :n], ALU.add, eng="pool")
            k.tt(mg[:, m, :n], tq[0][:, :n], tq[2][:, :n], ALU.add, eng="pool")
        for tt_ in range(n // 128):
            ti += 1
            tok0 = c0 + tt_ * 128
            xt, xo = xts[ti % 2], xos[ti % 2]
            k.dma(xt[:], xa[tok0:tok0 + 128, :])
            j = 1 if tok0 >= 4096 else 0
            for hf in R2:
                p = nps()
                for ft in range(8):
                    k.mm(p[:, :], mg[:, ft, tt_ * 128:(tt_ + 1) * 128], wmix[:, ft, hf * 512:(hf + 1) * 512], start=(ft == 0), stop=(ft == 7), inc=(ft == 7))
                k.tt(xo[:, hf * 512:(hf + 1) * 512], p[:, :], g1t[:, j, hf * 512:(hf + 1) * 512], ALU.mult)
            k.tt(xo[:], xo[:], xt[:], ALU.add, eng="pool")
            k.dma(x1[tok0:tok0 + 128, :], xo[:])
    return k.finish()


def scatter_fm(a, core):
    b, h = core // 2, core % 2
    return np.ascontiguousarray(np.concatenate([a[b][:, LC + h * 4096:LC + (h + 1) * 4096], a[b][:, h * 128:(h + 1) * 128]], 1))


def bc128(v):
    return np.broadcast_to(np.asarray(v, np.float32)[None], (128,) + np.asarray(v).shape)


def run_O1(xs, resA, y5, glf, glb, nao, cvo, mod_l, inp, l, cores=range(8)):
    maps = []
    for core in cores:
        b = core // 2
        g1b = np.ascontiguousarray(np.stack([bc128(mod_l[2, :, b]), bc128(mod_l[2, :, 4])], axis=1))
        maps.append({"xa": xs[core], "g1b": g1b, "s5y": scatter_fm(y5, core), "glf": scatter_fm(glf, core), "glb": scatter_fm(glb, core),
                     "grs": resA[core]["grsT"], "na": scatter_fm(nao, core), "cv": scatter_fm(cvo, core), "gs": resA[core]["gsT"],
                     "w_glu": inp["s5_w_glu"][l], "b_glu": np.ascontiguousarray(inp["s5_b_glu"][l].reshape(2, 128).T), "w_s5o": inp["s5_w_out"][l],
                     "gng": np.ascontiguousarray(inp["gla_norm_g"][l][:, None]), "w_glo": inp["gla_w_out"][l], "w_nao": inp["na_w_out"][l],
                     "lng": np.ascontiguousarray(inp["conv_ln_g"][l].reshape(2, 128).T), "lnb": np.ascontiguousarray(inp["conv_ln_b"][l].reshape(2, 128).T),
                     "w_cvo": inp["conv_w_out"][l], "w_mix": inp["w_mix_out"][l]})
    res = run(build_O1(), maps)
    return [r["x1"] for r in res]


O2PASS = [(0, 9), (9, 8), (17, 8), (25, 8)]


@functools.lru_cache(None)
def build_O2(last):
    k = K()
    x1 = k.din("x1", [NLOC, D]); modv = k.din("modv", [128, 8, 4]); g2n = k.din("g2n", [128, 8])
    g2b = k.din("g2b", [128, 2, D]); fgb = k.din("fgb", [128, D])
    wr = k.din("wr", [D, 36]); brb = k.din("brb", [128, 36])
    w1 = k.din("w1", [32, D, 256]); w3 = k.din("w3", [32, D, 256]); w2 = k.din("w2", [32, 256, D])
    ident = k.din("ident", [128, 128])
    out = k.dout("x2", [NLOC, D])

    idf = k.sb([128, 128]); k.dma(idf[:], ident[:, :])
    A_lat, B_lat, A_ctx, B_ctx = mod_prep(k, modv, g2n)
    wrt = k.sb([128, 8, 36]); brt = k.sb([128, 36]); g2t = k.sb([128, 2, D], name="g2t"); fgt = k.sb([128, D], name="fgt")
    k.dma(wrt[:], wr[:, :].re("(k p) c -> p k c", p=128)); k.dma(brt[:], brb[:, :]); k.dma(g2t[:], g2b[:, :, :]); k.dma(fgt[:], fgb[:, :])
    pss = [k.ps([128, 512]) for _ in range(8)]
    psi = [0]

    def nps():
        psi[0] += 1
        return pss[psi[0] % 8]

    R2 = range(2)
    MT = 9
    hT = k.sb([128, 8, MT * 128], BF16, "o2_hT"); yacc = k.sb([128, MT, D], name="o2_yacc"); comb = k.sb([128, 33, 32], name="o2_comb")
    st1 = k.sb([128, 8, 256], name="st1"); st3 = k.sb([128, 8, 256], name="st3"); st2 = k.sb([128, 2, D], name="st2")
    w1b = [k.sb([128, 8, 256], BF16) for _ in R2]; w3b = [k.sb([128, 8, 256], BF16) for _ in R2]; w2b = [k.sb([128, 2, D], BF16) for _ in R2]
    sa = [k.sb([128, 512]) for _ in R2]; actb = [[k.sb([128, 512], BF16) for _ in R2] for _ in R2]
    xt = [k.sb([128, D]) for _ in R2]; xn = k.sb([128, D]); junk = k.sb([128, D], BF16); tmpf = k.sb([128, 4, 128]); h32 = k.sb([128, 8, 128])
    yo = k.sb([128, D]); yo2 = k.sb([128, D])
    ss = k.sb([128, 1]); rr = k.sb([128, 1])
    lg = k.sb([128, 36]); gm = k.sb([128, 1]); ngm = k.sb([128, 1]); eg = k.sb([128, 4]); sgm = k.sb([128, 1]); gp = k.sb([128, 1])
    ohg = k.sb([128, 4]); t48 = k.sb([128, 4, 8]); sel = k.sb([128, 8]); sel2 = k.sb([128, 8]); m1 = k.sb([128, 1]); m2 = k.sb([128, 1])
    oh1 = k.sb([128, 8]); oh2 = k.sb([128, 8]); d21 = k.sb([128, 1]); e21 = k.sb([128, 1]); den = k.sb([128, 1]); wa = k.sb([128, 1]); wb_ = k.sb([128, 1])
    sw = k.sb([128, 8])

    def rms(src):
        k.act(junk[:], src, AF.Square, accum=ss[:])
        k.ts(rr[:], ss[:], 1.0 / D, ALU.mult, EPS, ALU.add)
        k.act(rr[:], rr[:], AF.Sqrt)
        k.recip(rr[:], rr[:])

    xi = 0
    for (p0, nt) in O2PASS:
        for li in range(nt):
            gi = p0 + li
            xi += 1
            x_ = xt[xi % 2]
            k.dma(x_[:], x1[gi * 128:(gi + 1) * 128, :])
            rms(x_[:])
            k.ts(xn[:], x_[:], rr[:, 0:1], ALU.mult)
            A_, B_ = (A_ctx, B_ctx) if gi == 32 else (A_lat, B_lat)
            for half in R2:
                tp = nps()
                for kk in range(4):
                    k.tr(tp[:, kk * 128:(kk + 1) * 128], xn[:, (half * 4 + kk) * 128:(half * 4 + kk + 1) * 128], idf[:], inc=(kk == 3))
                k.tt(tmpf[:], tp[:, :].re("p (k t) -> p k t", t=128),
                     View(A_, A_.t[:, half * 4:half * 4 + 4].unsqueeze(2).to_broadcast([128, 4, 128])), ALU.mult)
                k.tt(h32[:, half * 4:half * 4 + 4, :], tmpf[:],
                     View(B_, B_.t[:, half * 4:half * 4 + 4].unsqueeze(2).to_broadcast([128, 4, 128])), ALU.add)
            k.copy(hT[:, :, li * 128:(li + 1) * 128], h32[:], eng="pool")
            pr = nps()
            for kk in range(8):
                k.mm(pr[:, 0:36], h32[:, kk, :], wrt[:, kk, :], start=(kk == 0), stop=(kk == 7), inc=(kk == 7))
            k.tt(lg[:], pr[:, 0:36], brt[:], ALU.add)
            k.reduce(gm[:], lg[:, 0:4], ALU.max)
            k.ts(ngm[:], gm[:], -1.0, ALU.mult)
            k.act(eg[:], lg[:, 0:4], AF.Exp, bias=ngm[:, 0:1], accum=sgm[:])
            k.recip(gp[:], sgm[:])
            k.ts(ohg[:], lg[:, 0:4], gm[:, 0:1], ALU.is_equal)
            ohb = View(ohg, ohg.t[:, :].unsqueeze(2).to_broadcast([128, 4, 8]))
            k.tt(t48[:], lg[:, 4:36].re("p (g e) -> p g e", e=8), ohb, ALU.mult)
            k.reduce(sel[:], t48[:, :, :].re("p g e -> p e g"), ALU.add)
            k.reduce(m1[:], sel[:], ALU.max)
            k.ts(oh1[:], sel[:], m1[:, 0:1], ALU.is_equal)
            k.stt(sel2[:], oh1[:], -1.0e30, sel[:], ALU.mult, ALU.add)
            k.reduce(m2[:], sel2[:], ALU.max)
            k.ts(oh2[:], sel2[:], m2[:, 0:1], ALU.is_equal)
            k.tt(d21[:], m2[:], m1[:], ALU.subtract)
            k.act(e21[:], d21[:], AF.Exp)
            k.ts(den[:], e21[:], 1.0, ALU.add)
            k.recip(den[:], den[:])
            k.tt(wa[:], den[:], gp[:], ALU.mult)
            k.tt(wb_[:], wa[:], e21[:], ALU.mult)
            k.ts(sw[:], oh1[:], wa[:, 0:1], ALU.mult)
            k.stt(sw[:], oh2[:], wb_[:, 0:1], sw[:], ALU.mult, ALU.add)
            k.tt(comb[:, gi, :].re("p (g e) -> p g e", e=8), ohb, View(sw, sw.t[:, :].unsqueeze(1).to_broadcast([128, 4, 8])), ALU.mult)
        blocks = [(t0, min(4, nt - t0)) for t0 in range(0, nt, 4)]
        bi = 0
        pend = None
        for e in range(32):
            ws = e % 2
            k.dma(st1[:], w1[e].re("(k p) f -> p k f", p=128)); k.dma(st3[:], w3[e].re("(k p) f -> p k f", p=128))
            k.dma(st2[:], w2[e].re("(k p) f -> p k f", p=128))
            k.copy(w1b[ws][:], st1[:], eng="act"); k.copy(w3b[ws][:], st3[:], eng="pool"); k.copy(w2b[ws][:], st2[:], eng="pool")
            for (bt0, bnt) in blocks:
                bi += 1
                n = bnt * 128; c0 = bt0 * 128
                for ft in R2:
                    pa = nps(); pb = nps()
                    for kk in range(8):
                        k.mm(pa[:, :n], w1b[ws][:, kk, ft * 128:(ft + 1) * 128], hT[:, kk, c0:c0 + n], start=(kk == 0), stop=(kk == 7), inc=(kk == 7))
                    for kk in range(8):
                        k.mm(pb[:, :n], w3b[ws][:, kk, ft * 128:(ft + 1) * 128], hT[:, kk, c0:c0 + n], start=(kk == 0), stop=(kk == 7), inc=(kk == 7))
                    s_ = sa[ft]
                    k.act(s_[:, :n], pa[:, :n], AF.Silu)
                    k.tt(actb[bi % 2][ft][:, :n], pb[:, :n], s_[:, :n], ALU.mult)
                if pend is not None:
                    pend()

                def pend(e=e, ws=ws, bi=bi, bt0=bt0, bnt=bnt):
                    for t in range(bnt):
                        gi = p0 + bt0 + t
                        for hf in R2:
                            py = nps()
                            for ft in R2:
                                k.mm(py[:, :], actb[bi % 2][ft][:, t * 128:(t + 1) * 128], w2b[ws][:, ft, hf * 512:(hf + 1) * 512], start=(ft == 0), stop=(ft == 1), inc=(ft == 1))
                            ya = yacc[:, bt0 + t, hf * 512:(hf + 1) * 512]
                            if e == 0:
                                k.ts(ya, py[:, :], comb[:, gi, e:e + 1], ALU.mult)
                            else:
                                k.stt(ya, py[:, :], comb[:, gi, e:e + 1], ya, ALU.mult, ALU.add)
        pend()
        for li in range(nt):
            gi = p0 + li
            xi += 1
            x_ = xt[xi % 2]
            k.dma(x_[:], x1[gi * 128:(gi + 1) * 128, :])
            j = 1 if gi == 32 else 0
            k.tt(yo[:], yacc[:, li, :], g2t[:, j, :], ALU.mult)
            k.tt(yo[:], yo[:], x_[:], ALU.add, eng="pool")
            if last:
                rms(yo[:])
                k.stt(yo2[:], yo[:], rr[:, 0:1], fgt[:], ALU.mult, ALU.mult)
                k.dma(out[gi * 128:(gi + 1) * 128, :], yo2[:])
            else:
                k.dma(out[gi * 128:(gi + 1) * 128, :], yo[:])
    return k.finish()


def run_O2(x1s, mod_l, inp, l, last, cores=range(8)):
    wr = np.ascontiguousarray(np.concatenate([inp["moe_w_group"][l], inp["moe_w_expert"][l]], axis=1))
    brb = np.ascontiguousarray(bc128(np.concatenate([inp["moe_b_group"][l], inp["moe_b_expert"][l]])))
    w1 = inp["moe_w1"][l].reshape(32, D, 256); w3 = inp["moe_w3"][l].reshape(32, D, 256); w2 = inp["moe_w2"][l].reshape(32, 256, D)
    fgb = np.ascontiguousarray(bc128(inp["final_norm_g"])); ident = np.eye(128, dtype=np.float32)
    maps = []
    for i, core in enumerate(cores):
        b = core // 2
        modv = np.stack([pk(mod_l[3, :, b]), pk(mod_l[4, :, b]), pk(mod_l[3, :, 4]), pk(mod_l[4, :, 4])], axis=2)
        g2b = np.ascontiguousarray(np.stack([bc128(mod_l[5, :, b]), bc128(mod_l[5, :, 4])], axis=1))
        maps.append({"x1": x1s[i], "modv": np.ascontiguousarray(modv), "g2n": pk(inp["norm2_g"][l]), "g2b": g2b, "fgb": fgb,
                     "wr": wr, "brb": brb, "w1": w1, "w3": w3, "w2": w2, "ident": ident})
    res = run(build_O2(bool(last)), maps)
    return [r["x2"] for r in res]


def unshard(x2s):
    x = np.empty((B, L, D), np.float32); xc = np.empty((B, LC, D), np.float32)
    for core in range(8):
        b, h = core // 2, core % 2
        x[b, h * 4096:(h + 1) * 4096] = x2s[core][:4096]; xc[b, h * 128:(h + 1) * 128] = x2s[core][4096:]
    return x, xc


def kernel(**inp):
    inp = {k_: np.asarray(v_) for k_, v_ in inp.items()}
    mod = run_M(inp)
    x, xc = inp["x"], inp["ctx"]
    for l in range(2):
        xs = shard_tokens(x, xc)
        resA = run_A(x, xc, mod[l], inp, l)
        y5 = run_S5(gather_fm(resA, "uT"), inp, l)
        glf, glb = run_GLA(gather_fm(resA, "gqT"), gather_fm(resA, "gkT"), gather_fm(resA, "gaT"), gather_tm(resA, "gv"), inp, l)
        nao = run_NA(gather_fm(resA, "nqT"), gather_fm(resA, "nkT"), gather_tm(resA, "nv"), inp, l)
        cvo = run_CV(gather_fm(resA, "cyT"), inp, l)
        x1s = run_O1(xs, resA, y5, glf, glb, nao, cvo, mod[l], inp, l)
        del resA, y5, glf, glb, nao, cvo
        x2s = run_O2(x1s, mod[l], inp, l, l == 1)
        x, xc = unshard(x2s)
    return x
```

```python
import functools
import numpy as np
import ml_dtypes
import concourse.bass as bass
import concourse.mybir as mybir
from concourse.bass_utils import run_bass_kernel_spmd
from contextlib import ExitStack

F32 = mybir.dt.float32
BF16 = mybir.dt.bfloat16
AF = mybir.ActivationFunctionType
ALU = mybir.AluOpType
AX = mybir.AxisListType
NPBF = ml_dtypes.bfloat16
PI = float(np.pi)

NCORE = 8
D = 1024
B = 4
L = 8192
LC = 256
NLOC = 4096 + 128
EXT = LC + L
EPS = 1e-6


class View:
    __slots__ = ("b", "ap")

    def __init__(self, b, ap):
        self.b = b
        self.ap = ap

    def __getitem__(self, k):
        return View(self.b, self.ap[k])

    def bc(self, shape):
        return View(self.b, self.ap.to_broadcast(list(shape)))

    def re(self, s, **kw):
        return View(self.b, self.ap.rearrange(s, **kw))


class Buf:
    __slots__ = ("t", "name", "lw", "rd", "dsem", "dcnt", "dram")

    def __init__(self, t, name, dram=False):
        self.t = t
        self.name = name
        self.lw = None
        self.rd = []
        self.dsem = None
        self.dcnt = 0
        self.dram = dram

    def __getitem__(self, k):
        return View(self, self.t[k])


def _v(x):
    return x.ap if isinstance(x, View) else x


class K:
    def __init__(self):
        self.nc = bass.Bass("TRN2", target_bir_lowering=False)
        self.es = ExitStack()
        nc = self.nc
        self.eng = {"pe": nc.tensor, "act": nc.scalar, "dve": nc.vector, "pool": nc.gpsimd, "sp": nc.sync}
        self.sem = {}
        self.cnt = {}
        self.semobj = {}
        for e in self.eng:
            s = self.es.enter_context(nc.semaphore("s_" + e))
            self.sem[e] = s
            self.cnt[e] = 0
            self.semobj[("E", e)] = s
        self.waited = {e: {} for e in self.eng}
        self.nb = 0
        self.ninst = 0
        self.dbufs = []
        self.stk = [self.es]

    def sb(self, shape, dt=F32, name=None):
        self.nb += 1
        name = name or f"sb{self.nb}"
        return Buf(self.stk[-1].enter_context(self.nc.sbuf_tensor(name, list(shape), dt)), name)

    def barrier(self):
        deps = [(("E", e), self.cnt[e]) for e in self.eng] + [(b.dsem, b.dcnt) for b in self.dbufs]
        for e in self.eng:
            self._wait(e, deps)

    def push(self):
        self.stk.append(ExitStack())

    def pop(self):
        self.barrier()
        self.stk.pop().close()

    def ps(self, shape, dt=F32, name=None):
        self.nb += 1
        name = name or f"ps{self.nb}"
        return Buf(self.es.enter_context(self.nc.psum_tensor(name, list(shape), dt)), name)

    def din(self, name, shape, dt=F32):
        return Buf(self.nc.dram_tensor(name, list(shape), dt, kind="ExternalInput").ap(), name, dram=True)

    def dout(self, name, shape, dt=F32):
        return Buf(self.nc.dram_tensor(name, list(shape), dt, kind="ExternalOutput").ap(), name, dram=True)

    def _wait(self, e, deps):
        eng = self.eng[e]
        w = self.waited[e]
        best = {}
        for d in deps:
            if d is None:
                continue
            kk, v = d
            if kk == ("E", e) and v > self.cnt[e]:
                v = self.cnt[e]
            if best.get(kk, 0) < v:
                best[kk] = v
        for kk, v in best.items():
            if w.get(kk, 0) < v:
                eng.wait_ge(self.semobj[kk], v)
                w[kk] = v

    def op(self, e, fn, reads=(), writes=(), inc=True):
        reads = [r.b if isinstance(r, View) else r for r in reads if r is not None and not isinstance(r, (int, float))]
        writes = [r.b if isinstance(r, View) else r for r in writes]
        deps = []
        for b in reads:
            deps.append(b.lw)
        for b in writes:
            deps.append(b.lw)
            deps.extend(b.rd)
        self._wait(e, deps)
        inst = fn(self.eng[e])
        self.ninst += 1
        if inc:
            self.cnt[e] += 1
            inst.then_inc(self.sem[e], 1)
            tk = (("E", e), self.cnt[e])
        else:
            tk = (("E", e), self.cnt[e] + 1)
        for b in writes:
            b.lw = tk
            b.rd = []
        for b in reads:
            if b not in writes and not b.dram:
                b.rd.append(tk)
        return inst

    def dma(self, out, in_, q="sp", chain=False, **kw):
        ob, ib = out.b, in_.b
        own = ib if ob.dram else ob
        if own.dsem is None:
            s = self.es.enter_context(self.nc.semaphore("d_" + own.name))
            own.dsem = ("D", own.name)
            self.semobj[own.dsem] = s
            self.dbufs.append(own)
        key = own.dsem
        deps = [ib.lw]
        ch = chain and ob.lw is not None and ob.lw[0] == key
        if not ch:
            deps.append(ob.lw)
        deps.extend(ob.rd)
        self._wait(q, deps)
        inst = self.eng[q].dma_start(out=out.ap, in_=in_.ap, **kw)
        self.ninst += 1
        own.dcnt += 16
        inst.then_inc(self.semobj[key], 16)
        tk = (key, own.dcnt)
        if not ob.dram:
            ob.lw = tk
            if not ch:
                ob.rd = []
        if not ib.dram:
            ib.rd.append(tk)
        return inst

    def finish(self, q="sp"):
        self._wait(q, [(b.dsem, b.dcnt) for b in self.dbufs])
        self.es.close()
        return self.nc

    def mm(self, out, lhsT, rhs, start=True, stop=True, inc=True):
        return self.op("pe", lambda e: e.matmul(out.ap, lhsT=lhsT.ap, rhs=rhs.ap, start=start, stop=stop),
                       reads=[lhsT, rhs] + ([] if start else [out]), writes=[out], inc=inc)

    def tr(self, out, in_, ident, inc=True):
        return self.op("pe", lambda e: e.transpose(out.ap, in_.ap, ident.ap), reads=[in_, ident], writes=[out], inc=inc)

    def act(self, out, in_, func, bias=None, scale=None, accum=None, eng="act"):
        kw = {}
        if bias is not None:
            kw["bias"] = _v(bias)
        if scale is not None:
            kw["scale"] = _v(scale)
        if accum is not None:
            kw["accum_out"] = accum.ap
        return self.op(eng, lambda e: e.activation(out=out.ap, in_=in_.ap, func=func, **kw),
                       reads=[in_, bias, scale], writes=[out] + ([accum] if accum is not None else []))

    def tt(self, out, in0, in1, op, eng="dve"):
        return self.op(eng, lambda e: e.tensor_tensor(out=out.ap, in0=in0.ap, in1=in1.ap, op=op), reads=[in0, in1], writes=[out])

    def ts(self, out, in0, s1, op0, s2=None, op1=None, eng="dve"):
        if op1 is None:
            return self.op(eng, lambda e: e.tensor_scalar(out=out.ap, in0=in0.ap, scalar1=_v(s1), scalar2=None, op0=op0),
                           reads=[in0, s1], writes=[out])
        return self.op(eng, lambda e: e.tensor_scalar(out=out.ap, in0=in0.ap, scalar1=_v(s1), scalar2=_v(s2), op0=op0, op1=op1),
                       reads=[in0, s1, s2], writes=[out])

    def stt(self, out, in0, scalar, in1, op0, op1, eng="dve"):
        return self.op(eng, lambda e: e.scalar_tensor_tensor(out=out.ap, in0=in0.ap, scalar=_v(scalar), in1=in1.ap, op0=op0, op1=op1),
                       reads=[in0, scalar, in1], writes=[out])

    def scan(self, out, d0, d1, initial, op0=ALU.mult, op1=ALU.add):
        return self.op("dve", lambda e: e.tensor_tensor_scan(out=out.ap, data0=d0.ap, data1=d1.ap, initial=_v(initial), op0=op0, op1=op1),
                       reads=[d0, d1, initial], writes=[out])

    def copy(self, out, in_, eng="dve"):
        if eng == "act":
            return self.act(out, in_, AF.Copy)
        return self.op(eng, lambda e: e.tensor_copy(out=out.ap, in_=in_.ap), reads=[in_], writes=[out])

    def memset(self, out, val, eng="dve"):
        return self.op(eng, lambda e: e.memset(out.ap, val), writes=[out])

    def recip(self, out, in_):
        return self.op("dve", lambda e: e.reciprocal(out=out.ap, in_=in_.ap), reads=[in_], writes=[out])

    def reduce(self, out, in_, op, axis=AX.X):
        return self.op("dve", lambda e: e.tensor_reduce(out=out.ap, in_=in_.ap, axis=axis, op=op), reads=[in_], writes=[out])

    def wrap(self, out, in_, shift):
        return self.op("dve", lambda e: e.add_range_wrap(out=out.ap, in_=in_.ap, shift=shift, bound=PI, period=2 * PI),
                       reads=[in_], writes=[out])


def run(nc, in_maps):
    return run_bass_kernel_spmd(nc, in_maps, core_ids=list(range(len(in_maps)))).results


@functools.lru_cache(None)
def build_M():
    k = K()
    cT = k.din("cT", [128, 8, 8]); w = k.din("w", [1024, 1536]); bm = k.din("bm", [128, 12])
    out = k.dout("mod", [128, 12, 8])
    ct = k.sb([128, 8, 8]); ca = k.sb([128, 8, 8]); wt = k.sb([128, 8, 1536]); bt = k.sb([128, 12]); ot = k.sb([128, 12, 8])
    ps = [k.ps([128, 8]) for _ in range(2)]
    k.dma(ct[:], cT[:, :, :]); k.dma(bt[:], bm[:, :])
    wv = w[:, :].re("(k p) c -> p k c", p=128)
    for kk in range(8):
        k.dma(wt[:, kk, :], wv[:, kk, :], chain=True)
    k.act(ca[:], ct[:], AF.Silu)
    for m in range(12):
        p = ps[m % 2]
        for kk in range(8):
            k.mm(p[:], wt[:, kk, m * 128:(m + 1) * 128], ca[:, kk, :], start=(kk == 0), stop=(kk == 7), inc=(kk == 7))
        k.act(ot[:, m, :], p[:], AF.Identity, bias=bt[:, m:m + 1])
    k.dma(out[:, :, :], ot[:])
    return k.finish()


def run_M(inp):
    c, c_ctx, w_mod, b_mod = inp["c"], inp["c_ctx"], inp["w_mod"], inp["b_mod"]
    cc = np.zeros((8, D), np.float32); cc[:4] = c; cc[4] = c_ctx
    cT = np.ascontiguousarray(cc.T.reshape(8, 128, 8).transpose(1, 0, 2))
    maps = []
    for core in range(8):
        l, q = core // 4, core % 4
        maps.append({"cT": cT, "w": np.ascontiguousarray(w_mod[l][:, q * 1536:(q + 1) * 1536]),
                     "bm": np.ascontiguousarray(b_mod[l][q * 1536:(q + 1) * 1536].reshape(12, 128).T)})
    res = run(build_M(), maps)
    mod = np.zeros((2, 6 * D, 8), np.float32)
    for core in range(8):
        l, q = core // 4, core % 4
        mod[l, q * 1536:(q + 1) * 1536] = res[core]["mod"].transpose(1, 0, 2).reshape(1536, 8)
    return mod.reshape(2, 6, D, 8)


def pk(v):
    return np.ascontiguousarray(np.asarray(v, np.float32).reshape(8, 128).T)


TOKBLK = [(i * 512, 512) for i in range(8)] + [(4096, 128)]
OFF = dict(s5_u=0, gla_q=256, gla_k=512, gla_v=768, gla_r=1280, gla_a=1792, na_q=1824, na_k=2080, na_v=2336, conv_in=2592, gates=3104)


def norm_mod_T(k, x_dram, ntile, A_lat, B_lat, A_ctx, B_ctx, hT, ident_b, ps_t, hT32=None, ident_f=None, after_tile=None):
    xt = [k.sb([128, 1024]) for _ in range(2)]
    xn = [k.sb([128, 1024], BF16 if hT32 is None else F32) for _ in range(2)]
    junk = k.sb([128, 1024], BF16)
    tmpf = k.sb([128, 8, 128])
    ss = [k.sb([128, 1]) for _ in range(2)]
    rs = [k.sb([128, 1]) for _ in range(2)]
    for i in range(ntile):
        x_, n_, s_, r_ = xt[i % 2], xn[i % 2], ss[i % 2], rs[i % 2]
        k.dma(x_[:], x_dram[i * 128:(i + 1) * 128, :])
        k.act(junk[:], x_[:], AF.Square, accum=s_[:])
        k.ts(r_[:], s_[:], 1.0 / D, ALU.mult, EPS, ALU.add)
        k.act(r_[:], r_[:], AF.Sqrt)
        k.recip(r_[:], r_[:])
        k.ts(n_[:], x_[:], r_[:, 0:1], ALU.mult)
        tp = ps_t[i % 2]
        for kk in range(8):
            k.tr(tp[:, kk, :], n_[:, kk * 128:(kk + 1) * 128], (ident_b if hT32 is None else ident_f)[:], inc=(kk == 7))
        A_, B_ = (A_ctx, B_ctx) if i == ntile - 1 else (A_lat, B_lat)
        k.tt(tmpf[:], tp[:], View(A_, A_.t[:, :].unsqueeze(2).to_broadcast([128, 8, 128])), ALU.mult)
        Bb = View(B_, B_.t[:, :].unsqueeze(2).to_broadcast([128, 8, 128]))
        if hT32 is None:
            k.tt(hT[:, :, i * 128:(i + 1) * 128], tmpf[:], Bb, ALU.add, eng="pool")
        else:
            k.tt(hT32[:], tmpf[:], Bb, ALU.add)
            k.copy(hT[:, :, i * 128:(i + 1) * 128], hT32[:], eng="pool")
            after_tile(i)


def mod_prep(k, modv, gn):
    mt = k.sb([128, 8, 4]); gt = k.sb([128, 8])
    k.dma(mt[:], modv[:, :, :]); k.dma(gt[:], gn[:, :])
    res = []
    for j in (0, 2):
        A = k.sb([128, 8]); Bv = k.sb([128, 8])
        k.ts(A[:], mt[:, :, j + 1], 1.0, ALU.add)
        k.tt(A[:], A[:], gt[:], ALU.mult)
        k.copy(Bv[:], mt[:, :, j])
        res += [A, Bv]
    return res


@functools.lru_cache(None)
def build_A():
    k = K()
    xa = k.din("xa", [NLOC, D]); modv = k.din("modv", [128, 8, 4]); g1n = k.din("g1n", [128, 8])
    w_in = k.din("w_in", [D, 7200]); gate_b = k.din("gate_b", [128, 32])
    ropeC = k.din("ropeC", [128, NLOC]); ropeS = k.din("ropeS", [128, NLOC]); ident = k.din("ident", [128, 128])
    o_uT = k.dout("uT", [256, NLOC]); o_gq = k.dout("gqT", [256, NLOC]); o_gk = k.dout("gkT", [256, NLOC])
    o_ga = k.dout("gaT", [32, NLOC]); o_gr = k.dout("grsT", [512, NLOC], BF16); o_gv = k.dout("gv", [NLOC, 512], BF16)
    o_nq = k.dout("nqT", [256, NLOC], BF16); o_nk = k.dout("nkT", [256, NLOC], BF16); o_nv = k.dout("nv", [NLOC, 256], BF16)
    o_cy = k.dout("cyT", [256, NLOC], BF16); o_gs = k.dout("gsT", [4096, NLOC], BF16)

    hT = k.sb([128, 8, NLOC], BF16, "hT")
    idf = k.sb([128, 128]); idb = k.sb([128, 128], BF16)
    k.dma(idf[:], ident[:, :]); k.copy(idb[:], idf[:])
    A_lat, B_lat, A_ctx, B_ctx = mod_prep(k, modv, g1n)
    gb = k.sb([128, 32]); k.dma(gb[:], gate_b[:, :])
    rc = k.sb([128, NLOC]); rsn = k.sb([128, NLOC])
    k.dma(rc[:], ropeC[:, :]); k.dma(rsn[:], ropeS[:, :])
    ps_t = [k.ps([128, 8, 128], BF16) for _ in range(2)]
    norm_mod_T(k, xa, 33, A_lat, B_lat, A_ctx, B_ctx, hT, idb, ps_t)

    pss = [k.ps([128, 512]) for _ in range(6)]
    psi = [0]

    def nps():
        psi[0] += 1
        return pss[psi[0] % 6]

    wf = [k.sb([128, 8, 512]) for _ in range(2)]
    wq = k.sb([128, 8, 256])
    wbq = k.sb([128, 8, 256], BF16)
    wb = [k.sb([128, 8, 512], BF16) for _ in range(3)]
    wi = [0]
    stf = [k.sb([128, 512]) for _ in range(3)]
    stb = [k.sb([128, 512], BF16) for _ in range(3)]
    t1 = k.sb([128, 512]); t2 = k.sb([128, 512])
    si = [0]

    def load_w(col0, ncols, scale=None, swap=False):
        wi[0] += 1
        f = wq if swap else wf[wi[0] % 2]
        b = wbq if swap else wb[wi[0] % 3]
        for kk in range(8):
            k.dma(f[:, kk, :ncols], w_in[kk * 128:(kk + 1) * 128, col0:col0 + ncols], chain=True)
        if swap:
            sc = 1.0 if scale is None else scale
            fv = f[:, :, :].re("p k (g t c) -> p (k g) t c", t=2, c=16)
            bv = b[:, :, :].re("p k (g t c) -> p (k g) t c", t=2, c=16)
            k.act(bv[:, :, 0, :], fv[:, :, 1, :], AF.Copy, scale=-sc)
            k.act(bv[:, :, 1, :], fv[:, :, 0, :], AF.Copy, scale=sc)
        elif scale is not None:
            k.act(b[:, :, :ncols], f[:, :, :ncols], AF.Copy, scale=scale)
        else:
            k.copy(b[:, :, :ncols], f[:, :, :ncols], eng=("dve" if wi[0] % 2 else "pool"))
        return b

    def fm_mm(b, c0, M, tok0, n):
        p = nps()
        for kk in range(8):
            k.mm(p[:M, :n], b[:, kk, c0:c0 + M], hT[:, kk, tok0:tok0 + n], start=(kk == 0), stop=(kk == 7), inc=(kk == 7))
        return p

    def fm_simple(col0, ncols, out, post, bf, scale=None):
        for g0 in range(0, ncols, 512):
            gn = min(512, ncols - g0)
            b = load_w(col0 + g0, gn, scale=scale)
            for c0 in range(0, gn, 128):
                M = min(128, gn - c0)
                for (tok0, n) in TOKBLK:
                    p = fm_mm(b, c0, M, tok0, n)
                    si[0] += 1
                    st = (stb if bf else stf)[si[0] % 3]
                    post(st[:M, :n], p[:M, :n], (g0 + c0) // 128)
                    k.dma(out[g0 + c0:g0 + c0 + M, tok0:tok0 + n], st[:M, :n])

    def tm_part(col0, ncols, out):
        b = load_w(col0, ncols)
        for i in range(33):
            p = nps()
            for kk in range(8):
                k.mm(p[:, :ncols], hT[:, kk, i * 128:(i + 1) * 128], b[:, kk, :ncols], start=(kk == 0), stop=(kk == 7), inc=(kk == 7))
            si[0] += 1
            st = stb[si[0] % 3]
            k.copy(st[:, :ncols], p[:, :ncols], eng=("act" if i % 2 else "dve"))
            k.dma(out[i * 128:(i + 1) * 128, :], st[:, :ncols])

    def rope_part(col0, out, scale):
        b = load_w(col0, 256, scale=scale)
        bs = load_w(col0, 256, scale=scale, swap=True)
        for c0 in (0, 128):
            for (tok0, n) in TOKBLK:
                p = fm_mm(b, c0, 128, tok0, n)
                p2 = fm_mm(bs, c0, 128, tok0, n)
                si[0] += 1
                st = stf[si[0] % 3]
                k.tt(t1[:, :n], p[:, :n], rc[:, tok0:tok0 + n], ALU.mult)
                k.tt(t2[:, :n], p2[:, :n], rsn[:, tok0:tok0 + n], ALU.mult)
                k.tt(st[:, :n], t1[:, :n], t2[:, :n], ALU.add, eng="pool")
                k.dma(out[c0:c0 + 128, tok0:tok0 + n], st[:, :n])

    cp = [0]

    def post_copy(st, p, t):
        cp[0] += 1
        k.copy(st, p, eng=("act" if cp[0] % 2 else "dve"))

    fm_simple(OFF["s5_u"], 256, o_uT, post_copy, False)
    rope_part(OFF["gla_q"], o_gq, 0.125)
    rope_part(OFF["gla_k"], o_gk, None)
    tm_part(OFF["gla_v"], 512, o_gv)
    fm_simple(OFF["gla_r"], 512, o_gr, lambda st, p, t: k.act(st, p, AF.Silu), True)
    fm_simple(OFF["gla_a"], 32, o_ga, post_copy, False)
    fm_simple(OFF["na_q"], 256, o_nq, post_copy, True, scale=0.125)
    fm_simple(OFF["na_k"], 256, o_nk, post_copy, True)
    tm_part(OFF["na_v"], 256, o_nv)
    b = load_w(OFF["conv_in"], 512)
    for ct in range(2):
        for (tok0, n) in TOKBLK:
            pv = fm_mm(b, ct * 128, 128, tok0, n)
            pg = fm_mm(b, 256 + ct * 128, 128, tok0, n)
            si[0] += 1
            st = stb[si[0] % 3]
            k.act(t1[:, :n], pg[:, :n], AF.Sigmoid)
            k.tt(st[:, :n], pv[:, :n], t1[:, :n], ALU.mult)
            k.dma(o_cy[ct * 128:(ct + 1) * 128, tok0:tok0 + n], st[:, :n])
    for g0 in range(0, 4096, 512):
        b = load_w(OFF["gates"] + g0, 512)
        for c0 in range(0, 512, 128):
            t = (g0 + c0) // 128
            for (tok0, n) in TOKBLK:
                p = fm_mm(b, c0, 128, tok0, n)
                si[0] += 1
                st = stb[si[0] % 3]
                k.act(st[:, :n], p[:, :n], AF.Sigmoid, bias=gb[:, t:t + 1])
                k.dma(o_gs[t * 128:(t + 1) * 128, tok0:tok0 + n], st[:, :n])
    return k.finish()


def rope_tables():
    inv = (10000.0 ** (-np.arange(0, 32, 2, dtype=np.float32) / 32)).astype(np.float32)
    tabs = []
    for half in range(2):
        t = np.arange(4096) + half * 4096
        row = (t // 64).astype(np.float32); col = (t % 64).astype(np.float32)
        ang = np.zeros((64, 4096), np.float32)
        ang[0:16] = inv[:, None] * row[None]; ang[16:32] = ang[0:16]
        ang[32:48] = inv[:, None] * col[None]; ang[48:64] = ang[32:48]
        c = np.ones((128, NLOC), np.float32); s = np.zeros((128, NLOC), np.float32)
        c[:, :4096] = np.tile(np.cos(ang), (2, 1)); s[:, :4096] = np.tile(np.sin(ang), (2, 1))
        tabs.append((c, s))
    return tabs


def shard_tokens(x, xc):
    out = []
    for core in range(8):
        b, h = core // 2, core % 2
        out.append(np.ascontiguousarray(np.concatenate([x[b, h * 4096:(h + 1) * 4096], xc[b, h * 128:(h + 1) * 128]], 0)))
    return out


def gather_fm(res, name):
    C = res[0][name].shape[0]
    out = np.empty((B, C, EXT), res[0][name].dtype)
    for core in range(8):
        b, h = core // 2, core % 2
        a = res[core][name]
        out[b, :, LC + h * 4096:LC + (h + 1) * 4096] = a[:, :4096]
        out[b, :, h * 128:(h + 1) * 128] = a[:, 4096:]
    return out


def gather_tm(res, name):
    C = res[0][name].shape[1]
    out = np.empty((B, EXT, C), res[0][name].dtype)
    for core in range(8):
        b, h = core // 2, core % 2
        a = res[core][name]
        out[b, LC + h * 4096:LC + (h + 1) * 4096] = a[:4096]
        out[b, h * 128:(h + 1) * 128] = a[4096:]
    return out


def run_A(x, xc, mod_l, inp, l):
    tabs = rope_tables()
    xs = shard_tokens(x, xc)
    ident = np.eye(128, dtype=np.float32)
    maps = []
    for core in range(8):
        b, h = core // 2, core % 2
        modv = np.stack([pk(mod_l[0, :, b]), pk(mod_l[1, :, b]), pk(mod_l[0, :, 4]), pk(mod_l[1, :, 4])], axis=2)
        maps.append({"xa": xs[core], "modv": np.ascontiguousarray(modv), "g1n": pk(inp["norm1_g"][l]), "w_in": inp["w_in"][l],
                     "gate_b": np.ascontiguousarray(inp["gate_b"][l].reshape(32, 128).T),
                     "ropeC": tabs[h][0], "ropeS": tabs[h][1], "ident": ident})
    return run(build_A(), maps)


def sincos(k, th, shape):
    I32 = mybir.dt.int32
    res = []
    for shift in (PI / 2, 0.0):
        a = k.sb(shape); ni = k.sb(shape, I32); nf = k.sb(shape)
        k.ts(a[:], th, shift, ALU.add)
        k.ts(nf[:], a[:], 1.0 / (2 * PI), ALU.mult)
        k.copy(ni[:], nf[:])
        k.copy(nf[:], ni[:])
        k.stt(a[:], nf[:], -2 * PI, a[:], ALU.mult, ALU.add)
        k.ts(nf[:], a[:], PI, ALU.is_gt, -2 * PI, ALU.mult)
        k.tt(a[:], a[:], nf[:], ALU.add)
        k.ts(nf[:], a[:], -PI, ALU.is_lt, 2 * PI, ALU.mult)
        k.tt(a[:], a[:], nf[:], ALU.add)
        k.ts(a[:], a[:], -PI, ALU.max, PI, ALU.min)
        k.act(a[:], a[:], AF.Sin)
        res.append(a)
    return res[0], res[1]


S5BL = 256
S5NB = EXT // S5BL


@functools.lru_cache(None)
def build_S5():
    k = K()
    BL, NB = S5BL, S5NB
    uT = k.din("uT", [128, EXT]); lamS = k.din("lamS", [128, 16, 3]); lamR = k.din("lamR", [128, 16, 64, 2]); ldtR = k.din("ldtR", [128, 16])
    Bre = k.din("Bre", [128, 16, 64]); Bim = k.din("Bim", [128, 16, 64]); CW0 = k.din("CW0", [128, 16, 128]); CWs0 = k.din("CWs0", [128, 16, 128])
    dsk = k.din("dsk", [128, 1]); swapP = k.din("swapP", [128, 128]); sgn = k.din("sgn", [128, 1])
    yT = k.dout("yT", [128, EXT])

    Tc = k.sb([128, 16, BL], name="Tc"); Ts = k.sb([128, 16, BL], name="Ts"); magS = k.sb([128, 16])
    WB = k.sb([128, 16, 128], BF16); WBs = k.sb([128, 16, 128], BF16)
    CW = k.sb([128, 16, 128], BF16); CWs = k.sb([128, 16, 128], BF16)
    sw = k.sb([128, 128]); dk = k.sb([128, 1])
    k.push()
    ls = k.sb([128, 16, 3]); k.dma(ls[:], lamS[:, :, :])
    sg = k.sb([128, 1]); k.dma(sg[:], sgn[:, :])
    k.dma(dk[:], dsk[:, :])
    k.dma(sw[:], swapP[:, :])
    dtS = k.sb([128, 16]); thS = k.sb([128, 16])
    k.act(dtS[:], ls[:, :, 2], AF.Exp)
    k.tt(thS[:], ls[:, :, 1], dtS[:], ALU.mult)
    k.tt(magS[:], ls[:, :, 0], dtS[:], ALU.mult)
    k.act(magS[:], magS[:], AF.Exp)
    cosS, sinS = sincos(k, thS[:], [128, 16])
    t_a = k.sb([128, 16, BL // 2]); t_b = k.sb([128, 16, BL // 2])
    zc = k.sb([128, 16]); zs = k.sb([128, 16]); z1 = k.sb([128, 16]); z2 = k.sb([128, 16])
    k.copy(Tc[:, :, 0], cosS[:]); k.copy(Ts[:, :, 0], sinS[:])
    k.copy(zc[:], cosS[:]); k.copy(zs[:], sinS[:])
    m = 1
    while m < BL:
        zcb = View(zc, zc.t[:, :].unsqueeze(2).to_broadcast([128, 16, m]))
        zsb = View(zs, zs.t[:, :].unsqueeze(2).to_broadcast([128, 16, m]))
        k.tt(t_a[:, :, :m], Tc[:, :, 0:m], zcb, ALU.mult)
        k.tt(t_b[:, :, :m], Ts[:, :, 0:m], zsb, ALU.mult)
        k.tt(t_a[:, :, :m], t_a[:, :, :m], t_b[:, :, :m], ALU.subtract)
        k.tt(t_b[:, :, :m], Ts[:, :, 0:m], zcb, ALU.mult)
        k.copy(Tc[:, :, m:2 * m], t_a[:, :, :m])
        k.tt(t_a[:, :, :m], Tc[:, :, 0:m], zsb, ALU.mult)
        k.tt(Ts[:, :, m:2 * m], t_a[:, :, :m], t_b[:, :, :m], ALU.add)
        k.tt(z1[:], zc[:], zc[:], ALU.mult); k.tt(z2[:], zs[:], zs[:], ALU.mult)
        k.tt(zs[:], zc[:], zs[:], ALU.mult); k.ts(zs[:], zs[:], 2.0, ALU.mult)
        k.tt(zc[:], z1[:], z2[:], ALU.subtract)
        m *= 2
    Tsg = Ts
    k.ts(Tsg[:], Ts[:], sg[:, 0:1], ALU.mult)

    k.pop()
    k.push()
    lr_ = k.sb([128, 16, 64, 2]); k.dma(lr_[:], lamR[:, :, :, :])
    dR = k.sb([128, 16]); k.dma(dR[:], ldtR[:, :]); k.act(dR[:], dR[:], AF.Exp)
    dRb = View(dR, dR.t[:, :].unsqueeze(2).to_broadcast([128, 16, 64]))
    lrR = lr_[:, :, :, 0]; liR = lr_[:, :, :, 1]
    thR = k.sb([128, 16, 64]); mgR = k.sb([128, 16, 64])
    k.tt(thR[:], liR, dRb, ALU.mult)
    k.tt(mgR[:], lrR, dRb, ALU.mult)
    k.act(mgR[:], mgR[:], AF.Exp)
    cosR, sinR = sincos(k, thR[:], [128, 16, 64])
    are = cosR; aim = sinR
    k.tt(are[:], mgR[:], cosR[:], ALU.mult); k.tt(aim[:], mgR[:], sinR[:], ALU.mult)
    k.ts(are[:], are[:], -1.0, ALU.add)
    den = thR; tmp = mgR
    k.tt(den[:], lrR, lrR, ALU.mult); k.tt(tmp[:], liR, liR, ALU.mult); k.tt(den[:], den[:], tmp[:], ALU.add)
    k.recip(den[:], den[:])
    cre = k.sb([128, 16, 64]); cim = k.sb([128, 16, 64])
    k.tt(cre[:], are[:], lrR, ALU.mult); k.tt(tmp[:], aim[:], liR, ALU.mult); k.tt(cre[:], cre[:], tmp[:], ALU.add); k.tt(cre[:], cre[:], den[:], ALU.mult)
    k.tt(cim[:], aim[:], lrR, ALU.mult); k.tt(tmp[:], are[:], liR, ALU.mult); k.tt(cim[:], cim[:], tmp[:], ALU.subtract); k.tt(cim[:], cim[:], den[:], ALU.mult)
    br = k.sb([128, 16, 64]); bi = k.sb([128, 16, 64])
    k.dma(br[:], Bre[:, :, :]); k.dma(bi[:], Bim[:, :, :])
    WBf = k.sb([128, 16, 128])
    k.tt(WBf[:, :, 0:64], cre[:], br[:], ALU.mult); k.tt(tmp[:], cim[:], bi[:], ALU.mult); k.tt(WBf[:, :, 0:64], WBf[:, :, 0:64], tmp[:], ALU.subtract)
    k.tt(WBf[:, :, 64:128], cre[:], bi[:], ALU.mult); k.tt(tmp[:], cim[:], br[:], ALU.mult); k.tt(WBf[:, :, 64:128], WBf[:, :, 64:128], tmp[:], ALU.add)
    k.copy(WB[:], WBf[:])
    k.copy(WBs[:, :, 0:64], WBf[:, :, 64:128]); k.copy(WBs[:, :, 64:128], WBf[:, :, 0:64])
    cwf = k.sb([128, 16, 128]); cwsf = k.sb([128, 16, 128])
    k.dma(cwf[:], CW0[:, :, :]); k.dma(cwsf[:], CWs0[:, :, :])
    k.copy(CW[0:64], cwf[0:64]); k.act(CW[64:128], cwf[64:128], AF.Copy, scale=-1.0)
    k.act(CWs[0:64], cwsf[0:64], AF.Copy, scale=-1.0); k.copy(CWs[64:128], cwsf[64:128])

    k.pop()
    uf = k.sb([128, EXT], name="uf"); ub = k.sb([128, EXT], BF16, name="ub"); yacc = k.sb([128, EXT], name="yacc")
    for c in range(0, EXT, 2112):
        k.dma(uf[:, c:c + 2112], uT[:, c:c + 2112], chain=True)
    k.copy(ub[:], uf[:], eng="pool")
    k.ts(yacc[:], uf[:], dk[:, 0:1], ALU.mult)

    Gt = self_t = k.es.enter_context(k.nc.sbuf_tensor("Gbig", [128, 16, BL], F32))
    G = [Buf(Gt[:, g, :], f"G{g}") for g in range(16)]
    M1 = [k.sb([128, 8, BL], BF16) for _ in range(2)]; M2 = [k.sb([128, 8, BL], BF16) for _ in range(2)]
    Wt = [k.sb([128, BL]) for _ in range(4)]; T1 = [k.sb([128, BL]) for _ in range(4)]; T2 = [k.sb([128, BL]) for _ in range(4)]
    carry = [k.sb([128, 8]) for _ in range(2)]; gl = [k.sb([128, 8]) for _ in range(2)]
    c1 = [k.sb([128, 8]) for _ in range(2)]; c2 = [k.sb([128, 8]) for _ in range(2)]
    for d in range(2):
        k.memset(carry[d][:], 0.0)
    psSW = [k.ps([128, 2, BL]) for _ in range(4)]
    psY = [k.ps([128, BL]) for _ in range(2)]; psG = [k.ps([128, 8]) for _ in range(2)]
    it = 0
    pending = None
    for s in range(NB):
        for d in range(2):
            blk = s if d == 0 else (0 if s == 0 else NB - s)
            c0 = blk * BL
            rhs = ub[:, c0:c0 + BL]
            if d == 1:
                rhs = rhs[:, ::-1]
            for g8 in range(8):
                vg = d * 8 + g8
                it += 1
                pS, pW = psSW[it % 4][:, 0, :], psSW[it % 4][:, 1, :]
                k.mm(pS, WB[:, vg, :], rhs)
                k.mm(pW, WBs[:, vg, :], rhs)
                t1, t2, w = T1[it % 4], T2[it % 4], Wt[it % 4]
                k.tt(t1[:], pS, Tc[:, vg, :], ALU.mult)
                k.tt(t2[:], pW, Tsg[:, vg, :], ALU.mult)
                k.tt(w[:], t1[:], t2[:], ALU.add)
                k.scan(G[vg][:], View(magS, magS.t[:, vg:vg + 1].to_broadcast([128, BL])), w[:], carry[d][:, g8:g8 + 1])
                k.tt(M1[d][:, g8, :], G[vg][:], Tc[:, vg, :], ALU.mult, eng="pool")
                k.tt(M2[d][:, g8, :], G[vg][:], Tsg[:, vg, :], ALU.mult, eng="pool")
                k.copy(gl[d][:, g8:g8 + 1], G[vg][:, BL - 1:BL], eng="act")
            if pending is not None:
                pending()

            def pending(d=d, c0=c0):
                py = psY[d]
                for g8 in range(8):
                    vg = d * 8 + g8
                    k.mm(py[:], CW[:, vg, :], M1[d][:, g8, :], start=(g8 == 0), stop=False, inc=False)
                    k.mm(py[:], CWs[:, vg, :], M2[d][:, g8, :], start=False, stop=(g8 == 7), inc=(g8 == 7))
                src = py[:, :] if d == 0 else py[:, ::-1]
                k.tt(yacc[:, c0:c0 + BL], src, yacc[:, c0:c0 + BL], ALU.add)
                k.mm(psG[d][:], sw[:], gl[d][:])
                k.tt(c1[d][:], gl[d][:], Tc[:, 8 * d:8 * d + 8, BL - 1], ALU.mult)
                k.tt(c2[d][:], psG[d][:], Tsg[:, 8 * d:8 * d + 8, BL - 1], ALU.mult)
                k.tt(carry[d][:], c1[d][:], c2[d][:], ALU.subtract)
    pending()
    for c in range(0, EXT, 2112):
        k.dma(yT[:, c:c + 2112], yacc[:, c:c + 2112])
    return k.finish()


def s5_maps(uT_all, inp, l):
    maps = []
    swapP = np.zeros((128, 128), np.float32)
    for p in range(64):
        swapP[p, p + 64] = 1.0; swapP[p + 64, p] = 1.0
    sgn = np.ones((128, 1), np.float32); sgn[64:] = -1.0
    for core in range(8):
        b, ct = core // 2, core % 2
        gs = slice(8 * ct, 8 * ct + 8)
        fl = lambda a: np.asarray(a[l][:, gs]).reshape((16,) + a.shape[3:])
        lre, lim, ldt = fl(inp["s5_lam_re"]), fl(inp["s5_lam_im"]), fl(inp["s5_log_dt"])
        bre, bim, cre, cim = fl(inp["s5_b_re"]), fl(inp["s5_b_im"]), fl(inp["s5_c_re"]), fl(inp["s5_c_im"])
        lamS = np.zeros((128, 16, 3), np.float32)
        lamS[:64, :, 0] = lre.T; lamS[64:, :, 0] = lre.T; lamS[:64, :, 1] = lim.T; lamS[64:, :, 1] = lim.T; lamS[:, :, 2] = ldt[None, :]
        lamR = np.broadcast_to(np.stack([lre, lim], -1)[None], (128, 16, 64, 2)).astype(np.float32)
        ldtR = np.broadcast_to(ldt[None], (128, 16)).astype(np.float32)
        Bre = np.zeros((128, 16, 64), np.float32); Bim = np.zeros((128, 16, 64), np.float32)
        CW0 = np.zeros((128, 16, 128), np.float32); CWs0 = np.zeros((128, 16, 128), np.float32)
        for vg in range(16):
            r0 = 16 * (vg % 8)
            Bre[r0:r0 + 16, vg, :] = bre[vg].T; Bim[r0:r0 + 16, vg, :] = bim[vg].T
            CW0[:64, vg, r0:r0 + 16] = cre[vg].T; CW0[64:, vg, r0:r0 + 16] = cim[vg].T
            CWs0[:64, vg, r0:r0 + 16] = cim[vg].T; CWs0[64:, vg, r0:r0 + 16] = cre[vg].T
        maps.append({"uT": np.ascontiguousarray(uT_all[b, ct * 128:(ct + 1) * 128]), "lamS": lamS, "lamR": np.ascontiguousarray(lamR),
                     "ldtR": np.ascontiguousarray(ldtR), "Bre": Bre, "Bim": Bim, "CW0": CW0, "CWs0": CWs0,
                     "dsk": np.ascontiguousarray(inp["s5_d"][l][ct * 128:(ct + 1) * 128, None]), "swapP": swapP, "sgn": sgn})
    return maps


def run_S5(uT_all, inp, l):
    res = run(build_S5(), s5_maps(uT_all, inp, l))
    y = np.empty((B, 256, EXT), np.float32)
    for core in range(8):
        y[core // 2, (core % 2) * 128:(core % 2 + 1) * 128] = res[core]["yT"]
    return y


GBLK = [(0, 256)] + [(256 + 512 * i, 512) for i in range(16)]


@functools.lru_cache(None)
def build_GLA(DBG=99):
    k = K()
    qT = k.din("qT", [128, EXT]); kT = k.din("kT", [128, EXT]); gaT = k.din("gaT", [32, EXT]); v = k.din("v", [EXT, 256], BF16)
    wa2 = k.din("wa2", [16, 2, 128]); nba = k.din("nba", [128, 2]); maskd = k.din("mask", [64, 2, 512]); rmaskd = k.din("rmask", [128, 512])
    ident = k.din("ident", [128, 128])
    outs = [k.dout("oTf", [256, EXT]), k.dout("oTb", [256, EXT])]
    wa = k.sb([16, 2, 128]); nb_ = k.sb([128, 2]); mk = k.sb([64, 2, 512]); rm = k.sb([128, 512]); idf = k.sb([128, 128])
    k.dma(wa[:], wa2[:, :, :]); k.dma(nb_[:], nba[:, :]); k.dma(mk[:], maskd[:, :, :]); k.dma(rm[:], rmaskd[:, :]); k.dma(idf[:], ident[:, :])
    R2 = range(2)
    qf = [k.sb([128, 512]) for _ in R2]; kf = [k.sb([128, 512]) for _ in R2]; ga = [k.sb([16, 512]) for _ in R2]
    vb = [k.sb([64, 8, 256], BF16) for _ in R2]
    e1 = [k.sb([128, 512]) for _ in R2]; bp = [k.sb([128, 512]) for _ in R2]; eb = [k.sb([128, 512]) for _ in R2]; enb = [k.sb([128, 512]) for _ in R2]
    qt = [k.sb([128, 512]) for _ in R2]; kt = [k.sb([128, 512]) for _ in R2]; kend = [k.sb([128, 8, 64]) for _ in R2]
    dec = [k.sb([128, 8]) for _ in R2]; ktok = [k.sb([64, 8, 128], BF16) for _ in R2]
    att = [[k.sb([64, 8, 64], BF16) for _ in R2] for _ in R2]
    S = [k.sb([128, 9, 128]) for _ in R2]
    osb = [k.sb([128, 512]) for _ in range(4)]
    for d in R2:
        k.memset(S[d][:, 0, :], 0.0)
    z_ps = k.ps([128, 512]); tr_ps = [k.ps([64, 4, 128])] * 2; att_ps = [k.ps([64, 8, 64])] * 2
    otmp = k.sb([128, 512])
    kvb = k.ps([128, 4, 128])
    o_ps = [k.ps([128, 8, 64]) for _ in R2]; oi_ps = [k.ps([128, 8, 64]) for _ in R2]
    it = 0
    for s in range(17 if DBG == 99 else 1):
        for d in R2:
            bi = s if d == 0 else (0 if s == 0 else 17 - s)
            c0, n = GBLK[bi]
            nch = n // 64
            r = it % 2
            it += 1
            k.dma(qf[r][:, :n], qT[:, c0:c0 + n]); k.dma(kf[r][:, :n], kT[:, c0:c0 + n])
            k.dma(ga[r][:, :n], gaT[16 * d:16 * d + 16, c0:c0 + n])
            k.dma(vb[r][:, :nch, :], v[c0:c0 + n, :].re("(c j) e -> j c e", j=64))
            k.mm(z_ps[:, :n], wa[:, d, :], ga[r][:, :n])
            k.act(e1[r][:, :n], z_ps[:, :n], AF.Exp, bias=nb_[:, d:d + 1], scale=-1.0)
            k.act(e1[r][:, :n], e1[r][:, :n], AF.Ln, bias=1.0)
            if d == 0:
                k.scan(bp[r][:, :n], rm[:, :n], e1[r][:, :n], 0.0)
            else:
                k.scan(bp[r][:, :n][:, ::-1], rm[:, :n], e1[r][:, :n][:, ::-1], 0.0)
            if DBG < 2:
                continue
            k.act(eb[r][:, :n], bp[r][:, :n], AF.Exp, scale=-1.0 / 16)
            k.act(enb[r][:, :n], bp[r][:, :n], AF.Exp, scale=1.0 / 16)
            k.tt(qt[r][:, :n], qf[r][:, :n], eb[r][:, :n], ALU.mult)
            k.tt(kt[r][:, :n], kf[r][:, :n], enb[r][:, :n], ALU.mult, eng="pool")
            ebv = eb[r][:, :n].re("p (c j) -> p c j", j=64)
            k.copy(dec[r][:, :nch], ebv[:, :, 63] if d == 0 else ebv[:, :, 0])
            decb = View(dec[r], dec[r].t[:, :nch].unsqueeze(2).to_broadcast([128, nch, 64]))
            k.tt(kend[r][:, :nch, :], kt[r][:, :n].re("p (c j) -> p c j", j=64), decb, ALU.mult)
            for c in range(nch):
                tp = tr_ps[(c // 4) % 2]
                k.tr(tp[:, c % 4, :], kend[r][:, c, :], idf[:], inc=(c % 4 == 3))
                if c % 4 == 3:
                    k.copy(ktok[r][:, c - 3:c + 1, :], tp[:], eng="act")
            if DBG < 3:
                continue
            for h2 in R2:
                hs = slice(64 * h2, 64 * h2 + 64)
                for c in range(nch):
                    k.mm(att_ps[h2][:, c, :], kt[r][hs, c * 64:(c + 1) * 64], qt[r][hs, c * 64:(c + 1) * 64], inc=(c == nch - 1))
                k.tt(att[r][h2][:, :nch, :], att_ps[h2][:, :nch, :], mk[:, d, :n].re("p (c i) -> p c i", i=64), ALU.mult)
            if DBG < 4:
                continue
            order = list(range(nch)) if d == 0 else list(range(nch - 1, -1, -1))
            for g0 in range(0, nch, 4):
                steps = list(range(g0, min(g0 + 4, nch)))
                for step in steps:
                    c = order[step]
                    for h2 in R2:
                        k.mm(kvb[64 * h2:64 * h2 + 64, step % 4, :], ktok[r][:, c, 64 * h2:64 * h2 + 64], vb[r][:, c, 128 * h2:128 * h2 + 128],
                             inc=(h2 == 1 and step == steps[-1]))
                for step in steps:
                    c = order[step]
                    k.stt(S[d][:, step + 1, :], S[d][:, step, :], dec[r][:, c:c + 1], kvb[:, step % 4, :], ALU.mult, ALU.add)
            if DBG < 5:
                continue
            for h2 in R2:
                hs = slice(64 * h2, 64 * h2 + 64)
                for step, c in enumerate(order):
                    k.mm(o_ps[h2][:, c, :], vb[r][:, c, 128 * h2:128 * h2 + 128], att[r][h2][:, c, :], inc=(step == nch - 1))
                if DBG < 6:
                    continue
                for step, c in enumerate(order):
                    k.mm(oi_ps[h2][:, c, :], S[d][hs, step, :], qt[r][hs, c * 64:(c + 1) * 64], inc=(step == nch - 1))
                if DBG < 7:
                    continue
                ob = osb[(2 * it + h2) % 4]
                k.copy(otmp[:, :n], oi_ps[h2][:, :nch, :].re("p c i -> p (c i)"), eng="act")
                k.tt(ob[:, :n], o_ps[h2][:, :nch, :].re("p c i -> p (c i)"), otmp[:, :n], ALU.add)
                k.dma(outs[d][128 * h2:128 * h2 + 128, c0:c0 + n], ob[:, :n])
            if DBG < 8:
                continue
            k.copy(S[d][:, 0, :], S[d][:, nch, :])
    return k.finish()


def run_GLA(gq, gk, ga, gv, inp, l):
    i_ = np.arange(64)
    mf = (i_[None, :] >= i_[:, None]).astype(np.float32)
    mask = np.stack([np.tile(mf, (1, 8)), np.tile(mf.T, (1, 8))], axis=1)
    rmask = np.ones((128, 512), np.float32); rmask[:, 0::64] = 0.0
    ident = np.eye(128, dtype=np.float32)
    maps = []
    for core in range(8):
        b, hp = core // 2, core % 2
        cs = slice(128 * hp, 128 * hp + 128)
        wa2 = np.ascontiguousarray(inp["gla_w_a2"][l][:, :, cs].transpose(1, 0, 2))
        nba = np.ascontiguousarray((inp["gla_b_a"][l][:, cs]).T) * np.float32(-1.0)
        maps.append({"qT": np.ascontiguousarray(gq[b, cs]), "kT": np.ascontiguousarray(gk[b, cs]), "gaT": np.ascontiguousarray(ga[b]),
                     "v": np.ascontiguousarray(gv[b][:, 256 * hp:256 * hp + 256]), "wa2": wa2, "nba": nba, "mask": mask, "rmask": rmask, "ident": ident})
    res = run(build_GLA(), maps)
    of = np.empty((B, 512, EXT), np.float32); ob = np.empty((B, 512, EXT), np.float32)
    for core in range(8):
        b, hp = core // 2, core % 2
        of[b, 256 * hp:256 * hp + 256] = res[core]["oTf"]; ob[b, 256 * hp:256 * hp + 256] = res[core]["oTb"]
    return of, ob


@functools.lru_cache(None)
def build_NA():
    k = K()
    qT = k.din("qT", [128, EXT], BF16); kT = k.din("kT", [128, EXT], BF16); v = k.din("v", [EXT, 128], BF16)
    rpbg = k.din("rpbg", [128, 28, 64]); maskd = k.din("mask", [128, 64])
    out = k.dout("naT", [128, EXT], BF16)
    q_sb = k.sb([64, 2, EXT], BF16, "na_q"); k_sb = k.sb([64, 2, EXT], BF16, "na_k")
    v_ev = k.sb([128, 66, 128], BF16, "na_ve"); v_od = k.sb([128, 65, 128], BF16, "na_vo")
    o_sb = k.sb([128, EXT], BF16, "na_o")
    for h in range(2):
        k.dma(q_sb[:, h, :], qT[64 * h:64 * h + 64, :]); k.dma(k_sb[:, h, :], kT[64 * h:64 * h + 64, :])
    vv_e = v[:, :].re("(t p) e -> p t e", p=128); vv_o = v[64:64 + 65 * 128, :].re("(t p) e -> p t e", p=128)
    for t0 in range(0, 66, 22):
        k.dma(v_ev[:, t0:t0 + 22, :], vv_e[:, t0:t0 + 22, :], chain=True)
    for t0 in range(0, 65, 13):
        k.dma(v_od[:, t0:t0 + 13, :], vv_o[:, t0:t0 + 13, :], chain=True)
    rb = k.sb([128, 28, 64]); mk = k.sb([128, 64]); E = k.sb([128, 28, 64], BF16); ones = k.sb([128, 64], BF16)
    k.dma(rb[:], rpbg[:, :, :]); k.dma(mk[:], maskd[:, :])
    k.act(rb[:], rb[:], AF.Exp)
    k.tt(E[:], rb[:], View(mk, mk.t[:, :].unsqueeze(1).to_broadcast([128, 28, 64])), ALU.mult)
    k.memset(ones[:], 1.0)
    stl = [k.ps([128, 8, 64]) for _ in range(2)]; stc = [k.ps([128, 8, 64]) for _ in range(2)]; po = [k.ps([128, 8, 64]) for _ in range(2)]
    pt = [k.sb([128, 6, 64], BF16) for _ in range(2)]; pe_ = [k.sb([128, 4, 64], BF16) for _ in range(2)]
    rden = [k.sb([128, 64]) for _ in range(2)]
    it = 0
    rows = [("c", j) for j in range(4)] + [("l", r) for r in range(128)]
    for ri, (kind, r) in enumerate(rows):
        q0 = 64 * r if kind == "c" else LC + 64 * r
        p_o = po[ri % 2]
        for h in range(2):
            it += 1
            sl, sc, p_t, p_e = stl[it % 2], stc[it % 2], pt[it % 2], pe_[it % 2]
            qv = q_sb[:, h, q0:q0 + 64]
            for j in range(2):
                k.mm(sc[:, j, :], k_sb[:, h, 128 * j:128 * j + 128], qv, inc=(j == 1))
            k.act(p_t[:, 0:2, :], sc[:, 0:2, :], AF.Exp)
            tiles = [(v_ev, 0), (v_ev, 1)]
            if kind == "l":
                kr0 = min(max(r - 4, 0), 120)
                dr0 = kr0 - r + 7
                for j in range(4):
                    kt = LC + 64 * (kr0 + 2 * j)
                    k.mm(sl[:, j, :], k_sb[:, h, kt:kt + 128], qv, inc=(j == 3))
                    tiles.append((v_ev, kt // 128) if kt % 128 == 0 else (v_od, (kt - 64) // 128))
                k.act(p_e[:], sl[:, 0:4, :], AF.Exp)
                k.tt(p_t[:, 2:6, :], p_e[:], E[:, 14 * h + dr0:14 * h + dr0 + 7:2, :], ALU.mult, eng=("pool" if it % 2 else "dve"))
            n = len(tiles)
            ov = p_o[64 * h:64 * h + 64, 0, :]; dv = p_o[64 * h:64 * h + 64, 1, :]
            for i, (vb_, vt) in enumerate(tiles):
                k.mm(ov, vb_[:, vt, 64 * h:64 * h + 64], p_t[:, i, :], start=(i == 0), stop=(i == n - 1), inc=(i == n - 1))
            for i in range(n):
                k.mm(dv, ones[:], p_t[:, i, :], start=(i == 0), stop=(i == n - 1), inc=(i == n - 1))
        rd = rden[ri % 2]
        k.recip(rd[:], p_o[:, 1, :])
        k.tt(o_sb[:, q0:q0 + 64], p_o[:, 0, :], rd[:], ALU.mult)
    for c in range(0, EXT, 2112):
        k.dma(out[:, c:c + 2112], o_sb[:, c:c + 2112])
    return k.finish()


def na_tables(rpb_l):
    c = np.arange(64)
    dc = np.clip(c[:, None] - c[None, :] + 15, 0, 30)
    cs = np.clip(c - 8, 0, 48)
    mask = ((c[:, None] >= cs[None, :]) & (c[:, None] < cs[None, :] + 16)).astype(np.float32)
    g = np.asarray(rpb_l)[:, :, dc].transpose(2, 0, 1, 3)
    g2 = np.concatenate([g[:, :, 0:14], g[:, :, 1:15]], axis=0)
    return np.ascontiguousarray(g2), np.ascontiguousarray(np.tile(mask, (2, 1)))


def run_NA(nq, nk, nv, inp, l):
    g, mask = na_tables(inp["na_rpb"][l])
    maps = []
    for core in range(8):
        b, hp = core // 2, core % 2
        cs = slice(128 * hp, 128 * hp + 128)
        maps.append({"qT": np.ascontiguousarray(nq[b, cs]), "kT": np.ascontiguousarray(nk[b, cs]), "v": np.ascontiguousarray(nv[b][:, cs]),
                     "rpbg": np.ascontiguousarray(g[:, 2 * hp:2 * hp + 2].reshape(128, 28, 64)), "mask": mask})
    res = run(build_NA(), maps)
    o = np.empty((B, 256, EXT), NPBF)
    for core in range(8):
        o[core // 2, 128 * (core % 2):128 * (core % 2) + 128] = res[core]["naT"]
    return o


@functools.lru_cache(None)
def build_CV():
    k = K()
    cy = k.din("cy", [128, EXT], BF16); dw = k.din("dw", [128, 31]); dwb = k.din("dwb", [128, 1])
    out = k.dout("cvT", [128, EXT])
    W = 15 + LC + 30 + L + 15
    yp = k.sb([128, W], BF16, "cv_yp"); wt = k.sb([128, 31]); bt = k.sb([128, 1])
    k.dma(wt[:], dw[:, :]); k.dma(bt[:], dwb[:, :])
    k.memset(yp[:, 0:15], 0.0); k.memset(yp[:, 271:301], 0.0); k.memset(yp[:, 301 + L:W], 0.0)
    k.dma(yp[:, 15:271], cy[:, 0:LC]); k.dma(yp[:, 301:301 + L], cy[:, LC:EXT], chain=True)
    chunks = [(0, 0, LC)] + [(LC + c, 286 + c, 2048) for c in range(0, L, 2048)]
    for (o0, i0, n) in chunks:
        acc = k.sb([128, n])
        k.ts(acc[:], yp[:, i0:i0 + n], wt[:, 0:1], ALU.mult, bt[:, 0:1], ALU.add)
        for j in range(1, 31):
            k.stt(acc[:], yp[:, i0 + j:i0 + j + n], wt[:, j:j + 1], acc[:], ALU.mult, ALU.add)
        k.dma(out[:, o0:o0 + n], acc[:])
    return k.finish()


def run_CV(cy, inp, l):
    maps = []
    for core in range(8):
        b, ct = core // 2, core % 2
        cs = slice(128 * ct, 128 * ct + 128)
        maps.append({"cy": np.ascontiguousarray(cy[b, cs]), "dw": np.ascontiguousarray(inp["conv_dw"][l][:, cs].T),
                     "dwb": np.ascontiguousarray(inp["conv_dw_b"][l][cs, None])})
    res = run(build_CV(), maps)
    o = np.empty((B, 256, EXT), np.float32)
    for core in range(8):
        o[core // 2, 128 * (core % 2):128 * (core % 2) + 128] = res[core]["cvT"]
    return o


OBLK = [(i * 256, 256) for i in range(16)] + [(4096, 128)]


def fmv(dram, c0, n):
    return dram[:, c0:c0 + n].re("(t p) c -> p t c", p=128)


@functools.lru_cache(None)
def build_O1():
    k = K()
    xa = k.din("xa", [NLOC, D]); g1b = k.din("g1b", [128, 2, D])
    s5y = k.din("s5y", [256, NLOC]); glf = k.din("glf", [512, NLOC]); glb = k.din("glb", [512, NLOC])
    grs = k.din("grs", [512, NLOC], BF16); na = k.din("na", [256, NLOC], BF16); cv = k.din("cv", [256, NLOC])
    gs = k.din("gs", [4096, NLOC], BF16)
    w_glu = k.din("w_glu", [256, 256]); b_glu = k.din("b_glu", [128, 2]); w_s5o = k.din("w_s5o", [256, D])
    gng = k.din("gng", [128, 1]); w_glo = k.din("w_glo", [512, D]); w_nao = k.din("w_nao", [256, D])
    lng = k.din("lng", [128, 2]); lnb = k.din("lnb", [128, 2]); w_cvo = k.din("w_cvo", [256, D]); w_mix = k.din("w_mix", [D, D])
    x1 = k.dout("x1", [NLOC, D])

    stage = [k.sb([128, 1024]) for _ in range(2)]
    wi = [0]

    def load_cast(w_dram, kt, ncols, name):
        wb = k.sb([128, kt, ncols], BF16, name)
        for kk in range(kt):
            wi[0] += 1
            st = stage[wi[0] % 2]
            k.dma(st[:, :ncols], w_dram[kk * 128:(kk + 1) * 128, :])
            k.copy(wb[:, kk, :], st[:, :ncols], eng=("act" if wi[0] % 2 else "dve"))
        return wb

    wglu = load_cast(w_glu, 2, 256, "wglu"); ws5o = load_cast(w_s5o, 2, D, "ws5o"); wglo = load_cast(w_glo, 4, D, "wglo")
    wnao = load_cast(w_nao, 2, D, "wnao"); wcvo = load_cast(w_cvo, 2, D, "wcvo"); wmix = load_cast(w_mix, 8, D, "wmix")
    bglu = k.sb([128, 2]); gn = k.sb([128, 1]); lg = k.sb([128, 2]); lb = k.sb([128, 2]); g1t = k.sb([128, 2, D], name="g1t")
    k.dma(bglu[:], b_glu[:, :]); k.dma(gn[:], gng[:, :]); k.dma(lg[:], lng[:, :]); k.dma(lb[:], lnb[:, :]); k.dma(g1t[:], g1b[:, :, :])
    onesb = k.sb([128, 128], BF16); k.memset(onesb[:], 1.0)

    pss = [k.ps([128, 512]) for _ in range(8)]
    psi = [0]

    def nps():
        psi[0] += 1
        return pss[psi[0] % 8]

    R2 = range(2)
    NB = 256
    yt = [k.sb([128, 2, NB]) for _ in R2]; of_ = [k.sb([128, 4, NB]) for _ in R2]; ob_ = [k.sb([128, 4, NB]) for _ in R2]
    gr = [k.sb([128, 4, NB], BF16) for _ in R2]; cvt = [k.sb([128, 2, NB]) for _ in R2]; nat = [k.sb([128, 2, NB], BF16) for _ in R2]
    gst = k.sb([128, 32, NB], BF16, "gst")
    z = k.sb([128, 2, NB]); zb = k.sb([128, 2, NB], BF16); z2b = k.sb([128, 2, NB], BF16); sg = k.sb([128, NB])
    o_ = k.sb([128, 4, NB]); osq = k.sb([128, 4, NB], BF16); rs4 = k.sb([128, 4, NB]); of2 = k.sb([128, 4, NB], BF16)
    cvb = k.sb([128, 2, NB], BF16); cvq = k.sb([128, 2, NB], BF16); mean = k.sb([128, NB]); msq = k.sb([128, NB]); var = k.sb([128, NB])
    dd = k.sb([128, NB]); cvo = k.sb([128, 2, NB], BF16); tqs = [[k.sb([128, NB]) for _ in range(4)] for _ in range(2)]; mg = k.sb([128, 8, NB], BF16)
    xts = [k.sb([128, D]) for _ in R2]; xos = [k.sb([128, D]) for _ in R2]
    ti = 0
    for bi, (c0, n) in enumerate(OBLK):
        s = bi % 2
        k.dma(yt[s][:, :, :n], fmv(s5y, c0, n)); k.dma(of_[s][:, :, :n], fmv(glf, c0, n)); k.dma(ob_[s][:, :, :n], fmv(glb, c0, n))
        k.dma(gr[s][:, :, :n], fmv(grs, c0, n)); k.dma(cvt[s][:, :, :n], fmv(cv, c0, n)); k.dma(nat[s][:, :, :n], fmv(na, c0, n))
        k.dma(gst[:, :, :n], fmv(gs, c0, n))
        k.act(z[:, :, :n], yt[s][:, :, :n], AF.Gelu_apprx_tanh)
        k.copy(zb[:, :, :n], z[:, :, :n], eng="pool")
        for jt in R2:
            p = nps()
            for it_ in R2:
                k.mm(p[:, :n], wglu[:, it_, jt * 128:(jt + 1) * 128], zb[:, it_, :n], start=(it_ == 0), stop=(it_ == 1), inc=(it_ == 1))
            k.act(sg[:, :n], p[:, :n], AF.Sigmoid, bias=bglu[:, jt:jt + 1])
            k.tt(z2b[:, jt, :n], z[:, jt, :n], sg[:, :n], ALU.mult)
        k.tt(o_[:, :, :n], of_[s][:, :, :n], ob_[s][:, :, :n], ALU.add, eng="pool")
        k.tt(osq[:, :, :n], o_[:, :, :n], o_[:, :, :n], ALU.mult)
        pms = [nps(), nps()]
        for hh in range(4):
            k.mm(pms[hh // 2][:, (hh % 2) * NB:(hh % 2) * NB + n], onesb[:], osq[:, hh, :n], inc=(hh % 2 == 1))
        for j in R2:
            k.ts(rs4[:, 2 * j:2 * j + 2, :n], pms[j][:, :].re("p (h c) -> p h c", c=NB)[:, :, :n], 1.0 / 128, ALU.mult, EPS, ALU.add)
        k.act(rs4[:, :, :n], rs4[:, :, :n], AF.Sqrt)
        k.recip(rs4[:, :, :n], rs4[:, :, :n])
        k.tt(o_[:, :, :n], o_[:, :, :n], rs4[:, :, :n], ALU.mult)
        k.stt(of2[:, :, :n], o_[:, :, :n], gn[:, 0:1], gr[s][:, :, :n], ALU.mult, ALU.mult)
        k.copy(cvb[:, :, :n], cvt[s][:, :, :n], eng="pool")
        k.tt(cvq[:, :, :n], cvt[s][:, :, :n], cvt[s][:, :, :n], ALU.mult)
        p1 = nps(); p2 = nps()
        for t in R2:
            k.mm(p1[:, :n], onesb[:], cvb[:, t, :n], start=(t == 0), stop=(t == 1), inc=(t == 1))
        for t in R2:
            k.mm(p2[:, :n], onesb[:], cvq[:, t, :n], start=(t == 0), stop=(t == 1), inc=(t == 1))
        k.ts(mean[:, :n], p1[:, :n], 1.0 / 256, ALU.mult)
        k.tt(msq[:, :n], mean[:, :n], mean[:, :n], ALU.mult)
        k.stt(var[:, :n], p2[:, :n], 1.0 / 256, msq[:, :n], ALU.mult, ALU.subtract)
        k.ts(var[:, :n], var[:, :n], EPS, ALU.add)
        k.act(var[:, :n], var[:, :n], AF.Sqrt)
        k.recip(var[:, :n], var[:, :n])
        for t in R2:
            k.tt(dd[:, :n], cvt[s][:, t, :n], mean[:, :n], ALU.subtract)
            k.tt(dd[:, :n], dd[:, :n], var[:, :n], ALU.mult)
            k.act(cvo[:, t, :n], dd[:, :n], AF.Silu, scale=lg[:, t:t + 1], bias=lb[:, t:t + 1])
        srcs = [(ws5o, z2b, 2), (wglo, of2, 4), (wnao, nat[s], 2), (wcvo, cvo, 2)]
        for m in range(8):
            tq = tqs[m % 2]
            ps4 = [nps() for _ in range(4)]
            for i, (w_, a_, kt) in enumerate(srcs):
                for t in range(kt):
                    k.mm(ps4[i][:, :n], w_[:, t, m * 128:(m + 1) * 128], a_[:, t, :n], start=(t == 0), stop=(t == kt - 1), inc=(t == kt - 1))
            for i in range(4):
                k.tt(tq[i][:, :n], ps4[i][:, :n], gst[:, 8 * i + m, :n], ALU.mult)
            k.tt(tq[0][:, :n], tq[0][:, :n], tq[1][:, :n], ALU.add, eng="pool")
            k.tt(tq[2][:, :n], tq[2][:, :n], tq[3][:, :n], ALU.add, eng="pool")
            k.tt(mg[:, m, :n], tq[0][:, :n], tq[2][:, :n], ALU.add, eng="pool")
        for tt_ in range(n // 128):
            ti += 1
            tok0 = c0 + tt_ * 128
            xt, xo = xts[ti % 2], xos[ti % 2]
            k.dma(xt[:], xa[tok0:tok0 + 128, :])
            j = 1 if tok0 >= 4096 else 0
            for hf in R2:
                p = nps()
                for ft in range(8):
                    k.mm(p[:, :], mg[:, ft, tt_ * 128:(tt_ + 1) * 128], wmix[:, ft, hf * 512:(hf + 1) * 512], start=(ft == 0), stop=(ft == 7), inc=(ft == 7))
                k.tt(xo[:, hf * 512:(hf + 1) * 512], p[:, :], g1t[:, j, hf * 512:(hf + 1) * 512], ALU.mult)
            k.tt(xo[:], xo[:], xt[:], ALU.add, eng="pool")
            k.dma(x1[tok0:tok0 + 128, :], xo[:])
    return k.finish()


def scatter_fm(a, core):
    b, h = core // 2, core % 2
    return np.ascontiguousarray(np.concatenate([a[b][:, LC + h * 4096:LC + (h + 1) * 4096], a[b][:, h * 128:(h + 1) * 128]], 1))


def bc128(v):
    return np.broadcast_to(np.asarray(v, np.float32)[None], (128,) + np.asarray(v).shape)


def run_O1(xs, resA, y5, glf, glb, nao, cvo, mod_l, inp, l, cores=range(8)):
    maps = []
    for core in cores:
        b = core // 2
        g1b = np.ascontiguousarray(np.stack([bc128(mod_l[2, :, b]), bc128(mod_l[2, :, 4])], axis=1))
        maps.append({"xa": xs[core], "g1b": g1b, "s5y": scatter_fm(y5, core), "glf": scatter_fm(glf, core), "glb": scatter_fm(glb, core),
                     "grs": resA[core]["grsT"], "na": scatter_fm(nao, core), "cv": scatter_fm(cvo, core), "gs": resA[core]["gsT"],
                     "w_glu": inp["s5_w_glu"][l], "b_glu": np.ascontiguousarray(inp["s5_b_glu"][l].reshape(2, 128).T), "w_s5o": inp["s5_w_out"][l],
                     "gng": np.ascontiguousarray(inp["gla_norm_g"][l][:, None]), "w_glo": inp["gla_w_out"][l], "w_nao": inp["na_w_out"][l],
                     "lng": np.ascontiguousarray(inp["conv_ln_g"][l].reshape(2, 128).T), "lnb": np.ascontiguousarray(inp["conv_ln_b"][l].reshape(2, 128).T),
                     "w_cvo": inp["conv_w_out"][l], "w_mix": inp["w_mix_out"][l]})
    res = run(build_O1(), maps)
    return [r["x1"] for r in res]


O2PASS = [(0, 9), (9, 8), (17, 8), (25, 8)]


@functools.lru_cache(None)
def build_O2(last):
    k = K()
    x1 = k.din("x1", [NLOC, D]); modv = k.din("modv", [128, 8, 4]); g2n = k.din("g2n", [128, 8])
    g2b = k.din("g2b", [128, 2, D]); fgb = k.din("fgb", [128, D])
    wr = k.din("wr", [D, 36]); brb = k.din("brb", [128, 36])
    w1 = k.din("w1", [32, D, 256]); w3 = k.din("w3", [32, D, 256]); w2 = k.din("w2", [32, 256, D])
    ident = k.din("ident", [128, 128])
    out = k.dout("x2", [NLOC, D])

    idf = k.sb([128, 128]); k.dma(idf[:], ident[:, :])
    A_lat, B_lat, A_ctx, B_ctx = mod_prep(k, modv, g2n)
    wrt = k.sb([128, 8, 36]); brt = k.sb([128, 36]); g2t = k.sb([128, 2, D], name="g2t"); fgt = k.sb([128, D], name="fgt")
    k.dma(wrt[:], wr[:, :].re("(k p) c -> p k c", p=128)); k.dma(brt[:], brb[:, :]); k.dma(g2t[:], g2b[:, :, :]); k.dma(fgt[:], fgb[:, :])
    pss = [k.ps([128, 512]) for _ in range(8)]
    psi = [0]

    def nps():
        psi[0] += 1
        return pss[psi[0] % 8]

    R2 = range(2)
    MT = 9
    hT = k.sb([128, 8, MT * 128], BF16, "o2_hT"); yacc = k.sb([128, MT, D], name="o2_yacc"); comb = k.sb([128, 33, 32], name="o2_comb")
    st1 = k.sb([128, 8, 256], name="st1"); st3 = k.sb([128, 8, 256], name="st3"); st2 = k.sb([128, 2, D], name="st2")
    w1b = [k.sb([128, 8, 256], BF16) for _ in R2]; w3b = [k.sb([128, 8, 256], BF16) for _ in R2]; w2b = [k.sb([128, 2, D], BF16) for _ in R2]
    sa = [k.sb([128, 512]) for _ in R2]; actb = [[k.sb([128, 512], BF16) for _ in R2] for _ in R2]
    xt = [k.sb([128, D]) for _ in R2]; xn = k.sb([128, D]); junk = k.sb([128, D], BF16); tmpf = k.sb([128, 4, 128]); h32 = k.sb([128, 8, 128])
    yo = k.sb([128, D]); yo2 = k.sb([128, D])
    ss = k.sb([128, 1]); rr = k.sb([128, 1])
    lg = k.sb([128, 36]); gm = k.sb([128, 1]); ngm = k.sb([128, 1]); eg = k.sb([128, 4]); sgm = k.sb([128, 1]); gp = k.sb([128, 1])
    ohg = k.sb([128, 4]); t48 = k.sb([128, 4, 8]); sel = k.sb([128, 8]); sel2 = k.sb([128, 8]); m1 = k.sb([128, 1]); m2 = k.sb([128, 1])
    oh1 = k.sb([128, 8]); oh2 = k.sb([128, 8]); d21 = k.sb([128, 1]); e21 = k.sb([128, 1]); den = k.sb([128, 1]); wa = k.sb([128, 1]); wb_ = k.sb([128, 1])
    sw = k.sb([128, 8])

    def rms(src):
        k.act(junk[:], src, AF.Square, accum=ss[:])
        k.ts(rr[:], ss[:], 1.0 / D, ALU.mult, EPS, ALU.add)
        k.act(rr[:], rr[:], AF.Sqrt)
        k.recip(rr[:], rr[:])

    xi = 0
    for (p0, nt) in O2PASS:
        for li in range(nt):
            gi = p0 + li
            xi += 1
            x_ = xt[xi % 2]
            k.dma(x_[:], x1[gi * 128:(gi + 1) * 128, :])
            rms(x_[:])
            k.ts(xn[:], x_[:], rr[:, 0:1], ALU.mult)
            A_, B_ = (A_ctx, B_ctx) if gi == 32 else (A_lat, B_lat)
            for half in R2:
                tp = nps()
                for kk in range(4):
                    k.tr(tp[:, kk * 128:(kk + 1) * 128], xn[:, (half * 4 + kk) * 128:(half * 4 + kk + 1) * 128], idf[:], inc=(kk == 3))
                k.tt(tmpf[:], tp[:, :].re("p (k t) -> p k t", t=128),
                     View(A_, A_.t[:, half * 4:half * 4 + 4].unsqueeze(2).to_broadcast([128, 4, 128])), ALU.mult)
                k.tt(h32[:, half * 4:half * 4 + 4, :], tmpf[:],
                     View(B_, B_.t[:, half * 4:half * 4 + 4].unsqueeze(2).to_broadcast([128, 4, 128])), ALU.add)
            k.copy(hT[:, :, li * 128:(li + 1) * 128], h32[:], eng="pool")
            pr = nps()
            for kk in range(8):
                k.mm(pr[:, 0:36], h32[:, kk, :], wrt[:, kk, :], start=(kk == 0), stop=(kk == 7), inc=(kk == 7))
            k.tt(lg[:], pr[:, 0:36], brt[:], ALU.add)
            k.reduce(gm[:], lg[:, 0:4], ALU.max)
            k.ts(ngm[:], gm[:], -1.0, ALU.mult)
            k.act(eg[:], lg[:, 0:4], AF.Exp, bias=ngm[:, 0:1], accum=sgm[:])
            k.recip(gp[:], sgm[:])
            k.ts(ohg[:], lg[:, 0:4], gm[:, 0:1], ALU.is_equal)
            ohb = View(ohg, ohg.t[:, :].unsqueeze(2).to_broadcast([128, 4, 8]))
            k.tt(t48[:], lg[:, 4:36].re("p (g e) -> p g e", e=8), ohb, ALU.mult)
            k.reduce(sel[:], t48[:, :, :].re("p g e -> p e g"), ALU.add)
            k.reduce(m1[:], sel[:], ALU.max)
            k.ts(oh1[:], sel[:], m1[:, 0:1], ALU.is_equal)
            k.stt(sel2[:], oh1[:], -1.0e30, sel[:], ALU.mult, ALU.add)
            k.reduce(m2[:], sel2[:], ALU.max)
            k.ts(oh2[:], sel2[:], m2[:, 0:1], ALU.is_equal)
            k.tt(d21[:], m2[:], m1[:], ALU.subtract)
            k.act(e21[:], d21[:], AF.Exp)
            k.ts(den[:], e21[:], 1.0, ALU.add)
            k.recip(den[:], den[:])
            k.tt(wa[:], den[:], gp[:], ALU.mult)
            k.tt(wb_[:], wa[:], e21[:], ALU.mult)
            k.ts(sw[:], oh1[:], wa[:, 0:1], ALU.mult)
            k.stt(sw[:], oh2[:], wb_[:, 0:1], sw[:], ALU.mult, ALU.add)
            k.tt(comb[:, gi, :].re("p (g e) -> p g e", e=8), ohb, View(sw, sw.t[:, :].unsqueeze(1).to_broadcast([128, 4, 8])), ALU.mult)
        blocks = [(t0, min(4, nt - t0)) for t0 in range(0, nt, 4)]
        bi = 0
        pend = None
        for e in range(32):
            ws = e % 2
            k.dma(st1[:], w1[e].re("(k p) f -> p k f", p=128)); k.dma(st3[:], w3[e].re("(k p) f -> p k f", p=128))
            k.dma(st2[:], w2[e].re("(k p) f -> p k f", p=128))
            k.copy(w1b[ws][:], st1[:], eng="act"); k.copy(w3b[ws][:], st3[:], eng="pool"); k.copy(w2b[ws][:], st2[:], eng="pool")
            for (bt0, bnt) in blocks:
                bi += 1
                n = bnt * 128; c0 = bt0 * 128
                for ft in R2:
                    pa = nps(); pb = nps()
                    for kk in range(8):
                        k.mm(pa[:, :n], w1b[ws][:, kk, ft * 128:(ft + 1) * 128], hT[:, kk, c0:c0 + n], start=(kk == 0), stop=(kk == 7), inc=(kk == 7))
                    for kk in range(8):
                        k.mm(pb[:, :n], w3b[ws][:, kk, ft * 128:(ft + 1) * 128], hT[:, kk, c0:c0 + n], start=(kk == 0), stop=(kk == 7), inc=(kk == 7))
                    s_ = sa[ft]
                    k.act(s_[:, :n], pa[:, :n], AF.Silu)
                    k.tt(actb[bi % 2][ft][:, :n], pb[:, :n], s_[:, :n], ALU.mult)
                if pend is not None:
                    pend()

                def pend(e=e, ws=ws, bi=bi, bt0=bt0, bnt=bnt):
                    for t in range(bnt):
                        gi = p0 + bt0 + t
                        for hf in R2:
                            py = nps()
                            for ft in R2:
                                k.mm(py[:, :], actb[bi % 2][ft][:, t * 128:(t + 1) * 128], w2b[ws][:, ft, hf * 512:(hf + 1) * 512], start=(ft == 0), stop=(ft == 1), inc=(ft == 1))
                            ya = yacc[:, bt0 + t, hf * 512:(hf + 1) * 512]
                            if e == 0:
                                k.ts(ya, py[:, :], comb[:, gi, e:e + 1], ALU.mult)
                            else:
                                k.stt(ya, py[:, :], comb[:, gi, e:e + 1], ya, ALU.mult, ALU.add)
        pend()
        for li in range(nt):
            gi = p0 + li
            xi += 1
            x_ = xt[xi % 2]
            k.dma(x_[:], x1[gi * 128:(gi + 1) * 128, :])
            j = 1 if gi == 32 else 0
            k.tt(yo[:], yacc[:, li, :], g2t[:, j, :], ALU.mult)
            k.tt(yo[:], yo[:], x_[:], ALU.add, eng="pool")
            if last:
                rms(yo[:])
                k.stt(yo2[:], yo[:], rr[:, 0:1], fgt[:], ALU.mult, ALU.mult)
                k.dma(out[gi * 128:(gi + 1) * 128, :], yo2[:])
            else:
                k.dma(out[gi * 128:(gi + 1) * 128, :], yo[:])
    return k.finish()


def run_O2(x1s, mod_l, inp, l, last, cores=range(8)):
    wr = np.ascontiguousarray(np.concatenate([inp["moe_w_group"][l], inp["moe_w_expert"][l]], axis=1))
    brb = np.ascontiguousarray(bc128(np.concatenate([inp["moe_b_group"][l], inp["moe_b_expert"][l]])))
    w1 = inp["moe_w1"][l].reshape(32, D, 256); w3 = inp["moe_w3"][l].reshape(32, D, 256); w2 = inp["moe_w2"][l].reshape(32, 256, D)
    fgb = np.ascontiguousarray(bc128(inp["final_norm_g"])); ident = np.eye(128, dtype=np.float32)
    maps = []
    for i, core in enumerate(cores):
        b = core // 2
        modv = np.stack([pk(mod_l[3, :, b]), pk(mod_l[4, :, b]), pk(mod_l[3, :, 4]), pk(mod_l[4, :, 4])], axis=2)
        g2b = np.ascontiguousarray(np.stack([bc128(mod_l[5, :, b]), bc128(mod_l[5, :, 4])], axis=1))
        maps.append({"x1": x1s[i], "modv": np.ascontiguousarray(modv), "g2n": pk(inp["norm2_g"][l]), "g2b": g2b, "fgb": fgb,
                     "wr": wr, "brb": brb, "w1": w1, "w3": w3, "w2": w2, "ident": ident})
    res = run(build_O2(bool(last)), maps)
    return [r["x2"] for r in res]


def unshard(x2s):
    x = np.empty((B, L, D), np.float32); xc = np.empty((B, LC, D), np.float32)
    for core in range(8):
        b, h = core // 2, core % 2
        x[b, h * 4096:(h + 1) * 4096] = x2s[core][:4096]; xc[b, h * 128:(h + 1) * 128] = x2s[core][4096:]
    return x, xc


def kernel(**inp):
    inp = {k_: np.asarray(v_) for k_, v_ in inp.items()}
    mod = run_M(inp)
    x, xc = inp["x"], inp["ctx"]
    for l in range(2):
        xs = shard_tokens(x, xc)
        resA = run_A(x, xc, mod[l], inp, l)
        y5 = run_S5(gather_fm(resA, "uT"), inp, l)
        glf, glb = run_GLA(gather_fm(resA, "gqT"), gather_fm(resA, "gkT"), gather_fm(resA, "gaT"), gather_tm(resA, "gv"), inp, l)
        nao = run_NA(gather_fm(resA, "nqT"), gather_fm(resA, "nkT"), gather_tm(resA, "nv"), inp, l)
        cvo = run_CV(gather_fm(resA, "cyT"), inp, l)
        x1s = run_O1(xs, resA, y5, glf, glb, nao, cvo, mod[l], inp, l)
        del resA, y5, glf, glb, nao, cvo
        x2s = run_O2(x1s, mod[l], inp, l, l == 1)
        x, xc = unshard(x2s)
    return x
```

```python
import functools
import numpy as np
import ml_dtypes
import concourse.bass as bass
import concourse.mybir as mybir
from concourse.bass_utils import run_bass_kernel_spmd
from contextlib import ExitStack

F32 = mybir.dt.float32
BF16 = mybir.dt.bfloat16
AF = mybir.ActivationFunctionType
ALU = mybir.AluOpType
AX = mybir.AxisListType
NPBF = ml_dtypes.bfloat16
PI = float(np.pi)

NCORE = 8
D = 1024
B = 4
L = 8192
LC = 256
NLOC = 4096 + 128
EXT = LC + L
EPS = 1e-6


class View:
    __slots__ = ("b", "ap")

    def __init__(self, b, ap):
        self.b = b
        self.ap = ap

    def __getitem__(self, k):
        return View(self.b, self.ap[k])

    def bc(self, shape):
        return View(self.b, self.ap.to_broadcast(list(shape)))

    def re(self, s, **kw):
        return View(self.b, self.ap.rearrange(s, **kw))


class Buf:
    __slots__ = ("t", "name", "lw", "rd", "dsem", "dcnt", "dram")

    def __init__(self, t, name, dram=False):
        self.t = t
        self.name = name
        self.lw = None
        self.rd = []
        self.dsem = None
        self.dcnt = 0
        self.dram = dram

    def __getitem__(self, k):
        return View(self, self.t[k])


def _v(x):
    return x.ap if isinstance(x, View) else x


class K:
    def __init__(self):
        self.nc = bass.Bass("TRN2", target_bir_lowering=False)
        self.es = ExitStack()
        nc = self.nc
        self.eng = {"pe": nc.tensor, "act": nc.scalar, "dve": nc.vector, "pool": nc.gpsimd, "sp": nc.sync}
        self.sem = {}
        self.cnt = {}
        self.semobj = {}
        for e in self.eng:
            s = self.es.enter_context(nc.semaphore("s_" + e))
            self.sem[e] = s
            self.cnt[e] = 0
            self.semobj[("E", e)] = s
        self.waited = {e: {} for e in self.eng}
        self.nb = 0
        self.ninst = 0
        self.dbufs = []
        self.stk = [self.es]

    def sb(self, shape, dt=F32, name=None):
        self.nb += 1
        name = name or f"sb{self.nb}"
        return Buf(self.stk[-1].enter_context(self.nc.sbuf_tensor(name, list(shape), dt)), name)

    def barrier(self):
        deps = [(("E", e), self.cnt[e]) for e in self.eng] + [(b.dsem, b.dcnt) for b in self.dbufs]
        for e in self.eng:
            self._wait(e, deps)

    def push(self):
        self.stk.append(ExitStack())

    def pop(self):
        self.barrier()
        self.stk.pop().close()

    def ps(self, shape, dt=F32, name=None):
        self.nb += 1
        name = name or f"ps{self.nb}"
        return Buf(self.es.enter_context(self.nc.psum_tensor(name, list(shape), dt)), name)

    def din(self, name, shape, dt=F32):
        return Buf(self.nc.dram_tensor(name, list(shape), dt, kind="ExternalInput").ap(), name, dram=True)

    def dout(self, name, shape, dt=F32):
        return Buf(self.nc.dram_tensor(name, list(shape), dt, kind="ExternalOutput").ap(), name, dram=True)

    def _wait(self, e, deps):
        eng = self.eng[e]
        w = self.waited[e]
        best = {}
        for d in deps:
            if d is None:
                continue
            kk, v = d
            if kk == ("E", e) and v > self.cnt[e]:
                v = self.cnt[e]
            if best.get(kk, 0) < v:
                best[kk] = v
        for kk, v in best.items():
            if w.get(kk, 0) < v:
                eng.wait_ge(self.semobj[kk], v)
                w[kk] = v

    def op(self, e, fn, reads=(), writes=(), inc=True):
        reads = [r.b if isinstance(r, View) else r for r in reads if r is not None and not isinstance(r, (int, float))]
        writes = [r.b if isinstance(r, View) else r for r in writes]
        deps = []
        for b in reads:
            deps.append(b.lw)
        for b in writes:
            deps.append(b.lw)
            deps.extend(b.rd)
        self._wait(e, deps)
        inst = fn(self.eng[e])
        self.ninst += 1
        if inc:
            self.cnt[e] += 1
            inst.then_inc(self.sem[e], 1)
            tk = (("E", e), self.cnt[e])
        else:
            tk = (("E", e), self.cnt[e] + 1)
        for b in writes:
            b.lw = tk
            b.rd = []
        for b in reads:
            if b not in writes and not b.dram:
                b.rd.append(tk)
        return inst

    def dma(self, out, in_, q="sp", chain=False, **kw):
        ob, ib = out.b, in_.b
        own = ib if ob.dram else ob
        if own.dsem is None:
            s = self.es.enter_context(self.nc.semaphore("d_" + own.name))
            own.dsem = ("D", own.name)
            self.semobj[own.dsem] = s
            self.dbufs.append(own)
        key = own.dsem
        deps = [ib.lw]
        ch = chain and ob.lw is not None and ob.lw[0] == key
        if not ch:
            deps.append(ob.lw)
        deps.extend(ob.rd)
        self._wait(q, deps)
        inst = self.eng[q].dma_start(out=out.ap, in_=in_.ap, **kw)
        self.ninst += 1
        own.dcnt += 16
        inst.then_inc(self.semobj[key], 16)
        tk = (key, own.dcnt)
        if not ob.dram:
            ob.lw = tk
            if not ch:
                ob.rd = []
        if not ib.dram:
            ib.rd.append(tk)
        return inst

    def finish(self, q="sp"):
        self._wait(q, [(b.dsem, b.dcnt) for b in self.dbufs])
        self.es.close()
        return self.nc

    def mm(self, out, lhsT, rhs, start=True, stop=True, inc=True):
        return self.op("pe", lambda e: e.matmul(out.ap, lhsT=lhsT.ap, rhs=rhs.ap, start=start, stop=stop),
                       reads=[lhsT, rhs] + ([] if start else [out]), writes=[out], inc=inc)

    def tr(self, out, in_, ident, inc=True):
        return self.op("pe", lambda e: e.transpose(out.ap, in_.ap, ident.ap), reads=[in_, ident], writes=[out], inc=inc)

    def act(self, out, in_, func, bias=None, scale=None, accum=None, eng="act"):
        kw = {}
        if bias is not None:
            kw["bias"] = _v(bias)
        if scale is not None:
            kw["scale"] = _v(scale)
        if accum is not None:
            kw["accum_out"] = accum.ap
        return self.op(eng, lambda e: e.activation(out=out.ap, in_=in_.ap, func=func, **kw),
                       reads=[in_, bias, scale], writes=[out] + ([accum] if accum is not None else []))

    def tt(self, out, in0, in1, op, eng="dve"):
        return self.op(eng, lambda e: e.tensor_tensor(out=out.ap, in0=in0.ap, in1=in1.ap, op=op), reads=[in0, in1], writes=[out])

    def ts(self, out, in0, s1, op0, s2=None, op1=None, eng="dve"):
        if op1 is None:
            return self.op(eng, lambda e: e.tensor_scalar(out=out.ap, in0=in0.ap, scalar1=_v(s1), scalar2=None, op0=op0),
                           reads=[in0, s1], writes=[out])
        return self.op(eng, lambda e: e.tensor_scalar(out=out.ap, in0=in0.ap, scalar1=_v(s1), scalar2=_v(s2), op0=op0, op1=op1),
                       reads=[in0, s1, s2], writes=[out])

    def stt(self, out, in0, scalar, in1, op0, op1, eng="dve"):
        return self.op(eng, lambda e: e.scalar_tensor_tensor(out=out.ap, in0=in0.ap, scalar=_v(scalar), in1=in1.ap, op0=op0, op1=op1),
                       reads=[in0, scalar, in1], writes=[out])

    def scan(self, out, d0, d1, initial, op0=ALU.mult, op1=ALU.add):
        return self.op("dve", lambda e: e.tensor_tensor_scan(out=out.ap, data0=d0.ap, data1=d1.ap, initial=_v(initial), op0=op0, op1=op1),
                       reads=[d0, d1, initial], writes=[out])

    def copy(self, out, in_, eng="dve"):
        if eng == "act":
            return self.act(out, in_, AF.Copy)
        return self.op(eng, lambda e: e.tensor_copy(out=out.ap, in_=in_.ap), reads=[in_], writes=[out])

    def memset(self, out, val, eng="dve"):
        return self.op(eng, lambda e: e.memset(out.ap, val), writes=[out])

    def recip(self, out, in_):
        return self.op("dve", lambda e: e.reciprocal(out=out.ap, in_=in_.ap), reads=[in_], writes=[out])

    def reduce(self, out, in_, op, axis=AX.X):
        return self.op("dve", lambda e: e.tensor_reduce(out=out.ap, in_=in_.ap, axis=axis, op=op), reads=[in_], writes=[out])

    def wrap(self, out, in_, shift):
        return self.op("dve", lambda e: e.add_range_wrap(out=out.ap, in_=in_.ap, shift=shift, bound=PI, period=2 * PI),
                       reads=[in_], writes=[out])


def run(nc, in_maps):
    return run_bass_kernel_spmd(nc, in_maps, core_ids=list(range(len(in_maps)))).results


@functools.lru_cache(None)
def build_M():
    k = K()
    cT = k.din("cT", [128, 8, 8]); w = k.din("w", [1024, 1536]); bm = k.din("bm", [128, 12])
    out = k.dout("mod", [128, 12, 8])
    ct = k.sb([128, 8, 8]); ca = k.sb([128, 8, 8]); wt = k.sb([128, 8, 1536]); bt = k.sb([128, 12]); ot = k.sb([128, 12, 8])
    ps = [k.ps([128, 8]) for _ in range(2)]
    k.dma(ct[:], cT[:, :, :]); k.dma(bt[:], bm[:, :])
    wv = w[:, :].re("(k p) c -> p k c", p=128)
    for kk in range(8):
        k.dma(wt[:, kk, :], wv[:, kk, :], chain=True)
    k.act(ca[:], ct[:], AF.Silu)
    for m in range(12):
        p = ps[m % 2]
        for kk in range(8):
            k.mm(p[:], wt[:, kk, m * 128:(m + 1) * 128], ca[:, kk, :], start=(kk == 0), stop=(kk == 7), inc=(kk == 7))
        k.act(ot[:, m, :], p[:], AF.Identity, bias=bt[:, m:m + 1])
    k.dma(out[:, :, :], ot[:])
    return k.finish()


def run_M(inp):
    c, c_ctx, w_mod, b_mod = inp["c"], inp["c_ctx"], inp["w_mod"], inp["b_mod"]
    cc = np.zeros((8, D), np.float32); cc[:4] = c; cc[4] = c_ctx
    cT = np.ascontiguousarray(cc.T.reshape(8, 128, 8).transpose(1, 0, 2))
    maps = []
    for core in range(8):
        l, q = core // 4, core % 4
        maps.append({"cT": cT, "w": np.ascontiguousarray(w_mod[l][:, q * 1536:(q + 1) * 1536]),
                     "bm": np.ascontiguousarray(b_mod[l][q * 1536:(q + 1) * 1536].reshape(12, 128).T)})
    res = run(build_M(), maps)
    mod = np.zeros((2, 6 * D, 8), np.float32)
    for core in range(8):
        l, q = core // 4, core % 4
        mod[l, q * 1536:(q + 1) * 1536] = res[core]["mod"].transpose(1, 0, 2).reshape(1536, 8)
    return mod.reshape(2, 6, D, 8)


def pk(v):
    return np.ascontiguousarray(np.asarray(v, np.float32).reshape(8, 128).T)


TOKBLK = [(i * 512, 512) for i in range(8)] + [(4096, 128)]
OFF = dict(s5_u=0, gla_q=256, gla_k=512, gla_v=768, gla_r=1280, gla_a=1792, na_q=1824, na_k=2080, na_v=2336, conv_in=2592, gates=3104)


def norm_mod_T(k, x_dram, ntile, A_lat, B_lat, A_ctx, B_ctx, hT, ident_b, ps_t, hT32=None, ident_f=None, after_tile=None):
    xt = [k.sb([128, 1024]) for _ in range(2)]
    xn = [k.sb([128, 1024], BF16 if hT32 is None else F32) for _ in range(2)]
    junk = k.sb([128, 1024], BF16)
    tmpf = k.sb([128, 8, 128])
    ss = [k.sb([128, 1]) for _ in range(2)]
    rs = [k.sb([128, 1]) for _ in range(2)]
    for i in range(ntile):
        x_, n_, s_, r_ = xt[i % 2], xn[i % 2], ss[i % 2], rs[i % 2]
        k.dma(x_[:], x_dram[i * 128:(i + 1) * 128, :])
        k.act(junk[:], x_[:], AF.Square, accum=s_[:])
        k.ts(r_[:], s_[:], 1.0 / D, ALU.mult, EPS, ALU.add)
        k.act(r_[:], r_[:], AF.Sqrt)
        k.recip(r_[:], r_[:])
        k.ts(n_[:], x_[:], r_[:, 0:1], ALU.mult)
        tp = ps_t[i % 2]
        for kk in range(8):
            k.tr(tp[:, kk, :], n_[:, kk * 128:(kk + 1) * 128], (ident_b if hT32 is None else ident_f)[:], inc=(kk == 7))
        A_, B_ = (A_ctx, B_ctx) if i == ntile - 1 else (A_lat, B_lat)
        k.tt(tmpf[:], tp[:], View(A_, A_.t[:, :].unsqueeze(2).to_broadcast([128, 8, 128])), ALU.mult)
        Bb = View(B_, B_.t[:, :].unsqueeze(2).to_broadcast([128, 8, 128]))
        if hT32 is None:
            k.tt(hT[:, :, i * 128:(i + 1) * 128], tmpf[:], Bb, ALU.add, eng="pool")
        else:
            k.tt(hT32[:], tmpf[:], Bb, ALU.add)
            k.copy(hT[:, :, i * 128:(i + 1) * 128], hT32[:], eng="pool")
            after_tile(i)


def mod_prep(k, modv, gn):
    mt = k.sb([128, 8, 4]); gt = k.sb([128, 8])
    k.dma(mt[:], modv[:, :, :]); k.dma(gt[:], gn[:, :])
    res = []
    for j in (0, 2):
        A = k.sb([128, 8]); Bv = k.sb([128, 8])
        k.ts(A[:], mt[:, :, j + 1], 1.0, ALU.add)
        k.tt(A[:], A[:], gt[:], ALU.mult)
        k.copy(Bv[:], mt[:, :, j])
        res += [A, Bv]
    return res


@functools.lru_cache(None)
def build_A():
    k = K()
    xa = k.din("xa", [NLOC, D]); modv = k.din("modv", [128, 8, 4]); g1n = k.din("g1n", [128, 8])
    w_in = k.din("w_in", [D, 7200]); gate_b = k.din("gate_b", [128, 32])
    ropeC = k.din("ropeC", [128, NLOC]); ropeS = k.din("ropeS", [128, NLOC]); ident = k.din("ident", [128, 128])
    o_uT = k.dout("uT", [256, NLOC]); o_gq = k.dout("gqT", [256, NLOC]); o_gk = k.dout("gkT", [256, NLOC])
    o_ga = k.dout("gaT", [32, NLOC]); o_gr = k.dout("grsT", [512, NLOC], BF16); o_gv = k.dout("gv", [NLOC, 512], BF16)
    o_nq = k.dout("nqT", [256, NLOC], BF16); o_nk = k.dout("nkT", [256, NLOC], BF16); o_nv = k.dout("nv", [NLOC, 256], BF16)
    o_cy = k.dout("cyT", [256, NLOC], BF16); o_gs = k.dout("gsT", [4096, NLOC], BF16)

    hT = k.sb([128, 8, NLOC], BF16, "hT")
    idf = k.sb([128, 128]); idb = k.sb([128, 128], BF16)
    k.dma(idf[:], ident[:, :]); k.copy(idb[:], idf[:])
    A_lat, B_lat, A_ctx, B_ctx = mod_prep(k, modv, g1n)
    gb = k.sb([128, 32]); k.dma(gb[:], gate_b[:, :])
    rc = k.sb([128, NLOC]); rsn = k.sb([128, NLOC])
    k.dma(rc[:], ropeC[:, :]); k.dma(rsn[:], ropeS[:, :])
    ps_t = [k.ps([128, 8, 128], BF16) for _ in range(2)]
    norm_mod_T(k, xa, 33, A_lat, B_lat, A_ctx, B_ctx, hT, idb, ps_t)

    pss = [k.ps([128, 512]) for _ in range(6)]
    psi = [0]

    def nps():
        psi[0] += 1
        return pss[psi[0] % 6]

    wf = [k.sb([128, 8, 512]) for _ in range(2)]
    wq = k.sb([128, 8, 256])
    wbq = k.sb([128, 8, 256], BF16)
    wb = [k.sb([128, 8, 512], BF16) for _ in range(3)]
    wi = [0]
    stf = [k.sb([128, 512]) for _ in range(3)]
    stb = [k.sb([128, 512], BF16) for _ in range(3)]
    t1 = k.sb([128, 512]); t2 = k.sb([128, 512])
    si = [0]

    def load_w(col0, ncols, scale=None, swap=False):
        wi[0] += 1
        f = wq if swap else wf[wi[0] % 2]
        b = wbq if swap else wb[wi[0] % 3]
        for kk in range(8):
            k.dma(f[:, kk, :ncols], w_in[kk * 128:(kk + 1) * 128, col0:col0 + ncols], chain=True)
        if swap:
            sc = 1.0 if scale is None else scale
            fv = f[:, :, :].re("p k (g t c) -> p (k g) t c", t=2, c=16)
            bv = b[:, :, :].re("p k (g t c) -> p (k g) t c", t=2, c=16)
            k.act(bv[:, :, 0, :], fv[:, :, 1, :], AF.Copy, scale=-sc)
            k.act(bv[:, :, 1, :], fv[:, :, 0, :], AF.Copy, scale=sc)
        elif scale is not None:
            k.act(b[:, :, :ncols], f[:, :, :ncols], AF.Copy, scale=scale)
        else:
            k.copy(b[:, :, :ncols], f[:, :, :ncols], eng=("dve" if wi[0] % 2 else "pool"))
        return b

    def fm_mm(b, c0, M, tok0, n):
        p = nps()
        for kk in range(8):
            k.mm(p[:M, :n], b[:, kk, c0:c0 + M], hT[:, kk, tok0:tok0 + n], start=(kk == 0), stop=(kk == 7), inc=(kk == 7))
        return p

    def fm_simple(col0, ncols, out, post, bf, scale=None):
        for g0 in range(0, ncols, 512):
            gn = min(512, ncols - g0)
            b = load_w(col0 + g0, gn, scale=scale)
            for c0 in range(0, gn, 128):
                M = min(128, gn - c0)
                for (tok0, n) in TOKBLK:
                    p = fm_mm(b, c0, M, tok0, n)
                    si[0] += 1
                    st = (stb if bf else stf)[si[0] % 3]
                    post(st[:M, :n], p[:M, :n], (g0 + c0) // 128)
                    k.dma(out[g0 + c0:g0 + c0 + M, tok0:tok0 + n], st[:M, :n])

    def tm_part(col0, ncols, out):
        b = load_w(col0, ncols)
        for i in range(33):
            p = nps()
            for kk in range(8):
                k.mm(p[:, :ncols], hT[:, kk, i * 128:(i + 1) * 128], b[:, kk, :ncols], start=(kk == 0), stop=(kk == 7), inc=(kk == 7))
            si[0] += 1
            st = stb[si[0] % 3]
            k.copy(st[:, :ncols], p[:, :ncols], eng=("act" if i % 2 else "dve"))
            k.dma(out[i * 128:(i + 1) * 128, :], st[:, :ncols])

    def rope_part(col0, out, scale):
        b = load_w(col0, 256, scale=scale)
        bs = load_w(col0, 256, scale=scale, swap=True)
        for c0 in (0, 128):
            for (tok0, n) in TOKBLK:
                p = fm_mm(b, c0, 128, tok0, n)
                p2 = fm_mm(bs, c0, 128, tok0, n)
                si[0] += 1
                st = stf[si[0] % 3]
                k.tt(t1[:, :n], p[:, :n], rc[:, tok0:tok0 + n], ALU.mult)
                k.tt(t2[:, :n], p2[:, :n], rsn[:, tok0:tok0 + n], ALU.mult)
                k.tt(st[:, :n], t1[:, :n], t2[:, :n], ALU.add, eng="pool")
                k.dma(out[c0:c0 + 128, tok0:tok0 + n], st[:, :n])

    cp = [0]

    def post_copy(st, p, t):
        cp[0] += 1
        k.copy(st, p, eng=("act" if cp[0] % 2 else "dve"))

    fm_simple(OFF["s5_u"], 256, o_uT, post_copy, False)
    rope_part(OFF["gla_q"], o_gq, 0.125)
    rope_part(OFF["gla_k"], o_gk, None)
    tm_part(OFF["gla_v"], 512, o_gv)
    fm_simple(OFF["gla_r"], 512, o_gr, lambda st, p, t: k.act(st, p, AF.Silu), True)
    fm_simple(OFF["gla_a"], 32, o_ga, post_copy, False)
    fm_simple(OFF["na_q"], 256, o_nq, post_copy, True, scale=0.125)
    fm_simple(OFF["na_k"], 256, o_nk, post_copy, True)
    tm_part(OFF["na_v"], 256, o_nv)
    b = load_w(OFF["conv_in"], 512)
    for ct in range(2):
        for (tok0, n) in TOKBLK:
            pv = fm_mm(b, ct * 128, 128, tok0, n)
            pg = fm_mm(b, 256 + ct * 128, 128, tok0, n)
            si[0] += 1
            st = stb[si[0] % 3]
            k.act(t1[:, :n], pg[:, :n], AF.Sigmoid)
            k.tt(st[:, :n], pv[:, :n], t1[:, :n], ALU.mult)
            k.dma(o_cy[ct * 128:(ct + 1) * 128, tok0:tok0 + n], st[:, :n])
    for g0 in range(0, 4096, 512):
        b = load_w(OFF["gates"] + g0, 512)
        for c0 in range(0, 512, 128):
            t = (g0 + c0) // 128
            for (tok0, n) in TOKBLK:
                p = fm_mm(b, c0, 128, tok0, n)
                si[0] += 1
                st = stb[si[0] % 3]
                k.act(st[:, :n], p[:, :n], AF.Sigmoid, bias=gb[:, t:t + 1])
                k.dma(o_gs[t * 128:(t + 1) * 128, tok0:tok0 + n], st[:, :n])
    return k.finish()


def rope_tables():
    inv = (10000.0 ** (-np.arange(0, 32, 2, dtype=np.float32) / 32)).astype(np.float32)
    tabs = []
    for half in range(2):
        t = np.arange(4096) + half * 4096
        row = (t // 64).astype(np.float32); col = (t % 64).astype(np.float32)
        ang = np.zeros((64, 4096), np.float32)
        ang[0:16] = inv[:, None] * row[None]; ang[16:32] = ang[0:16]
        ang[32:48] = inv[:, None] * col[None]; ang[48:64] = ang[32:48]
        c = np.ones((128, NLOC), np.float32); s = np.zeros((128, NLOC), np.float32)
        c[:, :4096] = np.tile(np.cos(ang), (2, 1)); s[:, :4096] = np.tile(np.sin(ang), (2, 1))
        tabs.append((c, s))
    return tabs


def shard_tokens(x, xc):
    out = []
    for core in range(8):
        b, h = core // 2, core % 2
        out.append(np.ascontiguousarray(np.concatenate([x[b, h * 4096:(h + 1) * 4096], xc[b, h * 128:(h + 1) * 128]], 0)))
    return out


def gather_fm(res, name):
    C = res[0][name].shape[0]
    out = np.empty((B, C, EXT), res[0][name].dtype)
    for core in range(8):
        b, h = core // 2, core % 2
        a = res[core][name]
        out[b, :, LC + h * 4096:LC + (h + 1) * 4096] = a[:, :4096]
        out[b, :, h * 128:(h + 1) * 128] = a[:, 4096:]
    return out


def gather_tm(res, name):
    C = res[0][name].shape[1]
    out = np.empty((B, EXT, C), res[0][name].dtype)
    for core in range(8):
        b, h = core // 2, core % 2
        a = res[core][name]
        out[b, LC + h * 4096:LC + (h + 1) * 4096] = a[:4096]
        out[b, h * 128:(h + 1) * 128] = a[4096:]
    return out


def run_A(x, xc, mod_l, inp, l):
    tabs = rope_tables()
    xs = shard_tokens(x, xc)
    ident = np.eye(128, dtype=np.float32)
    maps = []
    for core in range(8):
        b, h = core // 2, core % 2
        modv = np.stack([pk(mod_l[0, :, b]), pk(mod_l[1, :, b]), pk(mod_l[0, :, 4]), pk(mod_l[1, :, 4])], axis=2)
        maps.append({"xa": xs[core], "modv": np.ascontiguousarray(modv), "g1n": pk(inp["norm1_g"][l]), "w_in": inp["w_in"][l],
                     "gate_b": np.ascontiguousarray(inp["gate_b"][l].reshape(32, 128).T),
                     "ropeC": tabs[h][0], "ropeS": tabs[h][1], "ident": ident})
    return run(build_A(), maps)


def sincos(k, th, shape):
    I32 = mybir.dt.int32
    res = []
    for shift in (PI / 2, 0.0):
        a = k.sb(shape); ni = k.sb(shape, I32); nf = k.sb(shape)
        k.ts(a[:], th, shift, ALU.add)
        k.ts(nf[:], a[:], 1.0 / (2 * PI), ALU.mult)
        k.copy(ni[:], nf[:])
        k.copy(nf[:], ni[:])
        k.stt(a[:], nf[:], -2 * PI, a[:], ALU.mult, ALU.add)
        k.ts(nf[:], a[:], PI, ALU.is_gt, -2 * PI, ALU.mult)
        k.tt(a[:], a[:], nf[:], ALU.add)
        k.ts(nf[:], a[:], -PI, ALU.is_lt, 2 * PI, ALU.mult)
        k.tt(a[:], a[:], nf[:], ALU.add)
        k.ts(a[:], a[:], -PI, ALU.max, PI, ALU.min)
        k.act(a[:], a[:], AF.Sin)
        res.append(a)
    return res[0], res[1]


S5BL = 256
S5NB = EXT // S5BL


@functools.lru_cache(None)
def build_S5():
    k = K()
    BL, NB = S5BL, S5NB
    uT = k.din("uT", [128, EXT]); lamS = k.din("lamS", [128, 16, 3]); lamR = k.din("lamR", [128, 16, 64, 2]); ldtR = k.din("ldtR", [128, 16])
    Bre = k.din("Bre", [128, 16, 64]); Bim = k.din("Bim", [128, 16, 64]); CW0 = k.din("CW0", [128, 16, 128]); CWs0 = k.din("CWs0", [128, 16, 128])
    dsk = k.din("dsk", [128, 1]); swapP = k.din("swapP", [128, 128]); sgn = k.din("sgn", [128, 1])
    yT = k.dout("yT", [128, EXT])

    Tc = k.sb([128, 16, BL], name="Tc"); Ts = k.sb([128, 16, BL], name="Ts"); magS = k.sb([128, 16])
    WB = k.sb([128, 16, 128], BF16); WBs = k.sb([128, 16, 128], BF16)
    CW = k.sb([128, 16, 128], BF16); CWs = k.sb([128, 16, 128], BF16)
    sw = k.sb([128, 128]); dk = k.sb([128, 1])
    k.push()
    ls = k.sb([128, 16, 3]); k.dma(ls[:], lamS[:, :, :])
    sg = k.sb([128, 1]); k.dma(sg[:], sgn[:, :])
    k.dma(dk[:], dsk[:, :])
    k.dma(sw[:], swapP[:, :])
    dtS = k.sb([128, 16]); thS = k.sb([128, 16])
    k.act(dtS[:], ls[:, :, 2], AF.Exp)
    k.tt(thS[:], ls[:, :, 1], dtS[:], ALU.mult)
    k.tt(magS[:], ls[:, :, 0], dtS[:], ALU.mult)
    k.act(magS[:], magS[:], AF.Exp)
    cosS, sinS = sincos(k, thS[:], [128, 16])
    t_a = k.sb([128, 16, BL // 2]); t_b = k.sb([128, 16, BL // 2])
    zc = k.sb([128, 16]); zs = k.sb([128, 16]); z1 = k.sb([128, 16]); z2 = k.sb([128, 16])
    k.copy(Tc[:, :, 0], cosS[:]); k.copy(Ts[:, :, 0], sinS[:])
    k.copy(zc[:], cosS[:]); k.copy(zs[:], sinS[:])
    m = 1
    while m < BL:
        zcb = View(zc, zc.t[:, :].unsqueeze(2).to_broadcast([128, 16, m]))
        zsb = View(zs, zs.t[:, :].unsqueeze(2).to_broadcast([128, 16, m]))
        k.tt(t_a[:, :, :m], Tc[:, :, 0:m], zcb, ALU.mult)
        k.tt(t_b[:, :, :m], Ts[:, :, 0:m], zsb, ALU.mult)
        k.tt(t_a[:, :, :m], t_a[:, :, :m], t_b[:, :, :m], ALU.subtract)
        k.tt(t_b[:, :, :m], Ts[:, :, 0:m], zcb, ALU.mult)
        k.copy(Tc[:, :, m:2 * m], t_a[:, :, :m])
        k.tt(t_a[:, :, :m], Tc[:, :, 0:m], zsb, ALU.mult)
        k.tt(Ts[:, :, m:2 * m], t_a[:, :, :m], t_b[:, :, :m], ALU.add)
        k.tt(z1[:], zc[:], zc[:], ALU.mult); k.tt(z2[:], zs[:], zs[:], ALU.mult)
        k.tt(zs[:], zc[:], zs[:], ALU.mult); k.ts(zs[:], zs[:], 2.0, ALU.mult)
        k.tt(zc[:], z1[:], z2[:], ALU.subtract)
        m *= 2
    Tsg = Ts
    k.ts(Tsg[:], Ts[:], sg[:, 0:1], ALU.mult)

    k.pop()
    k.push()
    lr_ = k.sb([128, 16, 64, 2]); k.dma(lr_[:], lamR[:, :, :, :])
    dR = k.sb([128, 16]); k.dma(dR[:], ldtR[:, :]); k.act(dR[:], dR[:], AF.Exp)
    dRb = View(dR, dR.t[:, :].unsqueeze(2).to_broadcast([128, 16, 64]))
    lrR = lr_[:, :, :, 0]; liR = lr_[:, :, :, 1]
    thR = k.sb([128, 16, 64]); mgR = k.sb([128, 16, 64])
    k.tt(thR[:], liR, dRb, ALU.mult)
    k.tt(mgR[:], lrR, dRb, ALU.mult)
    k.act(mgR[:], mgR[:], AF.Exp)
    cosR, sinR = sincos(k, thR[:], [128, 16, 64])
    are = cosR; aim = sinR
    k.tt(are[:], mgR[:], cosR[:], ALU.mult); k.tt(aim[:], mgR[:], sinR[:], ALU.mult)
    k.ts(are[:], are[:], -1.0, ALU.add)
    den = thR; tmp = mgR
    k.tt(den[:], lrR, lrR, ALU.mult); k.tt(tmp[:], liR, liR, ALU.mult); k.tt(den[:], den[:], tmp[:], ALU.add)
    k.recip(den[:], den[:])
    cre = k.sb([128, 16, 64]); cim = k.sb([128, 16, 64])
    k.tt(cre[:], are[:], lrR, ALU.mult); k.tt(tmp[:], aim[:], liR, ALU.mult); k.tt(cre[:], cre[:], tmp[:], ALU.add); k.tt(cre[:], cre[:], den[:], ALU.mult)
    k.tt(cim[:], aim[:], lrR, ALU.mult); k.tt(tmp[:], are[:], liR, ALU.mult); k.tt(cim[:], cim[:], tmp[:], ALU.subtract); k.tt(cim[:], cim[:], den[:], ALU.mult)
    br = k.sb([128, 16, 64]); bi = k.sb([128, 16, 64])
    k.dma(br[:], Bre[:, :, :]); k.dma(bi[:], Bim[:, :, :])
    WBf = k.sb([128, 16, 128])
    k.tt(WBf[:, :, 0:64], cre[:], br[:], ALU.mult); k.tt(tmp[:], cim[:], bi[:], ALU.mult); k.tt(WBf[:, :, 0:64], WBf[:, :, 0:64], tmp[:], ALU.subtract)
    k.tt(WBf[:, :, 64:128], cre[:], bi[:], ALU.mult); k.tt(tmp[:], cim[:], br[:], ALU.mult); k.tt(WBf[:, :, 64:128], WBf[:, :, 64:128], tmp[:], ALU.add)
    k.copy(WB[:], WBf[:])
    k.copy(WBs[:, :, 0:64], WBf[:, :, 64:128]); k.copy(WBs[:, :, 64:128], WBf[:, :, 0:64])
    cwf = k.sb([128, 16, 128]); cwsf = k.sb([128, 16, 128])
    k.dma(cwf[:], CW0[:, :, :]); k.dma(cwsf[:], CWs0[:, :, :])
    k.copy(CW[0:64], cwf[0:64]); k.act(CW[64:128], cwf[64:128], AF.Copy, scale=-1.0)
    k.act(CWs[0:64], cwsf[0:64], AF.Copy, scale=-1.0); k.copy(CWs[64:128], cwsf[64:128])

    k.pop()
    uf = k.sb([128, EXT], name="uf"); ub = k.sb([128, EXT], BF16, name="ub"); yacc = k.sb([128, EXT], name="yacc")
    for c in range(0, EXT, 2112):
        k.dma(uf[:, c:c + 2112], uT[:, c:c + 2112], chain=True)
    k.copy(ub[:], uf[:], eng="pool")
    k.ts(yacc[:], uf[:], dk[:, 0:1], ALU.mult)

    Gt = self_t = k.es.enter_context(k.nc.sbuf_tensor("Gbig", [128, 16, BL], F32))
    G = [Buf(Gt[:, g, :], f"G{g}") for g in range(16)]
    M1 = [k.sb([128, 8, BL], BF16) for _ in range(2)]; M2 = [k.sb([128, 8, BL], BF16) for _ in range(2)]
    Wt = [k.sb([128, BL]) for _ in range(4)]; T1 = [k.sb([128, BL]) for _ in range(4)]; T2 = [k.sb([128, BL]) for _ in range(4)]
    carry = [k.sb([128, 8]) for _ in range(2)]; gl = [k.sb([128, 8]) for _ in range(2)]
    c1 = [k.sb([128, 8]) for _ in range(2)]; c2 = [k.sb([128, 8]) for _ in range(2)]
    for d in range(2):
        k.memset(carry[d][:], 0.0)
    psSW = [k.ps([128, 2, BL]) for _ in range(4)]
    psY = [k.ps([128, BL]) for _ in range(2)]; psG = [k.ps([128, 8]) for _ in range(2)]
    it = 0
    pending = None
    for s in range(NB):
        for d in range(2):
            blk = s if d == 0 else (0 if s == 0 else NB - s)
            c0 = blk * BL
            rhs = ub[:, c0:c0 + BL]
            if d == 1:
                rhs = rhs[:, ::-1]
            for g8 in range(8):
                vg = d * 8 + g8
                it += 1
                pS, pW = psSW[it % 4][:, 0, :], psSW[it % 4][:, 1, :]
                k.mm(pS, WB[:, vg, :], rhs)
                k.mm(pW, WBs[:, vg, :], rhs)
                t1, t2, w = T1[it % 4], T2[it % 4], Wt[it % 4]
                k.tt(t1[:], pS, Tc[:, vg, :], ALU.mult)
                k.tt(t2[:], pW, Tsg[:, vg, :], ALU.mult)
                k.tt(w[:], t1[:], t2[:], ALU.add)
                k.scan(G[vg][:], View(magS, magS.t[:, vg:vg + 1].to_broadcast([128, BL])), w[:], carry[d][:, g8:g8 + 1])
                k.tt(M1[d][:, g8, :], G[vg][:], Tc[:, vg, :], ALU.mult, eng="pool")
                k.tt(M2[d][:, g8, :], G[vg][:], Tsg[:, vg, :], ALU.mult, eng="pool")
                k.copy(gl[d][:, g8:g8 + 1], G[vg][:, BL - 1:BL], eng="act")
            if pending is not None:
                pending()

            def pending(d=d, c0=c0):
                py = psY[d]
                for g8 in range(8):
                    vg = d * 8 + g8
                    k.mm(py[:], CW[:, vg, :], M1[d][:, g8, :], start=(g8 == 0), stop=False, inc=False)
                    k.mm(py[:], CWs[:, vg, :], M2[d][:, g8, :], start=False, stop=(g8 == 7), inc=(g8 == 7))
                src = py[:, :] if d == 0 else py[:, ::-1]
                k.tt(yacc[:, c0:c0 + BL], src, yacc[:, c0:c0 + BL], ALU.add)
                k.mm(psG[d][:], sw[:], gl[d][:])
                k.tt(c1[d][:], gl[d][:], Tc[:, 8 * d:8 * d + 8, BL - 1], ALU.mult)
                k.tt(c2[d][:], psG[d][:], Tsg[:, 8 * d:8 * d + 8, BL - 1], ALU.mult)
                k.tt(carry[d][:], c1[d][:], c2[d][:], ALU.subtract)
    pending()
    for c in range(0, EXT, 2112):
        k.dma(yT[:, c:c + 2112], yacc[:, c:c + 2112])
    return k.finish()


def s5_maps(uT_all, inp, l):
    maps = []
    swapP = np.zeros((128, 128), np.float32)
    for p in range(64):
        swapP[p, p + 64] = 1.0; swapP[p + 64, p] = 1.0
    sgn = np.ones((128, 1), np.float32); sgn[64:] = -1.0
    for core in range(8):
        b, ct = core // 2, core % 2
        gs = slice(8 * ct, 8 * ct + 8)
        fl = lambda a: np.asarray(a[l][:, gs]).reshape((16,) + a.shape[3:])
        lre, lim, ldt = fl(inp["s5_lam_re"]), fl(inp["s5_lam_im"]), fl(inp["s5_log_dt"])
        bre, bim, cre, cim = fl(inp["s5_b_re"]), fl(inp["s5_b_im"]), fl(inp["s5_c_re"]), fl(inp["s5_c_im"])
        lamS = np.zeros((128, 16, 3), np.float32)
        lamS[:64, :, 0] = lre.T; lamS[64:, :, 0] = lre.T; lamS[:64, :, 1] = lim.T; lamS[64:, :, 1] = lim.T; lamS[:, :, 2] = ldt[None, :]
        lamR = np.broadcast_to(np.stack([lre, lim], -1)[None], (128, 16, 64, 2)).astype(np.float32)
        ldtR = np.broadcast_to(ldt[None], (128, 16)).astype(np.float32)
        Bre = np.zeros((128, 16, 64), np.float32); Bim = np.zeros((128, 16, 64), np.float32)
        CW0 = np.zeros((128, 16, 128), np.float32); CWs0 = np.zeros((128, 16, 128), np.float32)
        for vg in range(16):
            r0 = 16 * (vg % 8)
            Bre[r0:r0 + 16, vg, :] = bre[vg].T; Bim[r0:r0 + 16, vg, :] = bim[vg].T
            CW0[:64, vg, r0:r0 + 16] = cre[vg].T; CW0[64:, vg, r0:r0 + 16] = cim[vg].T
            CWs0[:64, vg, r0:r0 + 16] = cim[vg].T; CWs0[64:, vg, r0:r0 + 16] = cre[vg].T
        maps.append({"uT": np.ascontiguousarray(uT_all[b, ct * 128:(ct + 1) * 128]), "lamS": lamS, "lamR": np.ascontiguousarray(lamR),
                     "ldtR": np.ascontiguousarray(ldtR), "Bre": Bre, "Bim": Bim, "CW0": CW0, "CWs0": CWs0,
                     "dsk": np.ascontiguousarray(inp["s5_d"][l][ct * 128:(ct + 1) * 128, None]), "swapP": swapP, "sgn": sgn})
    return maps


def run_S5(uT_all, inp, l):
    res = run(build_S5(), s5_maps(uT_all, inp, l))
    y = np.empty((B, 256, EXT), np.float32)
    for core in range(8):
        y[core // 2, (core % 2) * 128:(core % 2 + 1) * 128] = res[core]["yT"]
    return y


GBLK = [(0, 256)] + [(256 + 512 * i, 512) for i in range(16)]


@functools.lru_cache(None)
def build_GLA(DBG=99):
    k = K()
    qT = k.din("qT", [128, EXT]); kT = k.din("kT", [128, EXT]); gaT = k.din("gaT", [32, EXT]); v = k.din("v", [EXT, 256], BF16)
    wa2 = k.din("wa2", [16, 2, 128]); nba = k.din("nba", [128, 2]); maskd = k.din("mask", [64, 2, 512]); rmaskd = k.din("rmask", [128, 512])
    ident = k.din("ident", [128, 128])
    outs = [k.dout("oTf", [256, EXT]), k.dout("oTb", [256, EXT])]
    wa = k.sb([16, 2, 128]); nb_ = k.sb([128, 2]); mk = k.sb([64, 2, 512]); rm = k.sb([128, 512]); idf = k.sb([128, 128])
    k.dma(wa[:], wa2[:, :, :]); k.dma(nb_[:], nba[:, :]); k.dma(mk[:], maskd[:, :, :]); k.dma(rm[:], rmaskd[:, :]); k.dma(idf[:], ident[:, :])
    R2 = range(2)
    qf = [k.sb([128, 512]) for _ in R2]; kf = [k.sb([128, 512]) for _ in R2]; ga = [k.sb([16, 512]) for _ in R2]
    vb = [k.sb([64, 8, 256], BF16) for _ in R2]
    e1 = [k.sb([128, 512]) for _ in R2]; bp = [k.sb([128, 512]) for _ in R2]; eb = [k.sb([128, 512]) for _ in R2]; enb = [k.sb([128, 512]) for _ in R2]
    qt = [k.sb([128, 512]) for _ in R2]; kt = [k.sb([128, 512]) for _ in R2]; kend = [k.sb([128, 8, 64]) for _ in R2]
    dec = [k.sb([128, 8]) for _ in R2]; ktok = [k.sb([64, 8, 128], BF16) for _ in R2]
    att = [[k.sb([64, 8, 64], BF16) for _ in R2] for _ in R2]
    S = [k.sb([128, 9, 128]) for _ in R2]
    osb = [k.sb([128, 512]) for _ in range(4)]
    for d in R2:
        k.memset(S[d][:, 0, :], 0.0)
    z_ps = k.ps([128, 512]); tr_ps = [k.ps([64, 4, 128])] * 2; att_ps = [k.ps([64, 8, 64])] * 2
    otmp = k.sb([128, 512])
    kvb = k.ps([128, 4, 128])
    o_ps = [k.ps([128, 8, 64]) for _ in R2]; oi_ps = [k.ps([128, 8, 64]) for _ in R2]
    it = 0
    for s in range(17 if DBG == 99 else 1):
        for d in R2:
            bi = s if d == 0 else (0 if s == 0 else 17 - s)
            c0, n = GBLK[bi]
            nch = n // 64
            r = it % 2
            it += 1
            k.dma(qf[r][:, :n], qT[:, c0:c0 + n]); k.dma(kf[r][:, :n], kT[:, c0:c0 + n])
            k.dma(ga[r][:, :n], gaT[16 * d:16 * d + 16, c0:c0 + n])
            k.dma(vb[r][:, :nch, :], v[c0:c0 + n, :].re("(c j) e -> j c e", j=64))
            k.mm(z_ps[:, :n], wa[:, d, :], ga[r][:, :n])
            k.act(e1[r][:, :n], z_ps[:, :n], AF.Exp, bias=nb_[:, d:d + 1], scale=-1.0)
            k.act(e1[r][:, :n], e1[r][:, :n], AF.Ln, bias=1.0)
            if d == 0:
                k.scan(bp[r][:, :n], rm[:, :n], e1[r][:, :n], 0.0)
            else:
                k.scan(bp[r][:, :n][:, ::-1], rm[:, :n], e1[r][:, :n][:, ::-1], 0.0)
            if DBG < 2:
                continue
            k.act(eb[r][:, :n], bp[r][:, :n], AF.Exp, scale=-1.0 / 16)
            k.act(enb[r][:, :n], bp[r][:, :n], AF.Exp, scale=1.0 / 16)
            k.tt(qt[r][:, :n], qf[r][:, :n], eb[r][:, :n], ALU.mult)
            k.tt(kt[r][:, :n], kf[r][:, :n], enb[r][:, :n], ALU.mult, eng="pool")
            ebv = eb[r][:, :n].re("p (c j) -> p c j", j=64)
            k.copy(dec[r][:, :nch], ebv[:, :, 63] if d == 0 else ebv[:, :, 0])
            decb = View(dec[r], dec[r].t[:, :nch].unsqueeze(2).to_broadcast([128, nch, 64]))
            k.tt(kend[r][:, :nch, :], kt[r][:, :n].re("p (c j) -> p c j", j=64), decb, ALU.mult)
            for c in range(nch):
                tp = tr_ps[(c // 4) % 2]
                k.tr(tp[:, c % 4, :], kend[r][:, c, :], idf[:], inc=(c % 4 == 3))
                if c % 4 == 3:
                    k.copy(ktok[r][:, c - 3:c + 1, :], tp[:], eng="act")
            if DBG < 3:
                continue
            for h2 in R2:
                hs = slice(64 * h2, 64 * h2 + 64)
                for c in range(nch):
                    k.mm(att_ps[h2][:, c, :], kt[r][hs, c * 64:(c + 1) * 64], qt[r][hs, c * 64:(c + 1) * 64], inc=(c == nch - 1))
                k.tt(att[r][h2][:, :nch, :], att_ps[h2][:, :nch, :], mk[:, d, :n].re("p (c i) -> p c i", i=64), ALU.mult)
            if DBG < 4:
                continue
            order = list(range(nch)) if d == 0 else list(range(nch - 1, -1, -1))
            for g0 in range(0, nch, 4):
                steps = list(range(g0, min(g0 + 4, nch)))
                for step in steps:
                    c = order[step]
                    for h2 in R2:
                        k.mm(kvb[64 * h2:64 * h2 + 64, step % 4, :], ktok[r][:, c, 64 * h2:64 * h2 + 64], vb[r][:, c, 128 * h2:128 * h2 + 128],
                             inc=(h2 == 1 and step == steps[-1]))
                for step in steps:
                    c = order[step]
                    k.stt(S[d][:, step + 1, :], S[d][:, step, :], dec[r][:, c:c + 1], kvb[:, step % 4, :], ALU.mult, ALU.add)
            if DBG < 5:
                continue
            for h2 in R2:
                hs = slice(64 * h2, 64 * h2 + 64)
                for step, c in enumerate(order):
                    k.mm(o_ps[h2][:, c, :], vb[r][:, c, 128 * h2:128 * h2 + 128], att[r][h2][:, c, :], inc=(step == nch - 1))
                if DBG < 6:
                    continue
                for step, c in enumerate(order):
                    k.mm(oi_ps[h2][:, c, :], S[d][hs, step, :], qt[r][hs, c * 64:(c + 1) * 64], inc=(step == nch - 1))
                if DBG < 7:
                    continue
                ob = osb[(2 * it + h2) % 4]
                k.copy(otmp[:, :n], oi_ps[h2][:, :nch, :].re("p c i -> p (c i)"), eng="act")
                k.tt(ob[:, :n], o_ps[h2][:, :nch, :].re("p c i -> p (c i)"), otmp[:, :n], ALU.add)
                k.dma(outs[d][128 * h2:128 * h2 + 128, c0:c0 + n], ob[:, :n])
            if DBG < 8:
                continue
            k.copy(S[d][:, 0, :], S[d][:, nch, :])
    return k.finish()


def run_GLA(gq, gk, ga, gv, inp, l):
    i_ = np.arange(64)
    mf = (i_[None, :] >= i_[:, None]).astype(np.float32)
    mask = np.stack([np.tile(mf, (1, 8)), np.tile(mf.T, (1, 8))], axis=1)
    rmask = np.ones((128, 512), np.float32); rmask[:, 0::64] = 0.0
    ident = np.eye(128, dtype=np.float32)
    maps = []
    for core in range(8):
        b, hp = core // 2, core % 2
        cs = slice(128 * hp, 128 * hp + 128)
        wa2 = np.ascontiguousarray(inp["gla_w_a2"][l][:, :, cs].transpose(1, 0, 2))
        nba = np.ascontiguousarray((inp["gla_b_a"][l][:, cs]).T) * np.float32(-1.0)
        maps.append({"qT": np.ascontiguousarray(gq[b, cs]), "kT": np.ascontiguousarray(gk[b, cs]), "gaT": np.ascontiguousarray(ga[b]),
                     "v": np.ascontiguousarray(gv[b][:, 256 * hp:256 * hp + 256]), "wa2": wa2, "nba": nba, "mask": mask, "rmask": rmask, "ident": ident})
    res = run(build_GLA(), maps)
    of = np.empty((B, 512, EXT), np.float32); ob = np.empty((B, 512, EXT), np.float32)
    for core in range(8):
        b, hp = core // 2, core % 2
        of[b, 256 * hp:256 * hp + 256] = res[core]["oTf"]; ob[b, 256 * hp:256 * hp + 256] = res[core]["oTb"]
    return of, ob


@functools.lru_cache(None)
def build_NA():
    k = K()
    qT = k.din("qT", [128, EXT], BF16); kT = k.din("kT", [128, EXT], BF16); v = k.din("v", [EXT, 128], BF16)
    rpbg = k.din("rpbg", [128, 28, 64]); maskd = k.din("mask", [128, 64])
    out = k.dout("naT", [128, EXT], BF16)
    q_sb = k.sb([64, 2, EXT], BF16, "na_q"); k_sb = k.sb([64, 2, EXT], BF16, "na_k")
    v_ev = k.sb([128, 66, 128], BF16, "na_ve"); v_od = k.sb([128, 65, 128], BF16, "na_vo")
    o_sb = k.sb([128, EXT], BF16, "na_o")
    for h in range(2):
        k.dma(q_sb[:, h, :], qT[64 * h:64 * h + 64, :]); k.dma(k_sb[:, h, :], kT[64 * h:64 * h + 64, :])
    vv_e = v[:, :].re("(t p) e -> p t e", p=128); vv_o = v[64:64 + 65 * 128, :].re("(t p) e -> p t e", p=128)
    for t0 in range(0, 66, 22):
        k.dma(v_ev[:, t0:t0 + 22, :], vv_e[:, t0:t0 + 22, :], chain=True)
    for t0 in range(0, 65, 13):
        k.dma(v_od[:, t0:t0 + 13, :], vv_o[:, t0:t0 + 13, :], chain=True)
    rb = k.sb([128, 28, 64]); mk = k.sb([128, 64]); E = k.sb([128, 28, 64], BF16); ones = k.sb([128, 64], BF16)
    k.dma(rb[:], rpbg[:, :, :]); k.dma(mk[:], maskd[:, :])
    k.act(rb[:], rb[:], AF.Exp)
    k.tt(E[:], rb[:], View(mk, mk.t[:, :].unsqueeze(1).to_broadcast([128, 28, 64])), ALU.mult)
    k.memset(ones[:], 1.0)
    stl = [k.ps([128, 8, 64]) for _ in range(2)]; stc = [k.ps([128, 8, 64]) for _ in range(2)]; po = [k.ps([128, 8, 64]) for _ in range(2)]
    pt = [k.sb([128, 6, 64], BF16) for _ in range(2)]; pe_ = [k.sb([128, 4, 64], BF16) for _ in range(2)]
    rden = [k.sb([128, 64]) for _ in range(2)]
    it = 0
    rows = [("c", j) for j in range(4)] + [("l", r) for r in range(128)]
    for ri, (kind, r) in enumerate(rows):
        q0 = 64 * r if kind == "c" else LC + 64 * r
        p_o = po[ri % 2]
        for h in range(2):
            it += 1
            sl, sc, p_t, p_e = stl[it % 2], stc[it % 2], pt[it % 2], pe_[it % 2]
            qv = q_sb[:, h, q0:q0 + 64]
            for j in range(2):
                k.mm(sc[:, j, :], k_sb[:, h, 128 * j:128 * j + 128], qv, inc=(j == 1))
            k.act(p_t[:, 0:2, :], sc[:, 0:2, :], AF.Exp)
            tiles = [(v_ev, 0), (v_ev, 1)]
            if kind == "l":
                kr0 = min(max(r - 4, 0), 120)
                dr0 = kr0 - r + 7
                for j in range(4):
                    kt = LC + 64 * (kr0 + 2 * j)
                    k.mm(sl[:, j, :], k_sb[:, h, kt:kt + 128], qv, inc=(j == 3))
                    tiles.append((v_ev, kt // 128) if kt % 128 == 0 else (v_od, (kt - 64) // 128))
                k.act(p_e[:], sl[:, 0:4, :], AF.Exp)
                k.tt(p_t[:, 2:6, :], p_e[:], E[:, 14 * h + dr0:14 * h + dr0 + 7:2, :], ALU.mult, eng=("pool" if it % 2 else "dve"))
            n = len(tiles)
            ov = p_o[64 * h:64 * h + 64, 0, :]
            for i, (vb_, vt) in enumerate(tiles):
                k.mm(ov, vb_[:, vt, 64 * h:64 * h + 64], p_t[:, i, :], start=(i == 0), stop=(i == n - 1), inc=(i == n - 1))
            k.mm(p_o[64 * h:64 * h + 64, 1:1 + n, :].re("p i q -> p (i q)"), ones[:], p_t[:, 0:n, :].re("p i q -> p (i q)"))
        rd = rden[ri % 2]
        k.reduce(rd[:], p_o[:, 1:1 + n, :].re("p i q -> p q i"), ALU.add)
        k.recip(rd[:], rd[:])
        k.tt(o_sb[:, q0:q0 + 64], p_o[:, 0, :], rd[:], ALU.mult)
    for c in range(0, EXT, 2112):
        k.dma(out[:, c:c + 2112], o_sb[:, c:c + 2112])
    return k.finish()


def na_tables(rpb_l):
    c = np.arange(64)
    dc = np.clip(c[:, None] - c[None, :] + 15, 0, 30)
    cs = np.clip(c - 8, 0, 48)
    mask = ((c[:, None] >= cs[None, :]) & (c[:, None] < cs[None, :] + 16)).astype(np.float32)
    g = np.asarray(rpb_l)[:, :, dc].transpose(2, 0, 1, 3)
    g2 = np.concatenate([g[:, :, 0:14], g[:, :, 1:15]], axis=0)
    return np.ascontiguousarray(g2), np.ascontiguousarray(np.tile(mask, (2, 1)))


def run_NA(nq, nk, nv, inp, l):
    g, mask = na_tables(inp["na_rpb"][l])
    maps = []
    for core in range(8):
        b, hp = core // 2, core % 2
        cs = slice(128 * hp, 128 * hp + 128)
        maps.append({"qT": np.ascontiguousarray(nq[b, cs]), "kT": np.ascontiguousarray(nk[b, cs]), "v": np.ascontiguousarray(nv[b][:, cs]),
                     "rpbg": np.ascontiguousarray(g[:, 2 * hp:2 * hp + 2].reshape(128, 28, 64)), "mask": mask})
    res = run(build_NA(), maps)
    o = np.empty((B, 256, EXT), NPBF)
    for core in range(8):
        o[core // 2, 128 * (core % 2):128 * (core % 2) + 128] = res[core]["naT"]
    return o


@functools.lru_cache(None)
def build_CV():
    k = K()
    cy = k.din("cy", [128, EXT], BF16); dw = k.din("dw", [128, 31]); dwb = k.din("dwb", [128, 1])
    out = k.dout("cvT", [128, EXT])
    W = 15 + LC + 30 + L + 15
    yp = k.sb([128, W], BF16, "cv_yp"); wt = k.sb([128, 31]); bt = k.sb([128, 1])
    k.dma(wt[:], dw[:, :]); k.dma(bt[:], dwb[:, :])
    k.memset(yp[:, 0:15], 0.0); k.memset(yp[:, 271:301], 0.0); k.memset(yp[:, 301 + L:W], 0.0)
    k.dma(yp[:, 15:271], cy[:, 0:LC]); k.dma(yp[:, 301:301 + L], cy[:, LC:EXT], chain=True)
    chunks = [(0, 0, LC)] + [(LC + c, 286 + c, 2048) for c in range(0, L, 2048)]
    for (o0, i0, n) in chunks:
        acc = k.sb([128, n])
        k.ts(acc[:], yp[:, i0:i0 + n], wt[:, 0:1], ALU.mult, bt[:, 0:1], ALU.add)
        for j in range(1, 31):
            k.stt(acc[:], yp[:, i0 + j:i0 + j + n], wt[:, j:j + 1], acc[:], ALU.mult, ALU.add)
        k.dma(out[:, o0:o0 + n], acc[:])
    return k.finish()


def run_CV(cy, inp, l):
    maps = []
    for core in range(8):
        b, ct = core // 2, core % 2
        cs = slice(128 * ct, 128 * ct + 128)
        maps.append({"cy": np.ascontiguousarray(cy[b, cs]), "dw": np.ascontiguousarray(inp["conv_dw"][l][:, cs].T),
                     "dwb": np.ascontiguousarray(inp["conv_dw_b"][l][cs, None])})
    res = run(build_CV(), maps)
    o = np.empty((B, 256, EXT), np.float32)
    for core in range(8):
        o[core // 2, 128 * (core % 2):128 * (core % 2) + 128] = res[core]["cvT"]
    return o


OBLK = [(i * 256, 256) for i in range(16)] + [(4096, 128)]


def fmv(dram, c0, n):
    return dram[:, c0:c0 + n].re("(t p) c -> p t c", p=128)


@functools.lru_cache(None)
def build_O1():
    k = K()
    xa = k.din("xa", [NLOC, D]); g1b = k.din("g1b", [128, 2, D])
    s5y = k.din("s5y", [256, NLOC]); glf = k.din("glf", [512, NLOC]); glb = k.din("glb", [512, NLOC])
    grs = k.din("grs", [512, NLOC], BF16); na = k.din("na", [256, NLOC], BF16); cv = k.din("cv", [256, NLOC])
    gs = k.din("gs", [4096, NLOC], BF16)
    w_glu = k.din("w_glu", [256, 256]); b_glu = k.din("b_glu", [128, 2]); w_s5o = k.din("w_s5o", [256, D])
    gng = k.din("gng", [128, 1]); w_glo = k.din("w_glo", [512, D]); w_nao = k.din("w_nao", [256, D])
    lng = k.din("lng", [128, 2]); lnb = k.din("lnb", [128, 2]); w_cvo = k.din("w_cvo", [256, D]); w_mix = k.din("w_mix", [D, D])
    x1 = k.dout("x1", [NLOC, D])

    stage = [k.sb([128, 1024]) for _ in range(2)]
    wi = [0]

    def load_cast(w_dram, kt, ncols, name):
        wb = k.sb([128, kt, ncols], BF16, name)
        for kk in range(kt):
            wi[0] += 1
            st = stage[wi[0] % 2]
            k.dma(st[:, :ncols], w_dram[kk * 128:(kk + 1) * 128, :])
            k.copy(wb[:, kk, :], st[:, :ncols], eng=("act" if wi[0] % 2 else "dve"))
        return wb

    wglu = load_cast(w_glu, 2, 256, "wglu"); ws5o = load_cast(w_s5o, 2, D, "ws5o"); wglo = load_cast(w_glo, 4, D, "wglo")
    wnao = load_cast(w_nao, 2, D, "wnao"); wcvo = load_cast(w_cvo, 2, D, "wcvo"); wmix = load_cast(w_mix, 8, D, "wmix")
    bglu = k.sb([128, 2]); gn = k.sb([128, 1]); lg = k.sb([128, 2]); lb = k.sb([128, 2]); g1t = k.sb([128, 2, D], name="g1t")
    k.dma(bglu[:], b_glu[:, :]); k.dma(gn[:], gng[:, :]); k.dma(lg[:], lng[:, :]); k.dma(lb[:], lnb[:, :]); k.dma(g1t[:], g1b[:, :, :])
    onesb = k.sb([128, 128], BF16); k.memset(onesb[:], 1.0)

    pss = [k.ps([128, 512]) for _ in range(8)]
    psi = [0]

    def nps():
        psi[0] += 1
        return pss[psi[0] % 8]

    R2 = range(2)
    NB = 256
    yt = [k.sb([128, 2, NB]) for _ in R2]; of_ = [k.sb([128, 4, NB]) for _ in R2]; ob_ = [k.sb([128, 4, NB]) for _ in R2]
    gr = [k.sb([128, 4, NB], BF16) for _ in R2]; cvt = [k.sb([128, 2, NB]) for _ in R2]; nat = [k.sb([128, 2, NB], BF16) for _ in R2]
    z = k.sb([128, 2, NB]); zb = k.sb([128, 2, NB], BF16); z2b = k.sb([128, 2, NB], BF16); sgs = [k.sb([128, NB]) for _ in range(2)]
    o_ = k.sb([128, 4, NB]); osq = k.sb([128, 4, NB], BF16); rs4 = k.sb([128, 4, NB]); of2 = k.sb([128, 4, NB], BF16)
    cvb = k.sb([128, 2, NB], BF16); cvq = k.sb([128, 2, NB], BF16); mean = k.sb([128, NB]); msq = k.sb([128, NB]); var = k.sb([128, NB])
    dds = [k.sb([128, NB]) for _ in range(2)]; cvo = k.sb([128, 2, NB], BF16); tqs = [[k.sb([128, NB]) for _ in range(4)] for _ in range(2)]; mg = k.sb([128, 8, NB], BF16)
    xts = [k.sb([128, D]) for _ in R2]; xos = [k.sb([128, D]) for _ in R2]
    z2bs = [z2b, k.sb([128, 2, NB], BF16)]; of2s = [of2, k.sb([128, 4, NB], BF16)]; cvos = [cvo, k.sb([128, 2, NB], BF16)]
    gstm = [k.sb([128, 4, NB], BF16) for _ in range(8)]

    def load_gs(bi, m):
        c0, n = OBLK[bi]
        k.dma(gstm[m][:, :, :n], gs[:, c0:c0 + n].re("(i m p) c -> p i m c", i=4, m=8, p=128)[:, :, m, :])

    def prep(bi):
        c0, n = OBLK[bi]
        s = bi % 2
        z2b_, of2_, cvo_ = z2bs[s], of2s[s], cvos[s]
        k.dma(yt[s][:, :, :n], fmv(s5y, c0, n)); k.dma(of_[s][:, :, :n], fmv(glf, c0, n)); k.dma(ob_[s][:, :, :n], fmv(glb, c0, n))
        k.dma(gr[s][:, :, :n], fmv(grs, c0, n)); k.dma(cvt[s][:, :, :n], fmv(cv, c0, n)); k.dma(nat[s][:, :, :n], fmv(na, c0, n))
        yield
        k.act(z[:, :, :n], yt[s][:, :, :n], AF.Gelu_apprx_tanh)
        k.copy(zb[:, :, :n], z[:, :, :n], eng="pool")
        k.tt(o_[:, :, :n], of_[s][:, :, :n], ob_[s][:, :, :n], ALU.add, eng="pool")
        k.tt(osq[:, :, :n], o_[:, :, :n], o_[:, :, :n], ALU.mult)
        yield
        ps_glu = [nps(), nps()]
        for jt in R2:
            for it_ in R2:
                k.mm(ps_glu[jt][:, :n], wglu[:, it_, jt * 128:(jt + 1) * 128], zb[:, it_, :n], start=(it_ == 0), stop=(it_ == 1), inc=(it_ == 1))
        pms = [nps(), nps()]
        for hh in range(4):
            k.mm(pms[hh // 2][:, (hh % 2) * NB:(hh % 2) * NB + n], onesb[:], osq[:, hh, :n], inc=(hh % 2 == 1))
        k.copy(cvb[:, :, :n], cvt[s][:, :, :n], eng="pool")
        k.tt(cvq[:, :, :n], cvt[s][:, :, :n], cvt[s][:, :, :n], ALU.mult)
        yield
        for jt in R2:
            k.act(sgs[jt][:, :n], ps_glu[jt][:, :n], AF.Sigmoid, bias=bglu[:, jt:jt + 1])
            k.tt(z2b_[:, jt, :n], z[:, jt, :n], sgs[jt][:, :n], ALU.mult)
        for j in R2:
            k.ts(rs4[:, 2 * j:2 * j + 2, :n], pms[j][:, :].re("p (h c) -> p h c", c=NB)[:, :, :n], 1.0 / 128, ALU.mult, EPS, ALU.add)
        yield
        p1 = nps(); p2 = nps()
        for t in R2:
            k.mm(p1[:, :n], onesb[:], cvb[:, t, :n], start=(t == 0), stop=(t == 1), inc=(t == 1))
        for t in R2:
            k.mm(p2[:, :n], onesb[:], cvq[:, t, :n], start=(t == 0), stop=(t == 1), inc=(t == 1))
        k.act(rs4[:, :, :n], rs4[:, :, :n], AF.Sqrt)
        yield
        k.recip(rs4[:, :, :n], rs4[:, :, :n])
        k.ts(mean[:, :n], p1[:, :n], 1.0 / 256, ALU.mult)
        k.tt(msq[:, :n], mean[:, :n], mean[:, :n], ALU.mult)
        k.stt(var[:, :n], p2[:, :n], 1.0 / 256, msq[:, :n], ALU.mult, ALU.subtract)
        k.ts(var[:, :n], var[:, :n], EPS, ALU.add)
        yield
        k.act(var[:, :n], var[:, :n], AF.Sqrt)
        k.tt(o_[:, :, :n], o_[:, :, :n], rs4[:, :, :n], ALU.mult)
        k.stt(of2_[:, :, :n], o_[:, :, :n], gn[:, 0:1], gr[s][:, :, :n], ALU.mult, ALU.mult)
        yield
        k.recip(var[:, :n], var[:, :n])
        for t in R2:
            dd = dds[t]
            k.tt(dd[:, :n], cvt[s][:, t, :n], mean[:, :n], ALU.subtract)
            k.tt(dd[:, :n], dd[:, :n], var[:, :n], ALU.mult)
        yield
        for t in R2:
            k.act(cvo_[:, t, :n], dds[t][:, :n], AF.Silu, scale=lg[:, t:t + 1], bias=lb[:, t:t + 1])
        yield

    tic = [0]

    def mloop(bi):
        c0, n = OBLK[bi]
        s = bi % 2
        srcs = [(ws5o, z2bs[s], 2), (wglo, of2s[s], 4), (wnao, nat[s], 2), (wcvo, cvos[s], 2)]
        for m in range(8):
            tq = tqs[m % 2]
            ps4 = [nps() for _ in range(4)]
            for i, (w_, a_, kt) in enumerate(srcs):
                for t in range(kt):
                    k.mm(ps4[i][:, :n], w_[:, t, m * 128:(m + 1) * 128], a_[:, t, :n], start=(t == 0), stop=(t == kt - 1), inc=(t == kt - 1))
            for i in range(4):
                k.tt(tq[i][:, :n], ps4[i][:, :n], gstm[m][:, i, :n], ALU.mult)
            k.tt(tq[0][:, :n], tq[0][:, :n], tq[1][:, :n], ALU.add, eng="pool")
            k.tt(tq[2][:, :n], tq[2][:, :n], tq[3][:, :n], ALU.add, eng="pool")
            k.tt(mg[:, m, :n], tq[0][:, :n], tq[2][:, :n], ALU.add, eng="pool")
            if bi + 1 < len(OBLK):
                load_gs(bi + 1, m)
            yield
        for tt_ in range(n // 128):
            tic[0] += 1
            tok0 = c0 + tt_ * 128
            xt, xo = xts[tic[0] % 2], xos[tic[0] % 2]
            k.dma(xt[:], xa[tok0:tok0 + 128, :])
            j = 1 if tok0 >= 4096 else 0
            for hf in R2:
                p = nps()
                for ft in range(8):
                    k.mm(p[:, :], mg[:, ft, tt_ * 128:(tt_ + 1) * 128], wmix[:, ft, hf * 512:(hf + 1) * 512], start=(ft == 0), stop=(ft == 7), inc=(ft == 7))
                k.tt(xo[:, hf * 512:(hf + 1) * 512], p[:, :], g1t[:, j, hf * 512:(hf + 1) * 512], ALU.mult)
            k.tt(xo[:], xo[:], xt[:], ALU.add, eng="pool")
            k.dma(x1[tok0:tok0 + 128, :], xo[:])
            yield

    for m in range(8):
        load_gs(0, m)
    for _ in prep(0):
        pass
    for bi in range(len(OBLK)):
        ga = mloop(bi)
        gb = prep(bi + 1) if bi + 1 < len(OBLK) else iter(())
        done_a = done_b = False
        while not (done_a and done_b):
            if not done_a:
                try:
                    next(ga)
                except StopIteration:
                    done_a = True
            if not done_b:
                try:
                    next(gb)
                except StopIteration:
                    done_b = True
    return k.finish()


def scatter_fm(a, core):
    b, h = core // 2, core % 2
    return np.ascontiguousarray(np.concatenate([a[b][:, LC + h * 4096:LC + (h + 1) * 4096], a[b][:, h * 128:(h + 1) * 128]], 1))


def bc128(v):
    return np.broadcast_to(np.asarray(v, np.float32)[None], (128,) + np.asarray(v).shape)


def run_O1(xs, resA, y5, glf, glb, nao, cvo, mod_l, inp, l, cores=range(8)):
    maps = []
    for core in cores:
        b = core // 2
        g1b = np.ascontiguousarray(np.stack([bc128(mod_l[2, :, b]), bc128(mod_l[2, :, 4])], axis=1))
        maps.append({"xa": xs[core], "g1b": g1b, "s5y": scatter_fm(y5, core), "glf": scatter_fm(glf, core), "glb": scatter_fm(glb, core),
                     "grs": resA[core]["grsT"], "na": scatter_fm(nao, core), "cv": scatter_fm(cvo, core), "gs": resA[core]["gsT"],
                     "w_glu": inp["s5_w_glu"][l], "b_glu": np.ascontiguousarray(inp["s5_b_glu"][l].reshape(2, 128).T), "w_s5o": inp["s5_w_out"][l],
                     "gng": np.ascontiguousarray(inp["gla_norm_g"][l][:, None]), "w_glo": inp["gla_w_out"][l], "w_nao": inp["na_w_out"][l],
                     "lng": np.ascontiguousarray(inp["conv_ln_g"][l].reshape(2, 128).T), "lnb": np.ascontiguousarray(inp["conv_ln_b"][l].reshape(2, 128).T),
                     "w_cvo": inp["conv_w_out"][l], "w_mix": inp["w_mix_out"][l]})
    res = run(build_O1(), maps)
    return [r["x1"] for r in res]


O2PASS = [(0, 9), (9, 8), (17, 8), (25, 8)]


@functools.lru_cache(None)
def build_O2(last):
    k = K()
    x1 = k.din("x1", [NLOC, D]); modv = k.din("modv", [128, 8, 4]); g2n = k.din("g2n", [128, 8])
    g2b = k.din("g2b", [128, 2, D]); fgb = k.din("fgb", [128, D])
    wr = k.din("wr", [D, 36]); brb = k.din("brb", [128, 36])
    w1 = k.din("w1", [32, D, 256]); w3 = k.din("w3", [32, D, 256]); w2 = k.din("w2", [32, 256, D])
    ident = k.din("ident", [128, 128])
    out = k.dout("x2", [NLOC, D])

    idf = k.sb([128, 128]); k.dma(idf[:], ident[:, :])
    A_lat, B_lat, A_ctx, B_ctx = mod_prep(k, modv, g2n)
    wrt = k.sb([128, 8, 36]); brt = k.sb([128, 36]); g2t = k.sb([128, 2, D], name="g2t"); fgt = k.sb([128, D], name="fgt")
    k.dma(wrt[:], wr[:, :].re("(k p) c -> p k c", p=128)); k.dma(brt[:], brb[:, :]); k.dma(g2t[:], g2b[:, :, :]); k.dma(fgt[:], fgb[:, :])
    pss = [k.ps([128, 512]) for _ in range(8)]
    psi = [0]

    def nps():
        psi[0] += 1
        return pss[psi[0] % 8]

    R2 = range(2)
    MT = 9
    hT = k.sb([128, 8, MT * 128], BF16, "o2_hT"); yacc = k.sb([128, MT, D], name="o2_yacc"); comb = k.sb([128, 33, 32], name="o2_comb")
    st1 = k.sb([128, 8, 256], name="st1"); st3 = k.sb([128, 8, 256], name="st3"); st2 = k.sb([128, 2, D], name="st2")
    w1b = [k.sb([128, 8, 256], BF16) for _ in R2]; w3b = [k.sb([128, 8, 256], BF16) for _ in R2]; w2b = [k.sb([128, 2, D], BF16) for _ in R2]
    sa = [k.sb([128, 512]) for _ in R2]; actb = [[k.sb([128, 512], BF16) for _ in R2] for _ in R2]
    xt = [k.sb([128, D]) for _ in R2]; xn = k.sb([128, D]); junk = k.sb([128, D], BF16); tmpf = k.sb([128, 4, 128]); h32 = k.sb([128, 8, 128])
    yo = k.sb([128, D]); yo2 = k.sb([128, D])
    ss = k.sb([128, 1]); rr = k.sb([128, 1])
    lg = k.sb([128, 36]); gm = k.sb([128, 1]); ngm = k.sb([128, 1]); eg = k.sb([128, 4]); sgm = k.sb([128, 1]); gp = k.sb([128, 1])
    ohg = k.sb([128, 4]); t48 = k.sb([128, 4, 8]); sel = k.sb([128, 8]); sel2 = k.sb([128, 8]); m1 = k.sb([128, 1]); m2 = k.sb([128, 1])
    oh1 = k.sb([128, 8]); oh2 = k.sb([128, 8]); d21 = k.sb([128, 1]); e21 = k.sb([128, 1]); den = k.sb([128, 1]); wa = k.sb([128, 1]); wb_ = k.sb([128, 1])
    sw = k.sb([128, 8])

    def rms(src):
        k.act(junk[:], src, AF.Square, accum=ss[:])
        k.ts(rr[:], ss[:], 1.0 / D, ALU.mult, EPS, ALU.add)
        k.act(rr[:], rr[:], AF.Sqrt)
        k.recip(rr[:], rr[:])

    xi = 0
    for (p0, nt) in O2PASS:
        for li in range(nt):
            gi = p0 + li
            xi += 1
            x_ = xt[xi % 2]
            k.dma(x_[:], x1[gi * 128:(gi + 1) * 128, :])
            rms(x_[:])
            k.ts(xn[:], x_[:], rr[:, 0:1], ALU.mult)
            A_, B_ = (A_ctx, B_ctx) if gi == 32 else (A_lat, B_lat)
            for half in R2:
                tp = nps()
                for kk in range(4):
                    k.tr(tp[:, kk * 128:(kk + 1) * 128], xn[:, (half * 4 + kk) * 128:(half * 4 + kk + 1) * 128], idf[:], inc=(kk == 3))
                k.tt(tmpf[:], tp[:, :].re("p (k t) -> p k t", t=128),
                     View(A_, A_.t[:, half * 4:half * 4 + 4].unsqueeze(2).to_broadcast([128, 4, 128])), ALU.mult)
                k.tt(h32[:, half * 4:half * 4 + 4, :], tmpf[:],
                     View(B_, B_.t[:, half * 4:half * 4 + 4].unsqueeze(2).to_broadcast([128, 4, 128])), ALU.add)
            k.copy(hT[:, :, li * 128:(li + 1) * 128], h32[:], eng="pool")
            pr = nps()
            for kk in range(8):
                k.mm(pr[:, 0:36], h32[:, kk, :], wrt[:, kk, :], start=(kk == 0), stop=(kk == 7), inc=(kk == 7))
            k.tt(lg[:], pr[:, 0:36], brt[:], ALU.add)
            k.reduce(gm[:], lg[:, 0:4], ALU.max)
            k.ts(ngm[:], gm[:], -1.0, ALU.mult)
            k.act(eg[:], lg[:, 0:4], AF.Exp, bias=ngm[:, 0:1], accum=sgm[:])
            k.recip(gp[:], sgm[:])
            k.ts(ohg[:], lg[:, 0:4], gm[:, 0:1], ALU.is_equal)
            ohb = View(ohg, ohg.t[:, :].unsqueeze(2).to_broadcast([128, 4, 8]))
            k.tt(t48[:], lg[:, 4:36].re("p (g e) -> p g e", e=8), ohb, ALU.mult)
            k.reduce(sel[:], t48[:, :, :].re("p g e -> p e g"), ALU.add)
            k.reduce(m1[:], sel[:], ALU.max)
            k.ts(oh1[:], sel[:], m1[:, 0:1], ALU.is_equal)
            k.stt(sel2[:], oh1[:], -1.0e30, sel[:], ALU.mult, ALU.add)
            k.reduce(m2[:], sel2[:], ALU.max)
            k.ts(oh2[:], sel2[:], m2[:, 0:1], ALU.is_equal)
            k.tt(d21[:], m2[:], m1[:], ALU.subtract)
            k.act(e21[:], d21[:], AF.Exp)
            k.ts(den[:], e21[:], 1.0, ALU.add)
            k.recip(den[:], den[:])
            k.tt(wa[:], den[:], gp[:], ALU.mult)
            k.tt(wb_[:], wa[:], e21[:], ALU.mult)
            k.ts(sw[:], oh1[:], wa[:, 0:1], ALU.mult)
            k.stt(sw[:], oh2[:], wb_[:, 0:1], sw[:], ALU.mult, ALU.add)
            k.tt(comb[:, gi, :].re("p (g e) -> p g e", e=8), ohb, View(sw, sw.t[:, :].unsqueeze(1).to_broadcast([128, 4, 8])), ALU.mult)
        blocks = [(t0, min(4, nt - t0)) for t0 in range(0, nt, 4)]
        bi = 0
        pend = None
        for e in range(32):
            ws = e % 2
            k.dma(st1[:], w1[e].re("(k p) f -> p k f", p=128)); k.dma(st3[:], w3[e].re("(k p) f -> p k f", p=128))
            k.dma(st2[:], w2[e].re("(k p) f -> p k f", p=128))
            k.copy(w1b[ws][:], st1[:], eng="act"); k.copy(w3b[ws][:], st3[:], eng="pool"); k.copy(w2b[ws][:], st2[:], eng="pool")
            for (bt0, bnt) in blocks:
                bi += 1
                n = bnt * 128; c0 = bt0 * 128
                for ft in R2:
                    pa = nps(); pb = nps()
                    for kk in range(8):
                        k.mm(pa[:, :n], w1b[ws][:, kk, ft * 128:(ft + 1) * 128], hT[:, kk, c0:c0 + n], start=(kk == 0), stop=(kk == 7), inc=(kk == 7))
                    for kk in range(8):
                        k.mm(pb[:, :n], w3b[ws][:, kk, ft * 128:(ft + 1) * 128], hT[:, kk, c0:c0 + n], start=(kk == 0), stop=(kk == 7), inc=(kk == 7))
                    s_ = sa[ft]
                    k.act(s_[:, :n], pa[:, :n], AF.Silu)
                    k.tt(actb[bi % 2][ft][:, :n], pb[:, :n], s_[:, :n], ALU.mult)
                if pend is not None:
                    pend()

                def pend(e=e, ws=ws, bi=bi, bt0=bt0, bnt=bnt):
                    for t in range(bnt):
                        gi = p0 + bt0 + t
                        for hf in R2:
                            py = nps()
                            for ft in R2:
                                k.mm(py[:, :], actb[bi % 2][ft][:, t * 128:(t + 1) * 128], w2b[ws][:, ft, hf * 512:(hf + 1) * 512], start=(ft == 0), stop=(ft == 1), inc=(ft == 1))
                            ya = yacc[:, bt0 + t, hf * 512:(hf + 1) * 512]
                            if e == 0:
                                k.ts(ya, py[:, :], comb[:, gi, e:e + 1], ALU.mult)
                            else:
                                k.stt(ya, py[:, :], comb[:, gi, e:e + 1], ya, ALU.mult, ALU.add)
        pend()
        for li in range(nt):
            gi = p0 + li
            xi += 1
            x_ = xt[xi % 2]
            k.dma(x_[:], x1[gi * 128:(gi + 1) * 128, :])
            j = 1 if gi == 32 else 0
            k.tt(yo[:], yacc[:, li, :], g2t[:, j, :], ALU.mult)
            k.tt(yo[:], yo[:], x_[:], ALU.add, eng="pool")
            if last:
                rms(yo[:])
                k.stt(yo2[:], yo[:], rr[:, 0:1], fgt[:], ALU.mult, ALU.mult)
                k.dma(out[gi * 128:(gi + 1) * 128, :], yo2[:])
            else:
                k.dma(out[gi * 128:(gi + 1) * 128, :], yo[:])
    return k.finish()


def run_O2(x1s, mod_l, inp, l, last, cores=range(8)):
    wr = np.ascontiguousarray(np.concatenate([inp["moe_w_group"][l], inp["moe_w_expert"][l]], axis=1))
    brb = np.ascontiguousarray(bc128(np.concatenate([inp["moe_b_group"][l], inp["moe_b_expert"][l]])))
    w1 = inp["moe_w1"][l].reshape(32, D, 256); w3 = inp["moe_w3"][l].reshape(32, D, 256); w2 = inp["moe_w2"][l].reshape(32, 256, D)
    fgb = np.ascontiguousarray(bc128(inp["final_norm_g"])); ident = np.eye(128, dtype=np.float32)
    maps = []
    for i, core in enumerate(cores):
        b = core // 2
        modv = np.stack([pk(mod_l[3, :, b]), pk(mod_l[4, :, b]), pk(mod_l[3, :, 4]), pk(mod_l[4, :, 4])], axis=2)
        g2b = np.ascontiguousarray(np.stack([bc128(mod_l[5, :, b]), bc128(mod_l[5, :, 4])], axis=1))
        maps.append({"x1": x1s[i], "modv": np.ascontiguousarray(modv), "g2n": pk(inp["norm2_g"][l]), "g2b": g2b, "fgb": fgb,
                     "wr": wr, "brb": brb, "w1": w1, "w3": w3, "w2": w2, "ident": ident})
    res = run(build_O2(bool(last)), maps)
    return [r["x2"] for r in res]


def unshard(x2s):
    x = np.empty((B, L, D), np.float32); xc = np.empty((B, LC, D), np.float32)
    for core in range(8):
        b, h = core // 2, core % 2
        x[b, h * 4096:(h + 1) * 4096] = x2s[core][:4096]; xc[b, h * 128:(h + 1) * 128] = x2s[core][4096:]
    return x, xc


def kernel(**inp):
    inp = {k_: np.asarray(v_) for k_, v_ in inp.items()}
    mod = run_M(inp)
    x, xc = inp["x"], inp["ctx"]
    for l in range(2):
        xs = shard_tokens(x, xc)
        resA = run_A(x, xc, mod[l], inp, l)
        y5 = run_S5(gather_fm(resA, "uT"), inp, l)
        glf, glb = run_GLA(gather_fm(resA, "gqT"), gather_fm(resA, "gkT"), gather_fm(resA, "gaT"), gather_tm(resA, "gv"), inp, l)
        nao = run_NA(gather_fm(resA, "nqT"), gather_fm(resA, "nkT"), gather_tm(resA, "nv"), inp, l)
        cvo = run_CV(gather_fm(resA, "cyT"), inp, l)
        x1s = run_O1(xs, resA, y5, glf, glb, nao, cvo, mod[l], inp, l)
        del resA, y5, glf, glb, nao, cvo
        x2s = run_O2(x1s, mod[l], inp, l, l == 1)
        x, xc = unshard(x2s)
    return x
```
